# Optimizing a Trainium2 kernel written in Bass

```python
import math
import jax
import jax.numpy as jnp
from jax import lax
import numpy as np

D_MODEL = 1024
BATCH = 4
SEQ = 8192
DEPTH = 2

CTX_LEN = 256
GRID_W = 64

DN_HEADS = 4
DN_DK = 64
DN_DV = 64
DN_CONV = 5
DN_CHUNK = 64
DT_MIN = 0.001
DT_MAX = 0.1

S5_WIDTH = 256
S5_GROUP = 16
S5_GROUPS = S5_WIDTH // S5_GROUP
S5_STATE = 64

DA_HEADS = 4
DA_DK = 32
DA_DV = 2 * DA_DK
DA_QBLOCK = 128
ROPE_BASE = 10000.0

GLA_HEADS = 4
GLA_DK = 32
GLA_DV = 64
GLA_GATE_RANK = 16
GLA_TAU = 16.0
GLA_CHUNK = 16

N_BRANCH = 4
BRANCH_WIDTH = 256

N_EXPERTS = 16
N_GROUPS = 4
EXPERTS_PER_GROUP = N_EXPERTS // N_GROUPS
TOP_K = 2
D_EXPERT = 512
MOE_BLOCK = 128

NORM_EPS = 1e-6

PROJ_LAYOUT = (
    ("dn_qkv", DN_HEADS * (2 * DN_DK + DN_DV)),
    ("dn_z", DN_HEADS * DN_DV),
    ("dn_a", 2 * DN_HEADS),
    ("dn_b", 2 * DN_HEADS),
    ("s5_u", S5_WIDTH),
    ("da_q", DA_HEADS * 2 * DA_DK),
    ("da_k", DA_HEADS * 2 * DA_DK),
    ("da_v", DA_HEADS * DA_DV),
    ("gla_q", GLA_HEADS * GLA_DK),
    ("gla_k", GLA_HEADS * GLA_DK),
    ("gla_v", GLA_HEADS * GLA_DV),
    ("gla_r", GLA_HEADS * GLA_DV),
    ("gla_gate", 2 * GLA_GATE_RANK),
)
D_IN = sum(w for _, w in PROJ_LAYOUT)

kernel_name = "hybrid_flow_deltanet_s5_diffattn_gla_moe"

F32 = jnp.float32


def rmsnorm(x, g, eps=NORM_EPS):
    xf = x.astype(F32)
    y = xf * lax.rsqrt(jnp.mean(xf * xf, axis=-1, keepdims=True) + eps)
    return (y * g.astype(F32)).astype(x.dtype)


def l2norm(x, eps=1e-6):
    xf = x.astype(F32)
    return xf * lax.rsqrt(jnp.sum(xf * xf, axis=-1, keepdims=True) + eps)


def modulate(xn, shift, scale):
    return xn * (1.0 + scale) + shift


def split_proj(z):
    out, off = {}, 0
    for name, width in PROJ_LAYOUT:
        out[name] = z[..., off:off + width]
        off += width
    return out


def joint_order(ctx_part, lat_part, reverse):
    if reverse:
        ctx_part, lat_part = jnp.flip(ctx_part, 1), jnp.flip(lat_part, 1)
    return jnp.concatenate([ctx_part, lat_part], axis=1)


def split_order(y, n_ctx, reverse):
    y_ctx, y_lat = y[:, :n_ctx], y[:, n_ctx:]
    if reverse:
        y_ctx, y_lat = jnp.flip(y_ctx, 1), jnp.flip(y_lat, 1)
    return y_ctx, y_lat


def centred_dwconv(x, w):
    k = w.shape[0]
    return lax.conv_general_dilated(
        x, w[:, None, :].astype(x.dtype), window_strides=(1,), padding=[(k // 2, k // 2)],
        dimension_numbers=("NWC", "WIO", "NWC"), feature_group_count=x.shape[-1])


def axial_rope_tables(n_rows, head_dim):
    t = jnp.arange(n_rows * GRID_W)
    row = (t // GRID_W).astype(F32)
    col = (t % GRID_W).astype(F32)
    n_freq = head_dim // 4
    inv = ROPE_BASE ** (-jnp.arange(n_freq, dtype=F32) / n_freq)
    ang = jnp.concatenate([row[:, None] * inv, col[:, None] * inv], axis=-1)
    return jnp.cos(ang), jnp.sin(ang)


def apply_rope(x, cos, sin):
    half = x.shape[-1] // 2
    cos, sin = cos[:, None, None, :], sin[:, None, None, :]
    x1, x2 = x[..., :half], x[..., half:]
    return jnp.concatenate([x1 * cos - x2 * sin, x1 * sin + x2 * cos], axis=-1).astype(x.dtype)


def gated_delta_rule_chunked(q, k, v, g, beta):
    B, T, H, dk = q.shape
    dv = v.shape[-1]
    C = DN_CHUNK
    n = T // C

    def to_chunks(t):
        return jnp.moveaxis(t.astype(F32), 1, 2).reshape((B, H, n, C) + t.shape[3:])

    q = to_chunks(q) * dk ** -0.5
    k, v, beta = to_chunks(k), to_chunks(v), to_chunks(beta)
    g = jnp.cumsum(to_chunks(g), axis=-1)
    incl = jnp.tril(jnp.ones((C, C), bool))
    strict = jnp.tril(jnp.ones((C, C), bool), -1)
    decay = jnp.where(incl, jnp.exp(jnp.where(incl, g[..., :, None] - g[..., None, :], 0.0)), 0.0)
    kb = k * beta[..., None]
    lower = jnp.where(strict, jnp.einsum("bhnid,bhnjd->bhnij", kb, k) * decay, 0.0)
    rhs = jnp.concatenate([v * beta[..., None], kb * jnp.exp(g)[..., None]], axis=-1)
    sol = lax.linalg.triangular_solve(lower + jnp.eye(C, dtype=F32), rhs,
                                      left_side=True, lower=True, unit_diagonal=True)
    u, w = sol[..., :dv], sol[..., dv:]
    a_intra = jnp.where(incl, jnp.einsum("bhnid,bhnjd->bhnij", q, k) * decay, 0.0)
    q_dec = q * jnp.exp(g)[..., None]
    k_dec = k * jnp.exp(g[..., -1:] - g)[..., None]
    g_last = jnp.exp(g[..., -1])

    def step(state, xs):
        qd, kd, u_i, w_i, a_i, gl = xs
        v_new = u_i - w_i @ state
        o = qd @ state + a_i @ v_new
        state = state * gl[..., None, None] + jnp.einsum("bhcd,bhcv->bhdv", kd, v_new)
        return state, o

    xs = tuple(jnp.moveaxis(t, 2, 0) for t in (q_dec, k_dec, u, w, a_intra, g_last))
    _, o = lax.scan(step, jnp.zeros((B, H, dk, dv), F32), xs)
    return jnp.moveaxis(o, 0, 2).reshape(B, H, T, dv).transpose(0, 2, 1, 3)


def deltanet_mixer(p_lat, p_ctx, conv_w, a_log, dt_bias, norm_g, with_ctx):
    n_ctx = p_ctx["dn_qkv"].shape[1]

    def prep(p):
        qkv = jax.nn.silu(centred_dwconv(p["dn_qkv"], conv_w))
        b_, t_ = qkv.shape[:2]
        q, k, v = jnp.split(qkv, [DN_HEADS * DN_DK, 2 * DN_HEADS * DN_DK], axis=-1)
        q = l2norm(q.reshape(b_, t_, DN_HEADS, DN_DK))
        k = l2norm(k.reshape(b_, t_, DN_HEADS, DN_DK))
        return q, k, v.reshape(b_, t_, DN_HEADS, DN_DV)

    ql, kl, vl = prep(p_lat)
    qc, kc, vc = prep(p_ctx)
    o_lat, o_ctx = 0.0, 0.0
    for d in range(2):
        rev = d == 1
        cols = slice(d * DN_HEADS, (d + 1) * DN_HEADS)

        def gates(p):
            a = p["dn_a"][..., cols].astype(F32)
            b = p["dn_b"][..., cols].astype(F32)
            g = -jnp.exp(a_log[d].astype(F32)) * jax.nn.softplus(a + dt_bias[d].astype(F32))
            return g, jax.nn.sigmoid(b)

        gl, bl = gates(p_lat)
        gc, bc = gates(p_ctx)
        o = gated_delta_rule_chunked(joint_order(qc, ql, rev), joint_order(kc, kl, rev),
                                     joint_order(vc, vl, rev), joint_order(gc, gl, rev),
                                     joint_order(bc, bl, rev))
        oc, ol = split_order(o, n_ctx, rev)
        o_lat, o_ctx = o_lat + ol, o_ctx + oc

    def finish(o, p):
        z = p["dn_z"].reshape(o.shape).astype(F32)
        return (rmsnorm(o, norm_g) * jax.nn.silu(z)).reshape(o.shape[0], o.shape[1], DN_HEADS * DN_DV)

    return finish(o_lat, p_lat), (finish(o_ctx, p_ctx) if with_ctx else None)


def complex_affine_combine(e1, e2):
    a1r, a1i, b1r, b1i = e1
    a2r, a2i, b2r, b2i = e2
    return (a2r * a1r - a2i * a1i, a2r * a1i + a2i * a1r,
            a2r * b1r - a2i * b1i + b2r, a2r * b1i + a2i * b1r + b2i)


def s5_mixer(u_lat, u_ctx, a_re, a_im, log_dt, b_re, b_im, c_re, c_im, d_skip, glu_w, glu_b, with_ctx):
    n_ctx = u_ctx.shape[1]

    def grp(u):
        return u.astype(F32).reshape(u.shape[0], u.shape[1], S5_GROUPS, S5_GROUP)

    ul, uc = grp(u_lat), grp(u_ctx)
    br, bi = b_re.astype(F32), b_im.astype(F32)
    y_lat, y_ctx = 0.0, 0.0
    for d in range(2):
        rev = d == 1
        lam_re, lam_im = a_re[d].astype(F32), a_im[d].astype(F32)
        dt = jnp.exp(log_dt[d].astype(F32))[:, None]
        mag = jnp.exp(lam_re * dt)
        ab_re, ab_im = mag * jnp.cos(lam_im * dt), mag * jnp.sin(lam_im * dt)
        den = lam_re * lam_re + lam_im * lam_im
        f_re = ((ab_re - 1.0) * lam_re + ab_im * lam_im) / den
        f_im = (ab_im * lam_re - (ab_re - 1.0) * lam_im) / den
        bb_re = f_re[..., None] * br - f_im[..., None] * bi
        bb_im = f_re[..., None] * bi + f_im[..., None] * br
        u = joint_order(uc, ul, rev)
        bu_re = jnp.einsum("btgc,gpc->btgp", u, bb_re)
        bu_im = jnp.einsum("btgc,gpc->btgp", u, bb_im)
        a_r = jnp.broadcast_to(ab_re, bu_re.shape)
        a_i = jnp.broadcast_to(ab_im, bu_re.shape)
        _, _, x_re, x_im = lax.associative_scan(complex_affine_combine, (a_r, a_i, bu_re, bu_im), axis=1)
        y = (jnp.einsum("btgp,gcp->btgc", x_re, c_re[d].astype(F32))
             - jnp.einsum("btgp,gcp->btgc", x_im, c_im[d].astype(F32)))
        yc, yl = split_order(y, n_ctx, rev)
        y_lat, y_ctx = y_lat + yl, y_ctx + yc

    def finish(y, u):
        y = y.reshape(y.shape[0], y.shape[1], S5_WIDTH) + d_skip.astype(F32) * u.astype(F32)
        z = jax.nn.gelu(y)
        return z * jax.nn.sigmoid(z @ glu_w.astype(F32) + glu_b.astype(F32))

    return finish(y_lat, u_lat), (finish(y_ctx, u_ctx) if with_ctx else None)


def diff_core(q, k, v, lam):
    s = jnp.einsum("bhmqd,bhmkd->bhmqk", q, k).astype(F32) * (q.shape[-1] ** -0.5)
    p = jax.nn.softmax(s, axis=-1)
    a = p[:, :, 0] - lam * p[:, :, 1]
    return jnp.einsum("bhqk,bhkv->bhqv", a, v.astype(F32))


def diff_attention(q_lat, k_lat, v_lat, q_ctx, k_ctx, v_ctx, lam_vec, norm_g, lam_init, cos, sin, with_ctx):
    b_, s_ = q_lat.shape[:2]

    def heads(t):
        return t.reshape(t.shape[0], t.shape[1], DA_HEADS, 2, DA_DK)

    def to_bhm(t):
        return t.transpose(0, 2, 3, 1, 4)

    def vheads(t):
        return t.reshape(t.shape[0], t.shape[1], DA_HEADS, DA_DV).transpose(0, 2, 1, 3)

    lf = lam_vec.astype(F32)
    lam = jnp.exp(jnp.sum(lf[0] * lf[1])) - jnp.exp(jnp.sum(lf[2] * lf[3])) + lam_init
    ql = apply_rope(heads(q_lat), cos, sin)
    kl = apply_rope(heads(k_lat), cos, sin)
    kc = to_bhm(heads(k_ctx))
    vc = vheads(v_ctx)
    k_all = jnp.concatenate([to_bhm(kl), kc], axis=3)
    v_all = jnp.concatenate([vheads(v_lat), vc], axis=2)
    nb = s_ // DA_QBLOCK
    qb = jnp.moveaxis(to_bhm(ql).reshape(b_, DA_HEADS, 2, nb, DA_QBLOCK, DA_DK), 3, 0)
    o = lax.map(lambda qblk: diff_core(qblk, k_all, v_all, lam), qb)
    o = jnp.moveaxis(o, 0, 2).reshape(b_, DA_HEADS, s_, DA_DV).transpose(0, 2, 1, 3)

    def finish(o):
        o = rmsnorm(o, norm_g, eps=1e-5) * (1.0 - lam_init)
        return o.reshape(o.shape[0], o.shape[1], DA_HEADS * DA_DV)

    y_ctx = None
    if with_ctx:
        oc = diff_core(to_bhm(heads(q_ctx)), kc, vc, lam).transpose(0, 2, 1, 3)
        y_ctx = finish(oc)
    return finish(o), y_ctx


def gla_chunked(q, k, v, log_a):
    B, T, H, dk = q.shape
    dv = v.shape[-1]
    C = GLA_CHUNK
    n = T // C

    def to_chunks(t):
        return jnp.moveaxis(t.astype(F32), 1, 2).reshape(B, H, n, C, t.shape[-1])

    q = to_chunks(q) * dk ** -0.5
    k, v = to_chunks(k), to_chunks(v)
    b = jnp.cumsum(to_chunks(log_a), axis=3)
    incl = jnp.tril(jnp.ones((C, C), bool))[..., None]
    rel = b[..., :, None, :] - b[..., None, :, :]
    decay = jnp.exp(jnp.where(incl, rel, -jnp.inf))
    a_intra = jnp.einsum("bhnid,bhnjd,bhnijd->bhnij", q, k, decay)
    q_dec = q * jnp.exp(b)
    k_dec = k * jnp.exp(b[..., -1:, :] - b)
    a_last = jnp.exp(b[..., -1, :])

    def step(state, xs):
        qd, kd, v_i, a_i, al = xs
        o = qd @ state + a_i @ v_i
        state = state * al[..., :, None] + jnp.einsum("bhcd,bhcv->bhdv", kd, v_i)
        return state, o

    xs = tuple(jnp.moveaxis(t, 2, 0) for t in (q_dec, k_dec, v, a_intra, a_last))
    _, o = lax.scan(step, jnp.zeros((B, H, dk, dv), F32), xs)
    return jnp.moveaxis(o, 0, 2).reshape(B, H, T, dv).transpose(0, 2, 1, 3)


def gla_mixer(p_lat, p_ctx, w_gate2, b_gate2, norm_g, with_ctx):
    n_ctx = p_ctx["gla_q"].shape[1]

    def heads(t, dim):
        return t.reshape(t.shape[0], t.shape[1], GLA_HEADS, dim)

    ql, kl, vl = heads(p_lat["gla_q"], GLA_DK), heads(p_lat["gla_k"], GLA_DK), heads(p_lat["gla_v"], GLA_DV)
    qc, kc, vc = heads(p_ctx["gla_q"], GLA_DK), heads(p_ctx["gla_k"], GLA_DK), heads(p_ctx["gla_v"], GLA_DV)
    o_lat, o_ctx = 0.0, 0.0
    for d in range(2):
        rev = d == 1

        def log_forget(p):
            low = p["gla_gate"][..., d * GLA_GATE_RANK:(d + 1) * GLA_GATE_RANK].astype(F32)
            z = low @ w_gate2[d].astype(F32) + b_gate2[d].astype(F32)
            return heads(jax.nn.log_sigmoid(z) / GLA_TAU, GLA_DK)

        o = gla_chunked(joint_order(qc, ql, rev), joint_order(kc, kl, rev), joint_order(vc, vl, rev),
                        joint_order(log_forget(p_ctx), log_forget(p_lat), rev))
        oc, ol = split_order(o, n_ctx, rev)
        o_lat, o_ctx = o_lat + ol, o_ctx + oc

    def finish(o, p):
        r = heads(p["gla_r"], GLA_DV).astype(F32)
        return (rmsnorm(o, norm_g) * jax.nn.silu(r)).reshape(o.shape[0], o.shape[1], GLA_HEADS * GLA_DV)

    return finish(o_lat, p_lat), (finish(o_ctx, p_ctx) if with_ctx else None)


def merge_branches(a, ys, w_branch, w_gate, b_gate, w_out):
    dm = a.shape[-1]
    acc = 0.0
    for i, y in enumerate(ys):
        gate = jax.nn.sigmoid(a @ w_gate[:, i * dm:(i + 1) * dm] + b_gate[i * dm:(i + 1) * dm])
        acc = acc + gate * (y.astype(a.dtype) @ w_branch[i])
    return acc @ w_out


def moe_ffn(h, w_router, b_router, w_e_gate, w_e_up, w_e_down):
    n_tok, dm = h.shape
    scores = jax.nn.sigmoid((h @ w_router).astype(F32))
    grouped = (scores + b_router.astype(F32)).reshape(n_tok, N_GROUPS, EXPERTS_PER_GROUP)
    group_score = jnp.sum(lax.top_k(grouped, TOP_K)[0], axis=-1)
    best = jnp.argmax(group_score, axis=-1)
    cand = grouped[jnp.arange(n_tok), best]
    _, local = lax.top_k(cand, TOP_K)
    expert = (best[:, None] * EXPERTS_PER_GROUP + local).astype(jnp.int32)
    weight = jnp.take_along_axis(scores, expert, axis=1)
    weight = weight / jnp.sum(weight, axis=-1, keepdims=True)
    n_pair = n_tok * TOP_K
    flat_e = expert.reshape(n_pair)
    order = jnp.argsort(flat_e)
    counts = jnp.bincount(flat_e, length=N_EXPERTS)
    padded = (counts + MOE_BLOCK - 1) // MOE_BLOCK * MOE_BLOCK
    pad_end = jnp.cumsum(padded)
    start = jnp.cumsum(counts) - counts
    sorted_e = flat_e[order]
    slot_sorted = pad_end[sorted_e] - padded[sorted_e] + jnp.arange(n_pair) - start[sorted_e]
    slot = jnp.zeros((n_pair,), jnp.int32).at[order].set(slot_sorted.astype(jnp.int32))
    n_blocks = -(-n_pair // MOE_BLOCK) + N_EXPERTS
    token_of_pair = jnp.arange(n_pair) // TOP_K
    buf = jnp.zeros((n_blocks * MOE_BLOCK, dm), h.dtype).at[slot].set(h[token_of_pair])
    block_expert = jnp.minimum(
        jnp.searchsorted(pad_end, jnp.arange(n_blocks) * MOE_BLOCK, side="right"), N_EXPERTS - 1)

    def expert_block(args):
        rows, e = args
        hid = jax.nn.silu(rows @ w_e_gate[e]) * (rows @ w_e_up[e])
        return hid @ w_e_down[e]

    out = lax.map(expert_block, (buf.reshape(n_blocks, MOE_BLOCK, dm), block_expert)).reshape(-1, dm)
    pair_out = out[slot].reshape(n_tok, TOP_K, dm)
    return jnp.einsum("nk,nkd->nd", weight.astype(h.dtype), pair_out)


def setup_inputs(seed: int = 0) -> dict:
    key = jax.random.key(seed)
    keys = iter(jax.random.split(key, 48))

    def normal(shape, scale):
        return jax.random.normal(next(keys), shape, F32) * scale

    def gain(shape):
        return 1.0 + normal(shape, 0.05)

    def uniform(shape, lo, hi):
        return jax.random.uniform(next(keys), shape, F32, minval=lo, maxval=hi)

    dm, ly, ne, fe = D_MODEL, DEPTH, N_EXPERTS, D_EXPERT
    g5, p5, n5 = S5_GROUPS, S5_STATE, S5_GROUP
    dn_dt = jnp.exp(uniform((ly, 2, DN_HEADS), math.log(DT_MIN), math.log(DT_MAX)))
    return {
        "x": normal((BATCH, SEQ, dm), 1.0),
        "c": normal((BATCH, dm), 1.0),
        "ctx": normal((BATCH, CTX_LEN, dm), 1.0),
        "c_ctx": normal((dm,), 1.0),
        "w_mod": normal((ly, dm, 6 * dm), 0.5 * dm ** -0.5),
        "b_mod": normal((ly, 6 * dm), 0.01),
        "norm1_g": gain((ly, dm)),
        "norm2_g": gain((ly, dm)),
        "w_in": normal((ly, dm, D_IN), dm ** -0.5),
        "dn_conv": normal((ly, DN_CONV, DN_HEADS * (2 * DN_DK + DN_DV)), DN_CONV ** -0.5),
        "dn_a_log": jnp.log(uniform((ly, 2, DN_HEADS), 1.0, 16.0)),
        "dn_dt_bias": dn_dt + jnp.log(-jnp.expm1(-dn_dt)),
        "dn_norm_g": gain((ly, DN_DV)),
        "s5_a_re": -0.5 + normal((ly, 2, g5, p5), 0.01),
        "s5_a_im": math.pi * jnp.arange(p5, dtype=F32) + normal((ly, 2, g5, p5), 0.01),
        "s5_log_dt": uniform((ly, 2, g5), math.log(DT_MIN), math.log(DT_MAX)),
        "s5_b_re": normal((ly, g5, p5, n5), (2 * n5) ** -0.5),
        "s5_b_im": normal((ly, g5, p5, n5), (2 * n5) ** -0.5),
        "s5_c_re": normal((ly, 2, g5, n5, p5), (2 * p5) ** -0.5),
        "s5_c_im": normal((ly, 2, g5, n5, p5), (2 * p5) ** -0.5),
        "s5_d": normal((ly, S5_WIDTH), 1.0),
        "s5_glu_w": normal((ly, S5_WIDTH, S5_WIDTH), S5_WIDTH ** -0.5),
        "s5_glu_b": normal((ly, S5_WIDTH), 0.01),
        "da_lambda": normal((ly, 4, DA_DK), 0.1),
        "da_norm_g": gain((ly, DA_DV)),
        "gla_w_gate": normal((ly, 2, GLA_GATE_RANK, GLA_HEADS * GLA_DK), GLA_GATE_RANK ** -0.5),
        "gla_b_gate": normal((ly, 2, GLA_HEADS * GLA_DK), 0.1),
        "gla_norm_g": gain((ly, GLA_DV)),
        "w_branch": normal((ly, N_BRANCH, BRANCH_WIDTH, dm), BRANCH_WIDTH ** -0.5),
        "w_gate": normal((ly, dm, N_BRANCH * dm), dm ** -0.5),
        "b_gate": normal((ly, N_BRANCH * dm), 0.01),
        "w_out": normal((ly, dm, dm), dm ** -0.5),
        "w_router": normal((dm, ne), dm ** -0.5),
        "b_router": normal((ne,), 0.01),
        "w_e_gate": normal((ly, ne, dm, fe), dm ** -0.5),
        "w_e_up": normal((ly, ne, dm, fe), dm ** -0.5),
        "w_e_down": normal((ly, ne, fe, dm), fe ** -0.5),
        "final_g": gain((dm,)),
    }


def reference(x, c, ctx, c_ctx, w_mod, b_mod, norm1_g, norm2_g, w_in,
              dn_conv, dn_a_log, dn_dt_bias, dn_norm_g,
              s5_a_re, s5_a_im, s5_log_dt, s5_b_re, s5_b_im, s5_c_re, s5_c_im, s5_d, s5_glu_w, s5_glu_b,
              da_lambda, da_norm_g,
              gla_w_gate, gla_b_gate, gla_norm_g,
              w_branch, w_gate, b_gate, w_out,
              w_router, b_router, w_e_gate, w_e_up, w_e_down, final_g):
    b_, s_, dm = x.shape
    n_ctx = ctx.shape[1]
    ROWS = s_ // GRID_W
    cos, sin = axial_rope_tables(ROWS, DA_DK)
    sc_lat = jax.nn.silu(c)[:, None, :]
    sc_ctx = jax.nn.silu(c_ctx)[None, None, :]
    h_lat, h_ctx = x, ctx
    for layer in range(DEPTH):
        with_ctx = layer < DEPTH - 1
        lam_init = 0.8 - 0.6 * math.exp(-0.3 * layer)
        mod_lat = jnp.split(sc_lat @ w_mod[layer] + b_mod[layer], 6, axis=-1)
        mod_ctx = jnp.split(sc_ctx @ w_mod[layer] + b_mod[layer], 6, axis=-1)

        a_lat = modulate(rmsnorm(h_lat, norm1_g[layer]), mod_lat[0], mod_lat[1])
        a_ctx = modulate(rmsnorm(h_ctx, norm1_g[layer]), mod_ctx[0], mod_ctx[1])
        p_lat = split_proj(a_lat @ w_in[layer])
        p_ctx = split_proj(a_ctx @ w_in[layer])
        y_a = deltanet_mixer(p_lat, p_ctx, dn_conv[layer], dn_a_log[layer], dn_dt_bias[layer],
                             dn_norm_g[layer], with_ctx)
        y_b = s5_mixer(p_lat["s5_u"], p_ctx["s5_u"], s5_a_re[layer], s5_a_im[layer], s5_log_dt[layer],
                       s5_b_re[layer], s5_b_im[layer], s5_c_re[layer], s5_c_im[layer], s5_d[layer],
                       s5_glu_w[layer], s5_glu_b[layer], with_ctx)
        y_c = diff_attention(p_lat["da_q"], p_lat["da_k"], p_lat["da_v"],
                             p_ctx["da_q"], p_ctx["da_k"], p_ctx["da_v"],
                             da_lambda[layer], da_norm_g[layer], lam_init, cos, sin, with_ctx)
        y_d = gla_mixer(p_lat, p_ctx, gla_w_gate[layer], gla_b_gate[layer], gla_norm_g[layer], with_ctx)
        merge_w = (w_branch[layer], w_gate[layer], b_gate[layer], w_out[layer])
        h_lat = h_lat + mod_lat[2] * merge_branches(a_lat, (y_a[0], y_b[0], y_c[0], y_d[0]), *merge_w)

        expert_w = (w_e_gate[layer], w_e_up[layer], w_e_down[layer])
        f_lat = modulate(rmsnorm(h_lat, norm2_g[layer]), mod_lat[3], mod_lat[4])
        if with_ctx:
            h_ctx = h_ctx + mod_ctx[2] * merge_branches(a_ctx, (y_a[1], y_b[1], y_c[1], y_d[1]), *merge_w)
            f_ctx = modulate(rmsnorm(h_ctx, norm2_g[layer]), mod_ctx[3], mod_ctx[4])
            tokens = jnp.concatenate([f_ctx.reshape(-1, dm), f_lat.reshape(-1, dm)], axis=0)
            ffn = moe_ffn(tokens, w_router, b_router, *expert_w)
            h_ctx = h_ctx + mod_ctx[5] * ffn[:b_ * n_ctx].reshape(b_, n_ctx, dm)
            h_lat = h_lat + mod_lat[5] * ffn[b_ * n_ctx:].reshape(b_, s_, dm)
        else:
            ffn = moe_ffn(f_lat.reshape(-1, dm), w_router, b_router, *expert_w)
            h_lat = h_lat + mod_lat[5] * ffn.reshape(b_, s_, dm)
    return rmsnorm(h_lat, final_g)
```

```python
import contextlib
import numpy as np
import concourse.bass as bass
import concourse.mybir as mybir

F32 = mybir.dt.float32
BF16 = mybir.dt.bfloat16
I32 = mybir.dt.int32
U32 = mybir.dt.uint32
AF = mybir.ActivationFunctionType
ALU = mybir.AluOpType
AX = mybir.AxisListType

NDMA_SEMS = 40
NDMA_HW = 24
DBG = {}


def _box(ap):
    t = ap.tensor
    pat = ap.ap
    off = int(ap.offset)
    space = str(ap.space)
    if space == "PSUM":
        return (t.name, 0, 128, 0, 1 << 30)
    if space in ("SB", "PSUM"):
        psz = 1
        for s in list(t.shape)[1:]:
            psz *= int(s)
        p0 = off // psz
        f0 = off % psz
        st0, n0 = pat[0]
        pstep = (st0 // psz) if psz else 1
        p1 = p0 + (n0 - 1) * max(pstep, 0) + 1
        lo, hi = f0, f0
        for st, n in pat[1:]:
            if st < 0:
                lo += (n - 1) * st
            else:
                hi += (n - 1) * st
        return (t.name, p0, p1, lo, hi + 1)
    lo, hi = off, off
    for st, n in pat:
        if st < 0:
            lo += (n - 1) * st
        else:
            hi += (n - 1) * st
    return (t.name, 0, 1, lo, hi + 1)


class FW:
    def __init__(self, nc, stack, same_engine_waits=True):
        self.nc = nc
        self.stack = stack
        self.E = dict(pe=nc.tensor, act=nc.scalar, dve=nc.vector, pool=nc.gpsimd, sp=nc.sync)
        self.csem = {}
        self.ccnt = {}
        for e in ("pe", "act", "dve", "pool"):
            self.csem[e] = stack.enter_context(nc.semaphore("c_" + e))
            self.ccnt[e] = 0
        self.dsem = [stack.enter_context(nc.semaphore("d%d" % i)) for i in range(NDMA_SEMS)]
        self.dval = [0] * NDMA_SEMS
        self.dnext = 0
        self.dnext_sw = 0
        self.known = {q: {} for q in self.E}
        self.reg = {}
        self.same = same_engine_waits
        self.n_inst = 0
        self.n_wait = 0
        self.out_events = []
        self._uid = 0

    def sb(self, name, shape, dtype=F32):
        self._uid += 1
        return self.stack.enter_context(self.nc.sbuf_tensor("%s_%d" % (name, self._uid), list(shape), dtype))

    def ps(self, name, shape, dtype=F32):
        self._uid += 1
        return self.stack.enter_context(self.nc.psum_tensor("%s_%d" % (name, self._uid), list(shape), dtype))

    def _wait(self, q, ev):
        sem, val = ev
        k = id(sem)
        if self.known[q].get(k, 0) >= val:
            return
        self.E[q].wait_ge(sem, val)
        self.known[q][k] = val
        self.n_wait += 1

    def _deps(self, q, reads, writes):
        deps = []
        for ap in reads:
            b = _box(ap)
            for ent in self.reg.get(b[0], ()):
                eb = ent[0]
                if eb[1] < b[2] and b[1] < eb[2] and eb[3] < b[4] and b[3] < eb[4]:
                    if ent[1] is not None:
                        deps.append(ent[1])
        for ap in writes:
            b = _box(ap)
            for ent in self.reg.get(b[0], ()):
                eb = ent[0]
                if eb[1] < b[2] and b[1] < eb[2] and eb[3] < b[4] and b[3] < eb[4]:
                    if ent[1] is not None:
                        deps.append(ent[1])
                    deps.extend(ent[2].values())
        for ev in deps:
            if (not self.same or q == "pe") and q in self.csem and ev[0] is self.csem[q]:
                continue
            self._wait(q, ev)

    def _record(self, ev, reads, writes):
        for ap in reads:
            b = _box(ap)
            lst = self.reg.setdefault(b[0], [])
            for ent in lst:
                if ent[0] == b:
                    ent[2][id(ev[0])] = ev
                    break
            else:
                lst.append([b, None, {id(ev[0]): ev}])
        for ap in writes:
            b = _box(ap)
            lst = self.reg.setdefault(b[0], [])
            keep = []
            for ent in lst:
                eb = ent[0]
                if eb[1] >= b[1] and eb[2] <= b[2] and eb[3] >= b[3] and eb[4] <= b[4]:
                    continue
                keep.append(ent)
            keep.append([b, ev, {}])
            self.reg[b[0]] = keep

    def op(self, q, fn, reads, writes):
        reads = [r for r in reads if r is not None and not isinstance(r, (int, float))]
        self._deps(q, reads, writes)
        ins = fn()
        self.ccnt[q] += 1
        ev = (self.csem[q], self.ccnt[q])
        ins.then_inc(self.csem[q], 1)
        if not self.same or q == "pe":
            self.known[q][id(self.csem[q])] = self.ccnt[q]
        self._record(ev, reads, writes)
        self.n_inst += 1
        return ins

    def dma(self, out, in_, q="sp", is_output=False, **kw):
        if q == "pool":
            i = NDMA_HW + (self.dnext_sw % (NDMA_SEMS - NDMA_HW))
            self.dnext_sw += 1
        else:
            i = self.dnext % NDMA_HW
            self.dnext += 1
        if self.dval[i] > 0:
            self._wait(q, (self.dsem[i], self.dval[i]))
        self._deps(q, [in_], [out])
        ins = self.E[q].dma_start(out=out, in_=in_, **kw)
        self.dval[i] += 16
        ev = (self.dsem[i], self.dval[i])
        ins.then_inc(self.dsem[i], 16)
        self._record(ev, [in_], [out])
        if is_output:
            self.out_events.append(ev)
        self.n_inst += 1
        return ins

    def cc(self, kind, ins, outs, groups, op=None):
        q = "pool"
        i = NDMA_HW + (self.dnext_sw % (NDMA_SEMS - NDMA_HW))
        self.dnext_sw += 1
        if self.dval[i] > 0:
            self._wait(q, (self.dsem[i], self.dval[i]))
        self._deps(q, list(ins), list(outs))
        inst = self.nc.gpsimd.collective_compute(kind, op if op is not None else ALU.bypass, replica_groups=groups, ins=list(ins), outs=list(outs))
        self.dval[i] += 16
        ev = (self.dsem[i], self.dval[i])
        inst.then_inc(self.dsem[i], 16)
        self._record(ev, list(ins), list(outs))
        self.n_inst += 1
        return inst

    def barrier(self):
        evs = [(self.csem[e], self.ccnt[e]) for e in self.csem if self.ccnt[e] > 0]
        evs += [(self.dsem[i], self.dval[i]) for i in range(NDMA_SEMS) if self.dval[i] > 0]
        for q in self.E:
            for ev in evs:
                self._wait(q, ev)
        self.reg = {}

    def finish(self):
        evs = [(self.dsem[i], self.dval[i]) for i in range(NDMA_SEMS) if self.dval[i] > 0]
        evs += [(self.csem[e], self.ccnt[e]) for e in self.csem if self.ccnt[e] > 0]
        for ev in evs:
            self._wait("sp", ev)

    def mm(self, out, lhsT, rhs, start=True, stop=True, **kw):
        return self.op("pe", lambda: self.nc.tensor.matmul(out, lhsT, rhs, start=start, stop=stop, **kw),
                       [lhsT, rhs] + ([] if start else [out]), [out])

    def tr(self, out, in_, ident):
        return self.op("pe", lambda: self.nc.tensor.transpose(out, in_, ident), [in_, ident], [out])

    def act(self, out, in_, func, bias=None, scale=None, accum_out=None, q="act"):
        kw = {}
        if bias is not None:
            kw["bias"] = bias
        if scale is not None:
            kw["scale"] = scale
        if accum_out is not None:
            kw["accum_out"] = accum_out
        rd = [in_, bias, scale]
        wr = [out] + ([accum_out] if accum_out is not None else [])
        return self.op(q, lambda: self.nc.scalar.activation(out, in_, func, **kw), rd, wr)

    def tt(self, out, a, b, op, q="dve"):
        return self.op(q, lambda: self.E[q].tensor_tensor(out, a, b, op), [a, b], [out])

    def ts(self, out, a, s1, s2=None, op0=ALU.mult, op1=None, accum_out=None, q="dve"):
        kw = {}
        if op1 is not None:
            kw["op1"] = op1
        if accum_out is not None:
            kw["accum_out"] = accum_out
        wr = [out] + ([accum_out] if accum_out is not None else [])
        return self.op(q, lambda: self.E[q].tensor_scalar(out, a, s1, s2, op0, **kw), [a, s1, s2], wr)

    def stt(self, out, in0, scalar, in1, op0, op1, accum_out=None):
        kw = {}
        if accum_out is not None:
            kw["accum_out"] = accum_out
        wr = [out] + ([accum_out] if accum_out is not None else [])
        return self.op("dve", lambda: self.nc.vector.scalar_tensor_tensor(out, in0, scalar, in1, op0, op1, **kw),
                       [in0, scalar, in1], wr)

    def copy(self, out, in_, q="dve"):
        if q == "act":
            return self.op("act", lambda: self.nc.scalar.copy(out, in_), [in_], [out])
        return self.op(q, lambda: self.E[q].tensor_copy(out, in_), [in_], [out])

    def memset(self, out, val, q="dve"):
        return self.op(q, lambda: self.E[q].memset(out, val), [], [out])

    def reduce(self, out, in_, op, axis=AX.X, q="dve", **kw):
        return self.op(q, lambda: self.E[q].tensor_reduce(out, in_, axis, op, **kw), [in_], [out])

    def recip(self, out, in_):
        return self.op("dve", lambda: self.nc.vector.reciprocal(out, in_), [in_], [out])

    def scan(self, out, d0, d1, initial, op0=ALU.mult, op1=ALU.add):
        rd = [d0, d1, initial]
        return self.op("dve", lambda: self.nc.vector.tensor_tensor_scan(out, d0, d1, initial, op0, op1), rd, [out])


@contextlib.contextmanager
def fw_scope(fw):
    outer = fw.stack
    with contextlib.ExitStack() as st:
        fw.stack = st
        try:
            yield
        finally:
            fw.barrier()
            fw.stack = outer


D_MODEL = 1024
DN_CHUNK = 64
NORM_EPS = 1e-6
_PROJ = (("dn_qkv", 768), ("dn_z", 256), ("dn_a", 8), ("dn_b", 8), ("s5_u", 256), ("da_q", 256), ("da_k", 256),
         ("da_v", 256), ("gla_q", 128), ("gla_k", 128), ("gla_v", 256), ("gla_r", 256), ("gla_gate", 32))
_OFF = {}
_o = 0
for _n, _w in _PROJ:
    _OFF[_n] = _o
    _o += _w
D_IN = _o
NCOL = 12 * 128
T_DNQ, T_DNK, T_DNV, T_DNZ, T_S5U, T_DAQ, T_DAK, T_DAV, T_GQK, T_GV, T_GR, T_MISC = range(12)


def my_cols(j):
    hs = (2 * j, 2 * j + 1)
    ar = np.arange
    cols = []
    cols += [_OFF["dn_qkv"] + h * 64 + ar(64) for h in hs]
    cols += [_OFF["dn_qkv"] + 256 + h * 64 + ar(64) for h in hs]
    cols += [_OFF["dn_qkv"] + 512 + h * 64 + ar(64) for h in hs]
    cols += [_OFF["dn_z"] + h * 64 + ar(64) for h in hs]
    cols += [_OFF["s5_u"] + j * 128 + ar(128)]
    cols += [_OFF["da_q"] + h * 64 + ar(64) for h in hs]
    cols += [_OFF["da_k"] + h * 64 + ar(64) for h in hs]
    cols += [_OFF["da_v"] + h * 64 + ar(64) for h in hs]
    cols += [_OFF["gla_q"] + h * 32 + ar(32) for h in hs]
    cols += [_OFF["gla_k"] + h * 32 + ar(32) for h in hs]
    cols += [_OFF["gla_v"] + h * 64 + ar(64) for h in hs]
    cols += [_OFF["gla_r"] + h * 64 + ar(64) for h in hs]
    misc = -np.ones(128, np.int64)
    misc[0:16] = _OFF["gla_gate"] + ar(16)
    misc[32:48] = _OFF["gla_gate"] + 16 + ar(16)
    k = 64
    for nm in ("dn_a", "dn_b"):
        for d in range(2):
            for h in hs:
                misc[k] = _OFF[nm] + d * 4 + h
                k += 1
    cols.append(misc)
    cols = np.concatenate(cols)
    assert cols.shape[0] == NCOL
    return cols


class Cfg:
    def __init__(self, tc=256, tl=8192):
        self.TC = tc
        self.TL = tl
        self.T = tc + tl
        assert tc % 128 == 0 and tl % 128 == 0


def phase_mod(fw, c_col, wmod, bmod, lo, hi, out, ps, wbufs=None):
    n = hi - lo
    fw.dma(out[:, 0:n], bmod[lo:hi].partition_broadcast(128))
    with fw_scope(fw):
        sc = fw.sb("sc", [128, 8])
        rep = fw.sb("screp", [128, 8, 128])
        sg = fw.sb("scsg", [128, 8])
        wb2 = [fw.sb("wmodb%d" % i, [128, 8, 256]) for i in range(2)]
        fw.dma(sc[:], c_col)
        fw.act(sg[:], sc[:], AF.Sigmoid)
        fw.tt(sc[:], sc[:], sg[:], ALU.mult)
        fw.copy(rep[:], sc[:].unsqueeze(2).to_broadcast([128, 8, 128]))
        wv = wmod.rearrange("(k p) n -> p k n", p=128)
        i = 0
        for c0 in range(0, n, 256):
            wb = wb2[i % 2]
            fw.dma(wb[:], wv[:, :, lo + c0:lo + c0 + 256], q=("sp" if i % 2 == 0 else "pool"))
            for k in range(8):
                fw.mm(ps[:, :256], rep[:, k, :], wb[:, k, :], start=(k == 0), stop=(k == 7))
            fw.tt(out[:, c0:c0 + 256], ps[:, :256], out[:, c0:c0 + 256], ALU.add)
            i += 1


def phase_a1(fw, cfg, h_ctx, h_lat, w_my, g1, modc, modl, identb, p_tok, p_T, psb):
    nc = fw.nc
    wb = fw.sb("a1_w", [128, 8, NCOL], BF16)
    wv = w_my.rearrange("(k p) n -> p k n", p=128)
    for k in range(8):
        fw.dma(wb[:, k, :], wv[:, k, :], q="pool")
    g1b = fw.sb("a1_g1", [128, 1024])
    fw.dma(g1b[:], g1.partition_broadcast(128))
    gs = {}
    for nm, m in (("c", modc), ("l", modl)):
        t = fw.sb("a1_gs" + nm, [128, 1024])
        fw.stt(t[:], m[:, 1024:2048], 1.0, g1b[:], ALU.add, ALU.mult)
        gs[nm] = (t, m)
    xt = [fw.sb("a1_x%d" % i, [128, 1024]) for i in range(2)]
    junk = fw.sb("a1_junk", [128, 1024])
    tmp = fw.sb("a1_tmp", [128, 1024])
    ab = [fw.sb("a1_ab%d" % i, [128, 1024], BF16) for i in range(2)]
    ss = [fw.sb("a1_ss%d" % i, [128, 1]) for i in range(2)]
    rs = [fw.sb("a1_rs%d" % i, [128, 1]) for i in range(2)]
    aT = [fw.sb("a1_aT%d" % i, [128, 8, 512], BF16) for i in range(2)]
    otok = [fw.sb("a1_ot%d" % i, [128, NCOL]) for i in range(2)]
    ofm = [fw.sb("a1_of%d" % i, [128, 512]) for i in range(3)]
    pst = psb[0].bitcast(BF16)
    ntile = cfg.T // 128
    ti = 0
    gi = 0
    ev = 0
    for g0 in range(0, ntile, 4):
        gn = min(4, ntile - g0)
        aTg = aT[gi % 2]
        for tt_ in range(gn):
            t = g0 + tt_
            x = xt[ti % 2]
            if t * 128 < cfg.TC:
                src = h_ctx[t * 128:(t + 1) * 128, :]
                gst, m = gs["c"]
            else:
                r0 = t * 128 - cfg.TC
                src = h_lat[r0:r0 + 128, :]
                gst, m = gs["l"]
            fw.dma(x[:], src)
            s_, r_ = ss[ti % 2], rs[ti % 2]
            fw.act(junk[:], x[:], AF.Square, accum_out=s_[:])
            fw.act(r_[:], s_[:], AF.Sqrt, bias=NORM_EPS, scale=1.0 / D_MODEL)
            fw.recip(r_[:], r_[:])
            fw.stt(tmp[:], x[:], r_[:], gst[:], ALU.mult, ALU.mult)
            a_ = ab[ti % 2]
            fw.tt(a_[:], tmp[:], m[:, 0:1024], ALU.add, q="pool")
            for k in range(8):
                fw.tr(pst[:, k * 128:(k + 1) * 128], a_[:, k * 128:(k + 1) * 128], identb[:])
            fw.copy(aTg[:, :, tt_ * 128:(tt_ + 1) * 128], pst.rearrange("p (k t) -> p k t", k=8),
                    q=("act" if ti % 2 else "dve"))
            ti += 1
        ntok = gn * 128
        for tt_ in range(gn):
            t = g0 + tt_
            ot = otok[tt_ % 2]
            for cc in range(3):
                ps = psb[1 + (ev % 4)]
                for k in range(8):
                    fw.mm(ps[:, :512], aTg[:, k, tt_ * 128:(tt_ + 1) * 128], wb[:, k, cc * 512:(cc + 1) * 512],
                          start=(k == 0), stop=(k == 7))
                fw.copy(ot[:, cc * 512:(cc + 1) * 512], ps[:, :512], q=("act" if ev % 2 else "dve"))
                ev += 1
            fw.dma(p_tok[t * 128:(t + 1) * 128, :], ot[:], q="sp")
        for ct in range(12):
            ps = psb[1 + (ev % 4)]
            for k in range(8):
                fw.mm(ps[:, :ntok], wb[:, k, ct * 128:(ct + 1) * 128], aTg[:, k, :ntok], start=(k == 0), stop=(k == 7))
            of = ofm[ct % 3]
            fw.copy(of[:, :ntok], ps[:, :ntok], q=("act" if ev % 2 else "dve"))
            ev += 1
            fw.dma(p_T[ct, :, g0 * 128:g0 * 128 + ntok], of[:, :ntok], q="sp")
        gi += 1


def phase_da(fw, cfg, p_tok, rope, da_lambda_b, da_g_col, lam_init, with_ctx, identb, sel65, ones64, y_da, psb):
    TC, TL, T = cfg.TC, cfg.TL, cfg.T
    PQ = "dve" if DBG.get("nopool") else "pool"
    nkt = T // 128
    scale = 32 ** -0.5
    lb = fw.sb("da_lb", [64, 4, 32])
    fw.dma(lb[:], da_lambda_b.partition_broadcast(64).rearrange("p (a b) -> p a b", a=4))
    pr = fw.sb("da_pr", [64, 2, 32])
    fw.tt(pr[:, 0, :], lb[:, 0, :], lb[:, 1, :], ALU.mult)
    fw.tt(pr[:, 1, :], lb[:, 2, :], lb[:, 3, :], ALU.mult)
    s2 = fw.sb("da_s2", [64, 2])
    fw.reduce(s2[:], pr[:], ALU.add)
    fw.act(s2[:], s2[:], AF.Exp)
    neglam = fw.sb("da_nl", [64, 1])
    fw.tt(neglam[:], s2[:, 1:2], s2[:, 0:1], ALU.subtract)
    fw.ts(neglam[:], neglam[:], -float(lam_init), None, op0=ALU.add)
    gcol = fw.sb("da_g", [64, 1])
    fw.dma(gcol[:], da_g_col)
    fw.ts(gcol[:], gcol[:], 1.0 - float(lam_init), None, op0=ALU.mult)
    if DBG.get("da_stop") == 1:
        return
    qT = [fw.sb("da_qT%d" % h, [64, T], BF16) for h in range(2)]
    kT = [fw.sb("da_kT%d" % h, [64, T], BF16) for h in range(2)]
    va = fw.sb("da_va", [128, nkt, 2, 66], BF16)
    if not DBG.get("nova"):
        fw.memset(va[:], 1.0, q=PQ)
    qk = [fw.sb("da_qk%d" % i, [128, 256]) for i in range(2)]
    vt = [fw.sb("da_vt%d" % i, [128, 128]) for i in range(2)]
    cs = [fw.sb("da_cs%d" % i, [128, 32]) for i in range(2)]
    t1 = fw.sb("da_t1", [128, 8, 16])
    t2 = fw.sb("da_t2", [128, 8, 16])
    rb = [fw.sb("da_rb%d" % i, [128, 256], BF16) for i in range(2)]
    pst = psb[0].bitcast(BF16)
    c0q = T_DAQ * 128
    for t in range(nkt):
        x = qk[t % 2]
        v = vt[t % 2]
        fw.dma(x[:], p_tok[t * 128:(t + 1) * 128, c0q:c0q + 256])
        fw.dma(v[:], p_tok[t * 128:(t + 1) * 128, T_DAV * 128:(T_DAV + 1) * 128], q=("pool" if PQ == "pool" else "sp"))
        if not DBG.get("nova"):
            fw.copy(va[:, t, :, 0:64], v[:].rearrange("p (h d) -> p h d", h=2), q=PQ)
        r = rb[t % 2]
        if t * 128 >= TC and not DBG.get("norope"):
            c = cs[t % 2]
            fw.dma(c[:], rope[t * 128 - TC:(t + 1) * 128 - TC, :])
            xv = x[:].rearrange("p (g two d) -> p g two d", g=8, two=2)
            rv = r[:].rearrange("p (g two d) -> p g two d", g=8, two=2)
            cb = c[:, 0:16].unsqueeze(1).to_broadcast([128, 8, 16])
            sb_ = c[:, 16:32].unsqueeze(1).to_broadcast([128, 8, 16])
            fw.tt(t1[:], xv[:, :, 0, :], cb, ALU.mult)
            fw.tt(t2[:], xv[:, :, 1, :], sb_, ALU.mult)
            fw.tt(rv[:, :, 0, :], t1[:], t2[:], ALU.subtract)
            fw.tt(t1[:], xv[:, :, 0, :], sb_, ALU.mult, q=PQ)
            fw.tt(t2[:], xv[:, :, 1, :], cb, ALU.mult, q=PQ)
            fw.tt(rv[:, :, 1, :], t1[:], t2[:], ALU.add, q=PQ)
        else:
            fw.copy(r[:], x[:], q="act")
        if DBG.get("notr"):
            continue
        for i in range(4):
            fw.tr(pst[0:64, i * 128:(i + 1) * 128], r[:, i * 64:(i + 1) * 64], identb[:])
        if DBG.get("nocp"):
            continue
        for h in range(2):
            fw.copy(qT[h][:, t * 128:(t + 1) * 128], pst[0:64, h * 128:(h + 1) * 128], q="act")
            fw.copy(kT[h][:, t * 128:(t + 1) * 128], pst[0:64, (2 + h) * 128:(3 + h) * 128], q=("dve" if DBG.get("cpdve") else "act"))
    if DBG.get("da_stop") == 2:
        return
    pT = [fw.sb("da_pT%d" % i, [128, 512], BF16) for i in range(6)]
    sbanks = [psb[0], psb[3], psb[4], psb[5], psb[6], psb[7]]
    A = [fw.sb("da_A%d" % i, [65, 512]) for i in range(2)]
    rr = [fw.sb("da_rr%d" % i, [64, 512]) for i in range(2)]
    o = fw.sb("da_o", [64, 512])
    sq = fw.sb("da_sq", [64, 512])
    yo = [fw.sb("da_yo%d" % i, [64, 512]) for i in range(2)]
    chunks = []
    if with_ctx:
        chunks.append((0, TC, 0, TC // 128))
    for q0 in range(TC, T, 512):
        chunks.append((q0, min(512, T - q0), 0, nkt))
    it = 0
    yi = 0
    for (q0, nq, kt0, kt1) in chunks:
        for h in range(2):
            acc = [psb[1], psb[2]]
            its = [(kt, m) for kt in range(kt0, kt1) for m in range(2)]
            SK = 4
            base = it
            for idx in range(len(its) + SK):
                if idx < len(its):
                    kt, m = its[idx]
                    sp = sbanks[(base + idx) % 6]
                    fw.mm(sp[:, :nq], kT[h][m * 32:(m + 1) * 32, kt * 128:(kt + 1) * 128],
                          qT[h][m * 32:(m + 1) * 32, q0:q0 + nq])
                    pt = pT[(base + idx) % 6]
                    fw.act(pt[:, :nq], sp[:, :nq], AF.Exp, scale=scale)
                if idx >= SK:
                    kt, m = its[idx - SK]
                    pt = pT[(base + idx - SK) % 6]
                    fw.mm(acc[m][0:65, :nq], va[:, kt, h, 0:65], pt[:, :nq], start=(kt == kt0), stop=(kt == kt1 - 1))
            it += len(its)
            for m in range(2):
                fw.copy(A[m][:, :nq], acc[m][0:65, :nq], q=("dve" if m == 0 else "act"))
            if DBG.get("da_stop") == 3:
                continue
            for m in range(2):
                bp = psb[1 + m]
                fw.mm(bp[0:64, :nq], sel65[:], A[m][:, :nq])
                fw.recip(rr[m][:, :nq], bp[0:64, :nq])
            fw.tt(o[:, :nq], A[0][0:64, :nq], rr[0][:, :nq], ALU.mult)
            fw.tt(sq[:, :nq], A[1][0:64, :nq], rr[1][:, :nq], ALU.mult, q="pool")
            fw.stt(o[:, :nq], sq[:, :nq], neglam[:], o[:, :nq], ALU.mult, ALU.add)
            fw.tt(sq[:, :nq], o[:, :nq], o[:, :nq], ALU.mult, q="pool")
            bp = psb[1]
            fw.mm(bp[0:64, :nq], ones64[:], sq[:, :nq])
            fw.act(rr[0][:, :nq], bp[0:64, :nq], AF.Sqrt, bias=1e-5, scale=1.0 / 64)
            fw.recip(rr[0][:, :nq], rr[0][:, :nq])
            y = yo[yi % 2]
            yi += 1
            fw.stt(y[:, :nq], o[:, :nq], gcol[:], rr[0][:, :nq], ALU.mult, ALU.mult)
            fw.dma(y_da[h * 64:(h + 1) * 64, q0:q0 + nq], y[:, :nq])


def tile_groups(ntile, first):
    gs = []
    if first:
        gs.append((0, first, "A"))
    t = first
    while t < ntile:
        n = min(4, ntile - t)
        gs.append((t, n, "B"))
        t += n
    return gs


def norm_mod_tile(fw, x, tmp, ss, rs, gs_b, sh_b, out, out_q="pool"):
    fw.act(tmp[:], x[:], AF.Square, accum_out=ss[:])
    fw.act(rs[:], ss[:], AF.Sqrt, bias=NORM_EPS, scale=1.0 / D_MODEL)
    fw.recip(rs[:], rs[:])
    fw.stt(tmp[:], x[:], rs[:], gs_b, ALU.mult, ALU.mult)
    fw.tt(out[:], tmp[:], sh_b, ALU.add, q=out_q)


def phase_b1a(fw, ntile, nfirst, h_in, yT, cA, cB, wmod, bmod, g1, wgate, bgate_col, wbranch, wout,
              gluw, glub_col, identb, h_new, psb):
    N = ntile * 128
    wg = fw.sb("b1_wg", [128, 8, 4096], BF16)
    wgv = wgate.rearrange("(k p) n -> p k n", p=128)
    for k in range(8):
        for c in range(2):
            fw.dma(wg[:, k, c * 2048:(c + 1) * 2048], wgv[:, k, c * 2048:(c + 1) * 2048], q="pool")
    wbr = fw.sb("b1_wbr", [128, 4, 2, 1024], BF16)
    for i in range(4):
        fw.dma(wbr[:, i, :, :], wbranch[i].rearrange("(c p) n -> p c n", p=128), q="pool")
    wo = fw.sb("b1_wo", [128, 8, 1024], BF16)
    fw.dma(wo[:], wout.rearrange("(k p) n -> p k n", p=128), q="pool")
    glw = fw.sb("b1_glw", [128, 2, 256], BF16)
    fw.dma(glw[:], gluw.rearrange("(c p) n -> p c n", p=128), q="pool")
    bgc = fw.sb("b1_bgc", [128, 32])
    fw.dma(bgc[:], bgate_col)
    glb = fw.sb("b1_glb", [128, 2])
    fw.dma(glb[:], glub_col)
    mod = fw.sb("b1_mod", [128, 3072])
    x = [fw.sb("b1_x%d" % i, [128, 1024]) for i in range(2)]
    tmp = fw.sb("b1_tmp", [128, 1024])
    ab = [fw.sb("b1_ab%d" % i, [128, 1024], BF16) for i in range(2)]
    ss = [fw.sb("b1_ss%d" % i, [128, 1]) for i in range(2)]
    rs = [fw.sb("b1_rs%d" % i, [128, 1]) for i in range(2)]
    aT = fw.sb("b1_aT", [128, 8, 512], BF16)
    ybf = fw.sb("b1_ybf", [128, 4, 2, 512], BF16)
    y32 = fw.sb("b1_y32", [128, 2, 512])
    z32 = fw.sb("b1_z32", [128, 2, 512])
    u32 = fw.sb("b1_u32", [128, 512])
    zb = fw.sb("b1_zb", [128, 2, 512], BF16)
    gate = [fw.sb("b1_gate%d" % i, [128, 512], BF16) for i in range(2)]
    t2 = [fw.sb("b1_t2%d" % i, [128, 512]) for i in range(2)]
    acc = [fw.sb("b1_acc%d" % i, [128, 512]) for i in range(2)]
    accT = fw.sb("b1_accT", [128, 8, 512], BF16)
    hn = [fw.sb("b1_hn%d" % i, [128, 1024]) for i in range(2)]
    pst = psb[0].bitcast(BF16)
    cur = None
    ti = 0
    ev = 0
    for (t0, gn, which) in tile_groups(ntile, nfirst):
        if which != cur:
            phase_mod(fw, cA if which == "A" else cB, wmod, bmod, 0, 3072, mod, psb[7])
            fw.dma(tmp[:], g1.partition_broadcast(128))
            fw.stt(mod[:, 1024:2048], mod[:, 1024:2048], 1.0, tmp[:], ALU.add, ALU.mult)
            cur = which
        ntok = gn * 128
        tok0 = t0 * 128
        for tt_ in range(gn):
            xx = x[ti % 2]
            fw.dma(xx[:], h_in[tok0 + tt_ * 128: tok0 + (tt_ + 1) * 128, :])
            a_ = ab[ti % 2]
            norm_mod_tile(fw, xx, tmp, ss[ti % 2], rs[ti % 2], mod[:, 1024:2048], mod[:, 0:1024], a_)
            for k in range(8):
                fw.tr(pst[:, k * 128:(k + 1) * 128], a_[:, k * 128:(k + 1) * 128], identb[:])
            fw.copy(aT[:, :, tt_ * 128:(tt_ + 1) * 128], pst.rearrange("p (k t) -> p k t", k=8),
                    q=("act" if ti % 2 else "dve"))
            ti += 1
        for i in (0, 2, 3):
            for c in range(2):
                fw.dma(ybf[:, i, c, :ntok], yT[i, c * 128:(c + 1) * 128, tok0:tok0 + ntok], q="pool")
        for c in range(2):
            fw.dma(y32[:, c, :ntok], yT[1, c * 128:(c + 1) * 128, tok0:tok0 + ntok])
        yv, zv = y32[:, :, :ntok], z32[:, :, :ntok]
        fw.tt(zv, yv, yv, ALU.mult)
        fw.ts(zv, zv, 0.044715, 1.0, op0=ALU.mult, op1=ALU.add)
        fw.tt(zv, zv, yv, ALU.mult)
        fw.act(zv, zv, AF.Sigmoid, scale=1.5957691216057308)
        fw.tt(zv, zv, yv, ALU.mult)
        fw.copy(zb[:, :, :ntok], zv, q="pool")
        for oc in range(2):
            ps = psb[1 + (ev % 2)]
            ev += 1
            for c in range(2):
                fw.mm(ps[:, :ntok], glw[:, c, oc * 128:(oc + 1) * 128], zb[:, c, :ntok], start=(c == 0), stop=(c == 1))
            fw.act(u32[:, :ntok], ps[:, :ntok], AF.Sigmoid, bias=glb[:, oc:oc + 1])
            fw.tt(ybf[:, 1, oc, :ntok], u32[:, :ntok], z32[:, oc, :ntok], ALU.mult)
        gi = 0
        for ot in range(8):
            ac = acc[ot % 2]
            for i in range(4):
                psg = psb[1 + (gi % 2)]
                psy = psb[3 + (gi % 2)]
                for k in range(8):
                    fw.mm(psg[:, :ntok], wg[:, k, i * 1024 + ot * 128: i * 1024 + (ot + 1) * 128], aT[:, k, :ntok],
                          start=(k == 0), stop=(k == 7))
                for c in range(2):
                    fw.mm(psy[:, :ntok], wbr[:, i, c, ot * 128:(ot + 1) * 128], ybf[:, i, c, :ntok],
                          start=(c == 0), stop=(c == 1))
                g_ = gate[gi % 2]
                fw.act(g_[:, :ntok], psg[:, :ntok], AF.Sigmoid, bias=bgc[:, i * 8 + ot: i * 8 + ot + 1])
                if i == 0:
                    fw.tt(ac[:, :ntok], psy[:, :ntok], g_[:, :ntok], ALU.mult)
                else:
                    tq = t2[gi % 2]
                    fw.tt(tq[:, :ntok], psy[:, :ntok], g_[:, :ntok], ALU.mult)
                    if i < 3:
                        fw.tt(ac[:, :ntok], ac[:, :ntok], tq[:, :ntok], ALU.add, q="pool")
                    else:
                        fw.tt(accT[:, ot, :ntok], ac[:, :ntok], tq[:, :ntok], ALU.add, q="pool")
                gi += 1
        for tt_ in range(gn):
            xx = x[ti % 2]
            ti += 1
            fw.dma(xx[:], h_in[tok0 + tt_ * 128: tok0 + (tt_ + 1) * 128, :])
            hh = hn[tt_ % 2]
            for cc in range(2):
                ps = psb[5 + (ev % 2)]
                ev += 1
                for k in range(8):
                    fw.mm(ps[:, :512], accT[:, k, tt_ * 128:(tt_ + 1) * 128], wo[:, k, cc * 512:(cc + 1) * 512],
                          start=(k == 0), stop=(k == 7))
                fw.tt(hh[:, cc * 512:(cc + 1) * 512], ps[:, :512], mod[:, 2048 + cc * 512: 2048 + (cc + 1) * 512], ALU.mult)
            fw.tt(hh[:], hh[:], xx[:], ALU.add, q="pool")
            fw.dma(h_new[tok0 + tt_ * 128: tok0 + (tt_ + 1) * 128, :], hh[:])


def phase_b1b(fw, ntile, nfirst, h_new, cA, cB, wmod, bmod, g2, wrouter, brouter, identb, identf, fT, Wr, psb):
    mod = fw.sb("bb_mod", [128, 2048])
    wr = fw.sb("bb_wr", [128, 8, 16])
    fw.dma(wr[:], wrouter.rearrange("(k p) n -> p k n", p=128))
    brb = fw.sb("bb_brb", [128, 16])
    fw.dma(brb[:], brouter.partition_broadcast(128))
    x = [fw.sb("bb_x%d" % i, [128, 1024]) for i in range(2)]
    tmp = fw.sb("bb_tmp", [128, 1024])
    f32 = [fw.sb("bb_f%d" % i, [128, 1024]) for i in range(2)]
    fb = [fw.sb("bb_fb%d" % i, [128, 1024], BF16) for i in range(2)]
    ss = [fw.sb("bb_ss%d" % i, [128, 1]) for i in range(2)]
    rs = [fw.sb("bb_rs%d" % i, [128, 1]) for i in range(2)]
    fTg = [fw.sb("bb_fT%d" % i, [128, 8, 128], BF16) for i in range(2)]
    fT32 = [fw.sb("bb_fT32%d" % i, [128, 8, 128]) for i in range(2)]
    sc = [fw.sb("bb_sc%d" % i, [128, 16]) for i in range(2)]
    bi = fw.sb("bb_bi", [128, 4, 4])
    eq = fw.sb("bb_eq", [128, 4, 4])
    tb = fw.sb("bb_tb", [128, 4, 4])
    m1 = fw.sb("bb_m1", [128, 4])
    m2 = fw.sb("bb_m2", [128, 4])
    gsum = fw.sb("bb_gs", [128, 4])
    gmax = fw.sb("bb_gm", [128, 1])
    oh = fw.sb("bb_oh", [128, 4])
    den = fw.sb("bb_den", [128, 1])
    wout_t = [fw.sb("bb_w%d" % i, [128, 16]) for i in range(2)]
    pst = psb[0].bitcast(BF16)
    cur = None
    ti = 0
    for (t0, gn, which) in tile_groups(ntile, nfirst):
        if which != cur:
            phase_mod(fw, cA if which == "A" else cB, wmod, bmod, 3072, 5120, mod, psb[7])
            fw.dma(tmp[:], g2.partition_broadcast(128))
            fw.stt(mod[:, 1024:2048], mod[:, 1024:2048], 1.0, tmp[:], ALU.add, ALU.mult)
            cur = which
        for tt_ in range(gn):
            t = t0 + tt_
            xx = x[ti % 2]
            ff = f32[ti % 2]
            fbb = fb[ti % 2]
            fw.dma(xx[:], h_new[t * 128:(t + 1) * 128, :])
            norm_mod_tile(fw, xx, tmp, ss[ti % 2], rs[ti % 2], mod[:, 1024:2048], mod[:, 0:1024], ff)
            fw.copy(fbb[:], ff[:], q="act")
            for k in range(8):
                fw.tr(pst[:, k * 128:(k + 1) * 128], fbb[:, k * 128:(k + 1) * 128], identb[:])
            fg = fTg[ti % 2]
            fw.copy(fg[:], pst.rearrange("p (k t) -> p k t", k=8), q="dve")
            fw.dma(fT[:, :, t * 128:(t + 1) * 128].rearrange("k p t -> p k t"), fg[:])
            f3 = fT32[ti % 2]
            for half in range(2):
                pb = psb[1 + half]
                for kk in range(4):
                    k = half * 4 + kk
                    fw.tr(pb[:, kk * 128:(kk + 1) * 128], ff[:, k * 128:(k + 1) * 128], identf[:])
                fw.copy(f3[:, half * 4:(half + 1) * 4, :], pb[:].rearrange("p (k t) -> p k t", k=4),
                        q=("act" if half else "dve"))
            pl = psb[3]
            for k in range(8):
                fw.mm(pl[:, 0:16], f3[:, k, :], wr[:, k, :], start=(k == 0), stop=(k == 7))
            s_ = sc[ti % 2]
            fw.act(s_[:], pl[:, 0:16], AF.Sigmoid)
            biv = bi[:].rearrange("p a b -> p (a b)")
            fw.tt(biv, s_[:], brb[:], ALU.add)
            fw.reduce(m1[:], bi[:], ALU.max)
            fw.tt(eq[:], bi[:], m1[:].unsqueeze(2).to_broadcast([128, 4, 4]), ALU.is_equal)
            fw.stt(tb[:], eq[:], -1.0e9, bi[:], ALU.mult, ALU.add)
            fw.reduce(m2[:], tb[:], ALU.max)
            fw.tt(gsum[:], m1[:], m2[:], ALU.add)
            fw.reduce(gmax[:], gsum[:], ALU.max)
            fw.ts(oh[:], gsum[:], gmax[:], None, op0=ALU.is_equal)
            fw.tt(eq[:], bi[:], m2[:].unsqueeze(2).to_broadcast([128, 4, 4]), ALU.is_ge)
            fw.tt(eq[:], eq[:], oh[:].unsqueeze(2).to_broadcast([128, 4, 4]), ALU.mult)
            w_ = wout_t[ti % 2]
            fw.tt(w_[:], eq[:].rearrange("p a b -> p (a b)"), s_[:], ALU.mult)
            fw.reduce(den[:], w_[:], ALU.add)
            fw.recip(den[:], den[:])
            fw.ts(w_[:], w_[:], den[:], None, op0=ALU.mult)
            fw.dma(Wr[t * 128:(t + 1) * 128, :], w_[:])
            ti += 1


def phase_b2(fw, ntile, nfirst, sg_tiles, fT, Wr, h_new, cA, cB, wmod, bmod, weg, weu, wed, final_g, out, psb):
    gA = fw.sb("b2_gA", [128, 1024])
    gB = fw.sb("b2_gB", [128, 1024])
    phase_mod(fw, cA, wmod, bmod, 5120, 6144, gA, psb[7])
    phase_mod(fw, cB, wmod, bmod, 5120, 6144, gB, psb[7])
    fgb = None
    if final_g is not None:
        fgb = fw.sb("b2_fg", [128, 1024])
        fw.dma(fgb[:], final_g.partition_broadcast(128))
    SGT = sg_tiles
    fTs = fw.sb("b2_fT", [128, 8, SGT * 128], BF16)
    acc = fw.sb("b2_acc", [128, SGT, 1024])
    wrs = fw.sb("b2_wr", [128, SGT, 16])
    wgu = [fw.sb("b2_wgu%d" % i, [128, 8, 1024], BF16) for i in range(2)]
    wd = [fw.sb("b2_wd%d" % i, [128, 4, 1024], BF16) for i in range(2)]
    hid = [fw.sb("b2_hid%d" % i, [128, 4, 512], BF16) for i in range(2)]
    sgt = [fw.sb("b2_sg%d" % i, [128, 512]) for i in range(2)]
    hn = [fw.sb("b2_hn%d" % i, [128, 1024]) for i in range(2)]
    ot = [fw.sb("b2_ot%d" % i, [128, 1024]) for i in range(2)]
    ss = fw.sb("b2_ss", [128, 1])
    rs = fw.sb("b2_rs", [128, 1])
    wi = 0
    ev = 0
    for s0 in range(0, ntile, SGT):
        sn = min(SGT, ntile - s0)
        ntok = sn * 128
        for k in range(8):
            fw.dma(fTs[:, k, :ntok], fT[k, :, s0 * 128: s0 * 128 + ntok])
        fw.dma(wrs[:, :sn, :], Wr[s0 * 128: s0 * 128 + ntok, :].rearrange("(t p) e -> p t e", p=128))
        for e in range(16):
            wg_, wd_ = wgu[wi % 2], wd[wi % 2]
            wi += 1
            gv = weg[e].rearrange("(k p) n -> p k n", p=128)
            uv = weu[e].rearrange("(k p) n -> p k n", p=128)
            dv = wed[e].rearrange("(k p) n -> p k n", p=128)
            if not (DBG.get("b2_nodma") and wi > 2):
                for k in range(8):
                    fw.dma(wg_[:, k, 0:512], gv[:, k, :], q="pool")
                    fw.dma(wg_[:, k, 512:1024], uv[:, k, :], q="pool")
                for k in range(4):
                    fw.dma(wd_[:, k, :], dv[:, k, :], q="pool")
            for g0 in range(0, sn, 4):
                gn = min(4, sn - g0)
                gt = gn * 128
                c0 = g0 * 128
                hd = hid[ev % 2]
                for mt in range(4):
                    psg = psb[1 + (ev % 2)]
                    psu = psb[3 + (ev % 2)]
                    for k in range(8):
                        fw.mm(psg[:, :gt], wg_[:, k, mt * 128:(mt + 1) * 128], fTs[:, k, c0:c0 + gt],
                              start=(k == 0), stop=(k == 7))
                    for k in range(8):
                        fw.mm(psu[:, :gt], wg_[:, k, 512 + mt * 128: 512 + (mt + 1) * 128], fTs[:, k, c0:c0 + gt],
                              start=(k == 0), stop=(k == 7))
                    sg_ = sgt[ev % 2]
                    fw.act(sg_[:, :gt], psg[:, :gt], AF.Silu)
                    fw.tt(hd[:, mt, :gt], psu[:, :gt], sg_[:, :gt], ALU.mult)
                    ev += 1
                for tt_ in range(gn):
                    t = g0 + tt_
                    for cc in range(2):
                        ps = psb[5 + (ev % 2)]
                        ev += 1
                        for mt in range(4):
                            fw.mm(ps[:, :512], hd[:, mt, tt_ * 128:(tt_ + 1) * 128], wd_[:, mt, cc * 512:(cc + 1) * 512],
                                  start=(mt == 0), stop=(mt == 3))
                        a_ = acc[:, t, cc * 512:(cc + 1) * 512]
                        if e == 0:
                            fw.ts(a_, ps[:, :512], wrs[:, t, e:e + 1], None, op0=ALU.mult)
                        else:
                            fw.stt(a_, ps[:, :512], wrs[:, t, e:e + 1], a_, ALU.mult, ALU.add)
        for tt_ in range(sn):
            t = s0 + tt_
            h_ = hn[tt_ % 2]
            o_ = ot[tt_ % 2]
            fw.dma(h_[:], h_new[t * 128:(t + 1) * 128, :])
            gb = gA if t < nfirst else gB
            fw.tt(o_[:], acc[:, tt_, :], gb[:], ALU.mult, q="pool")
            fw.tt(o_[:], o_[:], h_[:], ALU.add, q="pool")
            if fgb is not None:
                fw.act(h_[:], o_[:], AF.Square, accum_out=ss[:])
                fw.act(rs[:], ss[:], AF.Sqrt, bias=NORM_EPS, scale=1.0 / D_MODEL)
                fw.recip(rs[:], rs[:])
                fw.stt(o_[:], o_[:], rs[:], fgb[:], ALU.mult, ALU.mult)
            fw.dma(out[t * 128:(t + 1) * 128, :], o_[:], is_output=True)


def blk_size(cfg):
    return 256 if cfg.TC % 256 == 0 else 128


def blk_src(cfg, d, i):
    B = blk_size(cfg)
    if d == 0:
        return i * B, (i + 1) * B, False
    nbc = cfg.TC // B
    nbl = cfg.TL // B
    if i < nbc:
        j = nbc - 1 - i
    else:
        j = nbc + (nbl - 1 - (i - nbc))
    return j * B, (j + 1) * B, True


def rows_ap(dram, t0, t1, c0, c1, rev, chunk=64):
    W = dram.shape[1]
    n = (t1 - t0) // chunk
    base = dram[t0:t1, c0:c1]
    if not rev:
        return base.rearrange("(c p) w -> p c w", p=chunk)
    off = int(dram.offset) + (t1 - 1) * W + c0
    return bass.AP(dram.tensor, off, [[-W, chunk], [-chunk * W, n], [1, c1 - c0]])


def frev(ap2d, rev):
    if not rev:
        return ap2d
    (ps, pn), (fs, fn) = ap2d.ap
    return bass.AP(ap2d.tensor, int(ap2d.offset) + (fn - 1) * fs, [[ps, pn], [-fs, fn]])


def tok_reverse(fw, dst, src, J, ps, nch, width, q="act"):
    for c in range(nch):
        fw.mm(ps[0:64, c * width:(c + 1) * width], J[:], src[:, nch - 1 - c, :])
    fw.copy(dst[:], ps[0:64, :nch * width].rearrange("p (c w) -> p c w", w=width), q=q)

def phase_gla(fw, cfg, p_tok, p_T, wg2, bg2neg_col, gla_g, maskU, cmask, identf, Jrev, o_scr, y_gla, psb):
    T, B = cfg.T, blk_size(cfg)
    NCH = B // 64
    nblk = T // B
    mU = fw.sb("gl_mU", [64, 64])
    fw.dma(mU[:], maskU)
    cm = fw.sb("gl_cm", [64, B])
    fw.dma(cm[:], cmask)
    w2 = fw.sb("gl_w2", [16, 2, 64])
    fw.dma(w2[:], wg2.rearrange("d r c -> r d c"))
    nb = fw.sb("gl_nb", [64, 2])
    for d in range(2):
        fw.dma(nb[:, d:d + 1], bg2neg_col[d])
    fw.ts(nb[:], nb[:], -1.0, None, op0=ALU.mult)
    S = [fw.sb("gl_S%d" % d, [64, 128]) for d in range(2)]
    for d in range(2):
        fw.memset(S[d][:], 0.0)
    two = range(2)
    qs = [fw.sb("gl_qs%d" % i, [64, B]) for i in two]
    ks = [fw.sb("gl_ks%d" % i, [64, B]) for i in two]
    ls = [fw.sb("gl_ls%d" % i, [16, B]) for i in two]
    vt = [fw.sb("gl_v%d" % i, [64, NCH, 128]) for i in two]
    vstg = fw.sb("gl_vstg", [64, NCH, 128])
    ostg = fw.sb("gl_ostg", [64, NCH, 128])
    lrev = fw.sb("gl_lrev", [16, B])
    la = fw.sb("gl_la", [64, B])
    bc = fw.sb("gl_bc", [64, B])
    eb = [fw.sb("gl_eb%d" % i, [64, B]) for i in two]
    en = fw.sb("gl_en", [64, B])
    qg = [fw.sb("gl_qg%d" % i, [64, B]) for i in two]
    kg = fw.sb("gl_kg", [64, B])
    kdT = fw.sb("gl_kdT", [64, B])
    kd = [fw.sb("gl_kd%d" % i, [64, NCH, 64]) for i in two]
    ai = [fw.sb("gl_ai%d" % i, [64, 2, NCH, 64]) for i in two]
    ob = [fw.sb("gl_ob%d" % i, [64, NCH, 128]) for i in two]
    it = 0
    for i in range(nblk):
        for d in DBG.get('gla_dirs', (0, 1)):
            t0, t1, rev = blk_src(cfg, d, i)
            STOP = DBG.get('gla_stop', 99)
            q_, k_, l_, v_ = qs[it % 2], ks[it % 2], ls[it % 2], vt[it % 2]
            fw.dma(q_[:], p_T[T_GQK, 0:64, t0:t1])
            fw.dma(k_[:], p_T[T_GQK, 64:128, t0:t1])
            fw.dma(l_[:], p_T[T_MISC, 32 * d:32 * d + 16, t0:t1])
            if rev:
                fw.dma(vstg[:], rows_ap(p_tok, t0, t1, T_GV * 128, (T_GV + 1) * 128, False))
                tok_reverse(fw, v_, vstg, Jrev, psb[0], NCH, 128)
                fw.copy(lrev[:], frev(l_[:], True))
                lv = lrev[:]
            else:
                fw.dma(v_[:], rows_ap(p_tok, t0, t1, T_GV * 128, (T_GV + 1) * 128, False))
                lv = l_[:]
            qv, kv = frev(q_[:], rev), frev(k_[:], rev)
            pz = psb[1]
            fw.mm(pz[0:64, :B], w2[:, d, :], lv)
            fw.act(la[:], pz[0:64, :B], AF.Exp, bias=nb[:, d:d + 1], scale=-1.0)
            fw.act(la[:], la[:], AF.Ln, bias=1.0)
            fw.ts(la[:], la[:], -1.0 / 16.0, None, op0=ALU.mult)
            if STOP <= 1:
                continue
            fw.scan(bc[:], cm[:], la[:], 0.0)
            e_ = eb[it % 2]
            fw.act(e_[:], bc[:], AF.Exp)
            fw.act(en[:], bc[:], AF.Exp, scale=-1.0)
            g_ = qg[it % 2]
            fw.stt(g_[:], qv, 32 ** -0.5, e_[:], ALU.mult, ALU.mult)
            fw.tt(kg[:], kv, en[:], ALU.mult, q="pool")
            bc3 = bc[:].rearrange("p (c t) -> p c t", t=64)
            fw.tt(kdT[:].rearrange("p (c t) -> p c t", t=64), bc3[:, :, 63:64].to_broadcast([64, NCH, 64]), bc3, ALU.subtract)
            fw.act(kdT[:], kdT[:], AF.Exp)
            fw.tt(kdT[:], kdT[:], kv, ALU.mult, q="pool")
            if STOP <= 2:
                continue
            a_ = ai[it % 2]
            for h in range(2):
                pa = psb[2 + h]
                for c in range(NCH):
                    fw.mm(pa[0:64, c * 64:(c + 1) * 64], kg[32 * h:32 * h + 32, c * 64:(c + 1) * 64],
                          g_[32 * h:32 * h + 32, c * 64:(c + 1) * 64])
                fw.tt(a_[:, h, :, :], pa[0:64, :NCH * 64].rearrange("p (a t) -> p a t", t=64),
                      mU[:].unsqueeze(1).to_broadcast([64, NCH, 64]), ALU.mult, q=("dve" if h == 0 else "pool") if False else "dve")
            if STOP <= 3:
                continue
            pk = psb[1]
            for c in range(NCH):
                fw.tr(pk[0:64, c * 64:(c + 1) * 64], kdT[:, c * 64:(c + 1) * 64], identf[0:64, 0:64])
            kd_ = kd[it % 2]
            fw.copy(kd_[:], pk[0:64, :NCH * 64].rearrange("p (c t) -> p c t", t=64), q="act")
            if STOP <= 4:
                continue
            o_ = ob[it % 2]
            Sd = S[d]
            for c in range(NCH):
                po = psb[4 + (c % 2)]
                fw.mm(po[0:64, 0:128], g_[:, c * 64:(c + 1) * 64], Sd[:], start=True, stop=False)
                for h in range(2):
                    fw.mm(po[0:64, h * 64:(h + 1) * 64], a_[:, h, c, :], v_[:, c, h * 64:(h + 1) * 64],
                          start=False, stop=(h == 1))
                fw.copy(o_[:, c, :], po[0:64, 0:128], q="act")
                pss = psb[6 + (c % 2)]
                fw.mm(pss[0:64, 0:128], kd_[:, c, :], v_[:, c, :])
                for h in range(2):
                    blk = Sd[32 * h:32 * h + 32, 64 * h:64 * h + 64]
                    fw.stt(blk, blk, e_[32 * h:32 * h + 32, c * 64 + 63:c * 64 + 64], pss[32 * h:32 * h + 32, 64 * h:64 * h + 64],
                           ALU.mult, ALU.add)
            if rev:
                tok_reverse(fw, ostg, o_, Jrev, psb[0], NCH, 128)
                fw.dma(rows_ap(o_scr[d], t0, t1, 0, 128, False), ostg[:])
            else:
                fw.dma(rows_ap(o_scr[d], t0, t1, 0, 128, False), o_[:])
            it += 1


def phase_gla_fin(fw, cfg, p_tok, o_scr, gla_g, identf, y_gla, psb, ctile=T_GR):
    T = cfg.T
    gb = fw.sb("gf_g", [128, 64])
    fw.dma(gb[:], gla_g.partition_broadcast(128))
    two = range(2)
    o0 = [fw.sb("gf_o0%d" % i, [128, 128]) for i in two]
    o1 = [fw.sb("gf_o1%d" % i, [128, 128]) for i in two]
    r_ = [fw.sb("gf_r%d" % i, [128, 128]) for i in two]
    sq = fw.sb("gf_sq", [128, 128])
    ss = fw.sb("gf_ss", [128, 2])
    yt = [fw.sb("gf_y%d" % i, [128, 128]) for i in two]
    yo = [fw.sb("gf_yo%d" % i, [128, 128]) for i in two]
    for t in range(T // 128):
        a, b, r, y = o0[t % 2], o1[t % 2], r_[t % 2], yt[t % 2]
        fw.dma(a[:], o_scr[0, t * 128:(t + 1) * 128, :])
        fw.dma(b[:], o_scr[1, t * 128:(t + 1) * 128, :])
        fw.dma(r[:], p_tok[t * 128:(t + 1) * 128, ctile * 128:(ctile + 1) * 128])
        fw.tt(a[:], a[:], b[:], ALU.add)
        fw.tt(sq[:], a[:], a[:], ALU.mult, q="pool")
        fw.reduce(ss[:], sq[:].rearrange("p (h d) -> p h d", h=2), ALU.add)
        fw.act(ss[:], ss[:], AF.Sqrt, bias=NORM_EPS, scale=1.0 / 64)
        fw.recip(ss[:], ss[:])
        fw.act(b[:], r[:], AF.Silu)
        for h in range(2):
            fw.stt(y[:, h * 64:(h + 1) * 64], a[:, h * 64:(h + 1) * 64], ss[:, h:h + 1], gb[:], ALU.mult, ALU.mult)
        fw.tt(y[:], y[:], b[:], ALU.mult, q="pool")
        pt = psb[1 + (t % 2)]
        fw.tr(pt[:, 0:128], y[:], identf[:])
        o = yo[t % 2]
        fw.copy(o[:], pt[:, 0:128], q="act")
        fw.dma(y_gla[:, t * 128:(t + 1) * 128], o[:])


def phase_dn_prep(fw, cfg, p_T, convw, alog_col, dtb_col, ones_bd, dn_qkv, dn_gates, psb):
    T, TC = cfg.T, cfg.TC
    cw = fw.sb("dp_cw", [128, 3, 5])
    fw.dma(cw[:], convw.rearrange("a p k -> p a k"))
    segs = []
    for (a, b) in ((0, TC), (TC, T)):
        s = a
        while s < b:
            e = min(s + 512, b)
            segs.append((s, e, a, b))
            s = e
    two = range(2)
    xp = [fw.sb("dp_xp%d" % i, [128, 516]) for i in two]
    acc = [fw.sb("dp_acc%d" % i, [128, 512]) for i in two]
    sq = fw.sb("dp_sq", [128, 512])
    rn = fw.sb("dp_rn", [128, 512])
    xo = [fw.sb("dp_xo%d" % i, [128, 512]) for i in two]
    it = 0
    for (s, e, a, b) in segs:
        n = e - s
        for ti, tile_id in enumerate((T_DNQ, T_DNK, T_DNV)):
            x = xp[it % 2]
            lo, hi = max(s - 2, a), min(e + 2, b)
            if lo > s - 2:
                fw.memset(x[:, 0:2], 0.0, q="pool")
            if hi < e + 2:
                fw.memset(x[:, n + 2:n + 4], 0.0, q="pool")
            fw.dma(x[:, lo - (s - 2): hi - (s - 2)], p_T[tile_id, :, lo:hi])
            ac = acc[it % 2]
            fw.ts(ac[:, :n], x[:, 0:n], cw[:, ti, 0:1], None, op0=ALU.mult)
            for k in range(1, 5):
                fw.stt(ac[:, :n], x[:, k:k + n], cw[:, ti, k:k + 1], ac[:, :n], ALU.mult, ALU.add)
            o = xo[it % 2]
            if ti < 2:
                fw.act(ac[:, :n], ac[:, :n], AF.Silu)
                fw.tt(sq[:, :n], ac[:, :n], ac[:, :n], ALU.mult, q="pool")
                ps = psb[1 + (it % 2)]
                fw.mm(ps[:, :n], ones_bd[:], sq[:, :n])
                fw.act(rn[:, :n], ps[:, :n], AF.Sqrt, bias=1e-6)
                fw.recip(rn[:, :n], rn[:, :n])
                fw.tt(o[:, :n], ac[:, :n], rn[:, :n], ALU.mult)
            else:
                fw.act(o[:, :n], ac[:, :n], AF.Silu)
            fw.dma(dn_qkv[ti, :, s:e], o[:, :n])
            it += 1
    ga = fw.sb("dp_ga", [4, T])
    gb = fw.sb("dp_gb", [4, T])
    al = fw.sb("dp_al", [4, 1])
    db = fw.sb("dp_db", [4, 1])
    fw.dma(al[:], alog_col)
    fw.dma(db[:], dtb_col)
    fw.act(al[:], al[:], AF.Exp)
    fw.ts(al[:], al[:], -1.0, None, op0=ALU.mult)
    fw.dma(ga[:], p_T[T_MISC, 64:68, :])
    fw.dma(gb[:], p_T[T_MISC, 68:72, :])
    fw.act(ga[:], ga[:], AF.Exp, bias=db[:])
    fw.act(ga[:], ga[:], AF.Ln, bias=1.0)
    fw.ts(ga[:], ga[:], al[:], None, op0=ALU.mult)
    fw.act(gb[:], gb[:], AF.Sigmoid)
    fw.dma(dn_gates[0], ga[:])
    fw.dma(dn_gates[1], gb[:])


def phase_dn(fw, cfg, dn_qkv, dn_gates, cmask2, Eh, maskLs, maskUs, maskUi, identf, Jrev, o_scr, psb):
    T, B = cfg.T, blk_size(cfg)
    NCH = B // 64
    NM = 2 * NCH
    nblk = T // B
    W = NM * 64
    sc = 64 ** -0.5
    cm = fw.sb("dn_cm", [2, B])
    fw.dma(cm[:], cmask2)
    eh = fw.sb("dn_eh", [2, 2, 64])
    fw.dma(eh[:], Eh.rearrange("h k m -> k h m"))
    mLs = fw.sb("dn_mLs", [64, 64]); fw.dma(mLs[:], maskLs)
    mUs = fw.sb("dn_mUs", [64, 64]); fw.dma(mUs[:], maskUs)
    mUi = fw.sb("dn_mUi", [64, 64]); fw.dma(mUi[:], maskUi)
    S = [fw.sb("dn_S%d" % d, [64, 2, 64]) for d in range(2)]
    for d in range(2):
        fw.memset(S[d][:], 0.0)
    mk = lambda n, shape: fw.sb(n, shape)
    ld = [[mk("dn_ld%d%d" % (i, j), [64, B]) for j in range(6)] for i in range(2)]
    qk2 = [[mk("dn_in%d%d" % (i, j), [64, B]) for j in range(6)] for i in range(2)]
    gl = [mk("dn_gl%d" % i, [2, 2, B]) for i in range(2)]
    g22 = [mk("dn_g2%d" % i, [2, B]) for i in range(2)]; ng22 = [mk("dn_ng2%d" % i, [2, B]) for i in range(2)]; be2 = [mk("dn_be%d" % i, [2, B]) for i in range(2)]
    eg2 = [mk("dn_eg%d" % i, [2, B]) for i in range(2)]; beg2 = [mk("dn_beg%d" % i, [2, B]) for i in range(2)]; ekd2 = [mk("dn_ekd%d" % i, [2, B]) for i in range(2)]
    ebc2 = [mk("dn_ebc%d" % i, [64, 2, B]) for i in range(2)]
    KbT2 = [mk("dn_KbT%d" % i, [64, 2, B]) for i in range(2)]; KbegT2 = [mk("dn_KbegT%d" % i, [64, 2, B]) for i in range(2)]; VbT2 = [mk("dn_VbT%d" % i, [64, 2, B]) for i in range(2)]
    KdT2 = [mk("dn_KdT%d" % i, [64, 2, B]) for i in range(2)]; QdT = [mk("dn_QdT%d" % i, [64, 2, B]) for i in range(2)]; QsT2 = [mk("dn_QsT%d" % i, [64, 2, B]) for i in range(2)]
    mG2 = [mk("dn_mG%d" % i, [64, W]) for i in range(2)]; mGT2 = [mk("dn_mGT%d" % i, [64, W]) for i in range(2)]
    t12 = [mk("dn_t1%d" % i, [64, W]) for i in range(2)]; t22 = [mk("dn_t2%d" % i, [64, W]) for i in range(2)]; t32 = [mk("dn_t3%d" % i, [64, W]) for i in range(2)]
    Bp2 = [[mk("dn_Bp%d%d" % (i, j), [64, W]) for j in range(2)] for i in range(2)]
    Np2 = [[mk("dn_Np%d%d" % (i, j), [64, W]) for j in range(2)] for i in range(2)]
    P2 = [mk("dn_P%d" % i, [64, W]) for i in range(2)]
    AiT = [mk("dn_AiT%d" % i, [64, W]) for i in range(2)]
    Rw2 = [mk("dn_Rw%d" % i, [64, W]) for i in range(2)]; Rv2 = [mk("dn_Rv%d" % i, [64, W]) for i in range(2)]
    Kd = [mk("dn_Kd%d" % i, [64, W]) for i in range(2)]
    U = [mk("dn_U%d" % i, [64, W]) for i in range(2)]
    WT = [mk("dn_WT%d" % i, [64, W]) for i in range(2)]
    vn2 = [[mk("dn_vn%d%d" % (i, j), [64, 2, 64]) for j in range(2)] for i in range(2)]
    ob = [mk("dn_ob%d" % i, [64, NCH, 128]) for i in range(2)]
    ostg2 = [mk("dn_ostg%d" % i, [64, NCH, 128]) for i in range(2)]
    v3 = lambda t: t[:].rearrange("p (m t) -> p m t", t=64)
    psb_rot = [psb[0]] + [psb[((k_ - 1 + 3) % 7) + 1] for k_ in range(1, 8)]

    def block(i, d):
        it = d
        PSB = psb if d == 0 else psb_rot
        if True:
            t0, t1_, rev = blk_src(cfg, d, i)
            par = it % 2
            qk, Bp, Np, vn = qk2[par], Bp2[par], Np2[par], vn2[par]
            g2 = g22[par]
            ng2 = ng22[par]
            be = be2[par]
            eg = eg2[par]
            beg = beg2[par]
            ekd = ekd2[par]
            ebc = ebc2[par]
            KbT = KbT2[par]
            KbegT = KbegT2[par]
            VbT = VbT2[par]
            KdT = KdT2[par]
            QsT = QsT2[par]
            mG = mG2[par]
            mGT = mGT2[par]
            t1 = t12[par]
            t2 = t22[par]
            t3 = t32[par]
            P = P2[par]
            Rw = Rw2[par]
            Rv = Rv2[par]
            ostg = ostg2[par]
            L = ld[it % 2]
            for a in range(3):
                for h in range(2):
                    fw.dma(L[a * 2 + h][:], dn_qkv[a, h * 64:(h + 1) * 64, t0:t1_], q=("sp" if h == 0 else "pool"))
            G = gl[it % 2]
            fw.dma(G[:], dn_gates[:, 2 * d:2 * d + 2, t0:t1_].rearrange("a h t -> h a t"))
            if rev:
                for j in range(6):
                    fw.copy(qk[j][:], frev(L[j][:], True), q=("pool" if j % 2 else "dve"))
                X = qk
            else:
                X = L
            q_, k_, v_ = (X[0], X[1]), (X[2], X[3]), (X[4], X[5])
            yield
            fw.scan(g2[:], cm[:], frev(G[:, 0, :], rev), 0.0)
            fw.ts(ng2[:], g2[:], -1.0, None, op0=ALU.mult)
            fw.copy(be[:], frev(G[:, 1, :], rev))
            fw.act(eg[:], g2[:], AF.Exp)
            fw.tt(beg[:], eg[:], be[:], ALU.mult)
            g3 = g2[:].rearrange("p (c t) -> p c t", t=64)
            fw.tt(ekd[:].rearrange("p (c t) -> p c t", t=64), g3[:, :, 63:64].to_broadcast([2, NCH, 64]), g3, ALU.subtract)
            fw.act(ekd[:], ekd[:], AF.Exp)
            yield
            for h in range(2):
                pb = PSB[1]
                fw.mm(pb[0:64, 0:B], eh[:, h, :], eg[:])
                fw.copy(ebc[:, h, :], pb[0:64, 0:B], q="act")
                pb2 = PSB[2]
                fw.mm(pb2[0:64, 0:B], eh[:, h, :], be[:])
                fw.tt(KbT[:, h, :], k_[h][:], pb2[0:64, 0:B], ALU.mult)
                fw.tt(VbT[:, h, :], v_[h][:], pb2[0:64, 0:B], ALU.mult)
                pb3 = PSB[3]
                fw.mm(pb3[0:64, 0:B], eh[:, h, :], beg[:])
                fw.tt(KbegT[:, h, :], k_[h][:], pb3[0:64, 0:B], ALU.mult)
                pb4 = PSB[4]
                fw.mm(pb4[0:64, 0:B], eh[:, h, :], ekd[:])
                fw.tt(KdT[:, h, :], k_[h][:], pb4[0:64, 0:B], ALU.mult)
                fw.stt(QdT[it % 2][:, h, :], q_[h][:], sc, ebc[:, h, :], ALU.mult, ALU.mult)
                fw.ts(QsT[:, h, :], q_[h][:], sc, None, op0=ALU.mult, q="pool")
            Qd = QdT[it % 2]
            yield
            pG, pGT, p1, p2, p3 = PSB[5], PSB[6], PSB[7], PSB[1], PSB[2]
            for h in range(2):
                for c in range(NCH):
                    m = h * NCH + c
                    cs = slice(c * 64, (c + 1) * 64)
                    ms = slice(m * 64, (m + 1) * 64)
                    fw.mm(pG[0:64, ms], g2[:, cs], eh[:, h, :], start=True, stop=False)
                    fw.mm(pG[0:64, ms], eh[:, h, :], ng2[:, cs], start=False, stop=True)
                    fw.mm(pGT[0:64, ms], eh[:, h, :], g2[:, cs], start=True, stop=False)
                    fw.mm(pGT[0:64, ms], ng2[:, cs], eh[:, h, :], start=False, stop=True)
                    fw.mm(p1[0:64, ms], k_[h][:, cs], KbT[:, h, cs])
                    fw.mm(p2[0:64, ms], KbT[:, h, cs], k_[h][:, cs])
                    fw.mm(p3[0:64, ms], k_[h][:, cs], QsT[:, h, cs])
            fw.ts(mG[:], pG[0:64, :W], 0.0, None, op0=ALU.min)
            fw.act(mG[:], mG[:], AF.Exp)
            fw.ts(mGT[:], pGT[0:64, :W], 0.0, None, op0=ALU.min)
            fw.act(mGT[:], mGT[:], AF.Exp)
            bcm = lambda mt: mt[:].unsqueeze(1).to_broadcast([64, NM, 64])
            fw.tt(t1[:], p1[0:64, :W], mGT[:], ALU.mult)
            Bc, Nc = Bp[0], Np[0]
            fw.stt(v3(Bc), v3(t1), -1.0, bcm(mUs), ALU.mult, ALU.mult)
            fw.tt(t2[:], p2[0:64, :W], mG[:], ALU.mult)
            fw.stt(v3(Nc), v3(t2), -1.0, bcm(mLs), ALU.mult, ALU.mult)
            fw.tt(t3[:], p3[0:64, :W], mGT[:], ALU.mult)
            Ai = AiT[it % 2]
            fw.tt(v3(Ai), v3(t3), bcm(mUi), ALU.mult, q="pool")
            yield
            fw.tt(v3(P), v3(Bc), identf[0:64, 0:64].unsqueeze(1).to_broadcast([64, NM, 64]), ALU.add)
            cur = 0
            for lvl in range(5):
                Bn, Nn = Bp[1 - cur], Np[1 - cur]
                pa, pb_, pc = PSB[3], PSB[4], PSB[5]
                for m in range(NM):
                    ms = slice(m * 64, (m + 1) * 64)
                    fw.mm(pa[0:64, ms], Np[cur][:, ms], Bp[cur][:, ms])
                    fw.mm(pb_[0:64, ms], Bp[cur][:, ms], Np[cur][:, ms])
                fw.copy(Bn[:], pa[0:64, :W], q="act")
                fw.copy(Nn[:], pb_[0:64, :W], q="dve")
                for m in range(NM):
                    ms = slice(m * 64, (m + 1) * 64)
                    fw.mm(pc[0:64, ms], Nn[:, ms], P[:, ms])
                fw.tt(P[:], P[:], pc[0:64, :W], ALU.add)
                cur = 1 - cur
                yield
            yield
            pr, pv, pk = PSB[6], PSB[7], PSB[1]
            for h in range(2):
                for c in range(NCH):
                    m = h * NCH + c
                    cs = slice(c * 64, (c + 1) * 64)
                    ms = slice(m * 64, (m + 1) * 64)
                    fw.tr(pr[0:64, ms], KbegT[:, h, cs], identf[0:64, 0:64])
                    fw.tr(pv[0:64, ms], VbT[:, h, cs], identf[0:64, 0:64])
                    fw.tr(pk[0:64, ms], KdT[:, h, cs], identf[0:64, 0:64])
            fw.copy(Rw[:], pr[0:64, :W], q="act")
            fw.copy(Rv[:], pv[0:64, :W], q="dve")
            Kd_ = Kd[it % 2]
            fw.copy(Kd_[:], pk[0:64, :W], q="act")
            yield
            pu, pw = PSB[2], PSB[3]
            for m in range(NM):
                ms = slice(m * 64, (m + 1) * 64)
                fw.mm(pu[0:64, ms], P[:, ms], Rv[:, ms])
                fw.mm(pw[0:64, ms], Rw[:, ms], P[:, ms])
            U_, WT_ = U[it % 2], WT[it % 2]
            fw.copy(U_[:], pu[0:64, :W], q="dve")
            fw.copy(WT_[:], pw[0:64, :W], q="act")
            yield
            Sd = S[d]
            o_ = ob[it % 2]
            for c in range(NCH):
                cs = slice(c * 64, (c + 1) * 64)
                ps1, ps2, ps3 = PSB[4], PSB[5 + (c % 2)], PSB[7]
                for h in range(2):
                    m = h * NCH + c
                    fw.mm(ps1[0:64, h * 64:(h + 1) * 64], WT_[:, m * 64:(m + 1) * 64], Sd[:, h, :])
                vn_ = vn[c % 2]
                for h in range(2):
                    m = h * NCH + c
                    fw.tt(vn_[:, h, :], U_[:, m * 64:(m + 1) * 64], ps1[0:64, h * 64:(h + 1) * 64], ALU.subtract)
                yield
                for h in range(2):
                    m = h * NCH + c
                    fw.mm(ps2[0:64, h * 64:(h + 1) * 64], Qd[:, h, cs], Sd[:, h, :], start=True, stop=False)
                    fw.mm(ps2[0:64, h * 64:(h + 1) * 64], Ai[:, m * 64:(m + 1) * 64], vn_[:, h, :], start=False, stop=True)
                fw.copy(o_[:, c, :], ps2[0:64, 0:128], q="act")
                yield
                for h in range(2):
                    m = h * NCH + c
                    fw.mm(ps3[0:64, h * 64:(h + 1) * 64], Kd_[:, m * 64:(m + 1) * 64], vn_[:, h, :])
                for h in range(2):
                    fw.stt(Sd[:, h, :], Sd[:, h, :], ebc[:, h, c * 64 + 63:c * 64 + 64], ps3[0:64, h * 64:(h + 1) * 64],
                           ALU.mult, ALU.add)
            yield
            if rev:
                tok_reverse(fw, ostg, o_, Jrev, PSB[0], NCH, 128)
                fw.dma(rows_ap(o_scr[d], t0, t1_, 0, 128, False), ostg[:])
            else:
                fw.dma(rows_ap(o_scr[d], t0, t1_, 0, 128, False), o_[:])

    for i in range(nblk):
        alive = [block(i, d) for d in DBG.get("dn_dirs", (0, 1))]
        while alive:
            for g_ in list(alive):
                try:
                    next(g_)
                except StopIteration:
                    alive.remove(g_)


def phase_s5(fw, cfg, p_T, lam_col, logdt_col, b_sm, c_nat, svals, mask4, mask4T, identf, ys5, psb):
    T, B = cfg.T, blk_size(cfg)
    nblk = T // B
    TWO_PI = 2.0 * np.pi
    sv = fw.sb("s5_sv", [128, B + 1])
    fw.dma(sv[:], svals)
    m4 = fw.sb("s5_m4", [128, 4, 128]); fw.dma(m4[:], mask4.rearrange("s p q -> p s q"))
    m4T = fw.sb("s5_m4T", [128, 4, 128]); fw.dma(m4T[:], mask4T.rearrange("s p q -> p s q"))
    bsm = fw.sb("s5_bsm", [128, 2, 4, 16])
    for r in range(2):
        fw.dma(bsm[:, r, :, :], b_sm[r])
    rho, cosT, sinT, BbT, CT = [], [], [], [], []
    for d in range(2):
        rho.append(fw.sb("s5_rho%d" % d, [128, 4]))
        cosT.append(fw.sb("s5_cos%d" % d, [128, 4, B + 1]))
        sinT.append(fw.sb("s5_sin%d" % d, [128, 4, B + 1]))
        BbT.append(fw.sb("s5_BbT%d" % d, [128, 2, 4, 128]))
        CT.append(fw.sb("s5_CT%d" % d, [128, 2, 4, 128]))
    with fw_scope(fw):
        lam = fw.sb("s5_lam", [128, 2, 4]); ldt = fw.sb("s5_ldt", [128, 4]); th = fw.sb("s5_th", [128, 4])
        ang = fw.sb("s5_ang", [128, B + 1]); wi = fw.sb("s5_wi", [128, B + 1], I32); wf = fw.sb("s5_wf", [128, B + 1])
        fx = fw.sb("s5_fx", [128, B + 1])
        col = lambda n: fw.sb(n, [128, 4])
        abr, abi, den, fre, fim, tmpc = col("s5_abr"), col("s5_abi"), col("s5_den"), col("s5_fre"), col("s5_fim"), col("s5_tmpc")
        bb = fw.sb("s5_bb", [128, 2, 4, 16]); tb = fw.sb("s5_tb", [128, 4, 16])
        M = fw.sb("s5_M", [128, 128]); cn = fw.sb("s5_cn", [128, 2, 64])
        for d in range(2):
            for r in range(2):
                fw.dma(lam[:, r, :], lam_col[d, r])
            fw.dma(ldt[:], logdt_col[d])
            fw.act(ldt[:], ldt[:], AF.Exp)
            fw.tt(th[:], lam[:, 1, :], ldt[:], ALU.mult)
            fw.tt(rho[d][:], lam[:, 0, :], ldt[:], ALU.mult)
            fw.act(rho[d][:], rho[d][:], AF.Exp)
            for st in range(4):
                for (tab, shift) in ((sinT[d], 0.0), (cosT[d], 0.25)):
                    fw.ts(ang[:], sv[:], th[:, st:st + 1], 1.0 / TWO_PI, op0=ALU.mult, op1=ALU.mult)
                    if shift:
                        fw.ts(ang[:], ang[:], shift, None, op0=ALU.add)
                    fw.copy(wi[:], ang[:])
                    fw.copy(wf[:], wi[:])
                    fw.tt(ang[:], ang[:], wf[:], ALU.subtract)
                    fw.ts(fx[:], ang[:], 0.5, None, op0=ALU.is_gt)
                    fw.tt(ang[:], ang[:], fx[:], ALU.subtract)
                    fw.ts(fx[:], ang[:], -0.5, None, op0=ALU.is_lt)
                    fw.tt(ang[:], ang[:], fx[:], ALU.add)
                    fw.act(tab[:, st, :], ang[:], AF.Sin, scale=TWO_PI)
            fw.tt(abr[:], rho[d][:], cosT[d][:, :, 1], ALU.mult)
            fw.tt(abi[:], rho[d][:], sinT[d][:, :, 1], ALU.mult)
            fw.ts(abr[:], abr[:], -1.0, None, op0=ALU.add)
            fw.tt(den[:], lam[:, 0, :], lam[:, 0, :], ALU.mult)
            fw.tt(tmpc[:], lam[:, 1, :], lam[:, 1, :], ALU.mult)
            fw.tt(den[:], den[:], tmpc[:], ALU.add)
            fw.recip(den[:], den[:])
            fw.tt(fre[:], abr[:], lam[:, 0, :], ALU.mult)
            fw.tt(tmpc[:], abi[:], lam[:, 1, :], ALU.mult)
            fw.tt(fre[:], fre[:], tmpc[:], ALU.add)
            fw.tt(fre[:], fre[:], den[:], ALU.mult)
            fw.tt(fim[:], abi[:], lam[:, 0, :], ALU.mult)
            fw.tt(tmpc[:], abr[:], lam[:, 1, :], ALU.mult)
            fw.tt(fim[:], fim[:], tmpc[:], ALU.subtract)
            fw.tt(fim[:], fim[:], den[:], ALU.mult)
            frb = fre[:].unsqueeze(2).to_broadcast([128, 4, 16])
            fib = fim[:].unsqueeze(2).to_broadcast([128, 4, 16])
            fw.tt(bb[:, 0, :, :], bsm[:, 0, :, :], frb, ALU.mult)
            fw.tt(tb[:], bsm[:, 1, :, :], fib, ALU.mult)
            fw.tt(bb[:, 0, :, :], bb[:, 0, :, :], tb[:], ALU.subtract)
            fw.tt(bb[:, 1, :, :], bsm[:, 1, :, :], frb, ALU.mult)
            fw.tt(tb[:], bsm[:, 0, :, :], fib, ALU.mult)
            fw.tt(bb[:, 1, :, :], bb[:, 1, :, :], tb[:], ALU.add)
            for r in range(2):
                fw.dma(cn[:, r, :], c_nat[d, r])
            for st in range(4):
                for r in range(2):
                    fw.tt(M[:].rearrange("p (g c) -> p g c", g=8), bb[:, r, st, :].unsqueeze(1).to_broadcast([128, 8, 16]),
                          m4T[:, st, :].rearrange("p (g c) -> p g c", g=8), ALU.mult)
                    pt = psb[1 + r]
                    fw.tr(pt[:, 0:128], M[:], identf[:])
                    fw.copy(BbT[d][:, r, st, :], pt[:, 0:128], q="act")
                    fw.tt(M[:].rearrange("p (l q) -> p l q", l=2), cn[:, r, :].unsqueeze(1).to_broadcast([128, 2, 64]),
                          m4[:, st, :].rearrange("p (l q) -> p l q", l=2), ALU.mult)
                    pt2 = psb[3 + r]
                    fw.tr(pt2[:, 0:128], M[:], identf[:])
                    if r == 0:
                        fw.copy(CT[d][:, r, st, :], pt2[:, 0:128], q="act")
                    else:
                        fw.act(CT[d][:, r, st, :], pt2[:, 0:128], AF.Copy, scale=-1.0)
    init = [fw.sb("s5_init%d" % d, [128, 2, 4]) for d in range(2)]
    for d in range(2):
        fw.memset(init[d][:], 0.0)
    two = range(2)
    ub = [fw.sb("s5_u%d" % i, [128, B]) for i in two]
    ur = fw.sb("s5_ur", [128, B])
    bur = [fw.sb("s5_bur%d" % i, [128, B]) for i in two]
    bui = [fw.sb("s5_bui%d" % i, [128, B]) for i in two]
    a1 = fw.sb("s5_a1", [128, B]); a2 = fw.sb("s5_a2", [128, B]); a3 = fw.sb("s5_a3", [128, B]); a4 = fw.sb("s5_a4", [128, B])
    rre = fw.sb("s5_rre", [128, B]); rim = fw.sb("s5_rim", [128, B])
    xtr = [fw.sb("s5_xtr%d" % i, [128, B]) for i in two]
    xti = [fw.sb("s5_xti%d" % i, [128, B]) for i in two]
    xre = [fw.sb("s5_xre%d" % i, [128, B]) for i in two]
    xim = [fw.sb("s5_xim%d" % i, [128, B]) for i in two]
    c1 = fw.sb("s5_c1", [128, 1]); c2 = fw.sb("s5_c2", [128, 1])
    yb = [fw.sb("s5_yb%d" % i, [128, B]) for i in two]
    yr = fw.sb("s5_yr", [128, B])
    it = 0
    k = 0
    for i in range(nblk):
        for d in DBG.get("s5_dirs", (0, 1)):
            t0, t1, rev = blk_src(cfg, d, i)
            u = ub[it % 2]
            fw.dma(u[:], p_T[T_S5U, :, t0:t1])
            if rev:
                fw.copy(ur[:], frev(u[:], True), q="pool")
                u = ur
            py = psb[7]
            for st in range(4):
                pr_, pi_ = psb[1 + (k % 2)], psb[3 + (k % 2)]
                fw.mm(pr_[:, :B], BbT[d][:, 0, st, :], u[:])
                fw.mm(pi_[:, :B], BbT[d][:, 1, st, :], u[:])
                br, bi_ = bur[k % 2], bui[k % 2]
                fw.copy(br[:], pr_[:, :B], q="act")
                fw.copy(bi_[:], pi_[:, :B], q="act")
                cs, sn = cosT[d][:, st, 0:B], sinT[d][:, st, 0:B]
                fw.tt(a1[:], br[:], cs, ALU.mult)
                fw.tt(a2[:], bi_[:], sn, ALU.mult, q="pool")
                fw.tt(a3[:], bi_[:], cs, ALU.mult, q="pool")
                fw.tt(a4[:], br[:], sn, ALU.mult)
                fw.tt(rre[:], a1[:], a2[:], ALU.add, q="pool")
                fw.tt(rim[:], a3[:], a4[:], ALU.subtract)
                xr_, xi_ = xtr[k % 2], xti[k % 2]
                rb = rho[d][:, st:st + 1].to_broadcast([128, B])
                fw.scan(xr_[:], rb, rre[:], init[d][:, 0, st:st + 1])
                fw.scan(xi_[:], rb, rim[:], init[d][:, 1, st:st + 1])
                er, ei = cosT[d][:, st, B:B + 1], sinT[d][:, st, B:B + 1]
                fw.tt(c1[:], xr_[:, B - 1:B], er, ALU.mult, q="pool")
                fw.tt(c2[:], xi_[:, B - 1:B], ei, ALU.mult, q="pool")
                fw.tt(init[d][:, 0, st:st + 1], c1[:], c2[:], ALU.subtract, q="pool")
                fw.tt(c1[:], xi_[:, B - 1:B], er, ALU.mult, q="pool")
                fw.tt(c2[:], xr_[:, B - 1:B], ei, ALU.mult, q="pool")
                fw.tt(init[d][:, 1, st:st + 1], c1[:], c2[:], ALU.add, q="pool")
                fw.tt(a1[:], xr_[:], cs, ALU.mult)
                fw.tt(a2[:], xi_[:], sn, ALU.mult, q="pool")
                fw.tt(a3[:], xi_[:], cs, ALU.mult, q="pool")
                fw.tt(a4[:], xr_[:], sn, ALU.mult)
                xr2, xi2 = xre[k % 2], xim[k % 2]
                fw.tt(xr2[:], a1[:], a2[:], ALU.subtract, q="pool")
                fw.tt(xi2[:], a3[:], a4[:], ALU.add)
                fw.mm(py[:, :B], CT[d][:, 0, st, :], xr2[:], start=(st == 0), stop=False)
                fw.mm(py[:, :B], CT[d][:, 1, st, :], xi2[:], start=False, stop=(st == 3))
                k += 1
            y = yb[it % 2]
            fw.copy(y[:], py[:, :B], q="act")
            if rev:
                fw.copy(yr[:], frev(y[:], True), q="pool")
                fw.dma(ys5[d, :, t0:t1], yr[:])
            else:
                fw.dma(ys5[d, :, t0:t1], y[:])
            it += 1


def phase_s5_fin(fw, cfg, p_T, ys5, dcol, y_s5):
    T = cfg.T
    dc = fw.sb("sf_d", [128, 1])
    fw.dma(dc[:], dcol)
    two = range(2)
    a = [fw.sb("sf_a%d" % i, [128, 512]) for i in two]
    b = [fw.sb("sf_b%d" % i, [128, 512]) for i in two]
    u = [fw.sb("sf_u%d" % i, [128, 512]) for i in two]
    i = 0
    for s in range(0, T, 512):
        e = min(s + 512, T)
        n = e - s
        aa, bb, uu = a[i % 2], b[i % 2], u[i % 2]
        fw.dma(aa[:, :n], ys5[0, :, s:e])
        fw.dma(bb[:, :n], ys5[1, :, s:e])
        fw.dma(uu[:, :n], p_T[T_S5U, :, s:e], q="pool")
        fw.tt(aa[:, :n], aa[:, :n], bb[:, :n], ALU.add)
        fw.stt(aa[:, :n], uu[:, :n], dc[:], aa[:, :n], ALU.mult, ALU.add)
        fw.dma(y_s5[:, s:e], aa[:, :n])
        i += 1


import ml_dtypes
from concourse.bass_utils import run_bass_kernel_spmd

DEPTH = 2
GRID_W = 64


def _col(v, n):
    return np.ascontiguousarray(np.asarray(v, np.float32).reshape(n, 128).T)


def _consts(cfg):
    B = blk_size(cfg)
    c = {}
    c["identb"] = np.eye(128).astype(ml_dtypes.bfloat16)
    c["identf"] = np.eye(128, dtype=np.float32)
    t = np.arange(cfg.TL)
    row = (t // GRID_W).astype(np.float32)
    colp = (t % GRID_W).astype(np.float32)
    inv = (10000.0 ** (-np.arange(8, dtype=np.float32) / 8)).astype(np.float32)
    ang = np.concatenate([row[:, None] * inv, colp[:, None] * inv], -1)
    c["rope"] = np.concatenate([np.cos(ang), np.sin(ang)], -1).astype(np.float32)
    sel = np.zeros((65, 64), np.float32)
    sel[64] = 1
    c["sel65"] = sel
    c["ones64"] = np.ones((64, 64), np.float32)
    c["maskU"] = np.triu(np.ones((64, 64), np.float32))
    cm = np.ones((64, B), np.float32)
    cm[:, ::64] = 0
    c["cmask"] = cm
    c["cmask2"] = np.ascontiguousarray(cm[:2])
    c["Jr"] = np.ascontiguousarray(np.eye(64, dtype=np.float32)[::-1])
    obd = np.zeros((128, 128), np.float32)
    obd[:64, :64] = 1
    obd[64:, 64:] = 1
    c["onesbd"] = obd
    Eh = np.zeros((2, 2, 64), np.float32)
    Eh[0, 0] = 1
    Eh[1, 1] = 1
    c["Eh"] = Eh
    ii, jj = np.meshgrid(np.arange(64), np.arange(64), indexing="ij")
    c["mLs"] = (ii > jj).astype(np.float32)
    c["mUs"] = (jj > ii).astype(np.float32)
    c["mUi"] = (jj >= ii).astype(np.float32)
    c["svals"] = np.tile(np.arange(B + 1, dtype=np.float32), (128, 1))
    m4 = np.zeros((4, 128, 128), np.float32)
    for st in range(4):
        for gl2 in range(2):
            g8 = 2 * st + gl2
            m4[st, g8 * 16:(g8 + 1) * 16, gl2 * 64:(gl2 + 1) * 64] = 1
    c["mask4"] = m4
    c["mask4T"] = np.ascontiguousarray(m4.transpose(0, 2, 1))
    return c


def _s5_layout(a_re, a_im, log_dt, b_re, b_im, c_re, c_im, j):
    gs = slice(8 * j, 8 * j + 8)

    def colz(x):
        return np.ascontiguousarray(x.reshape(4, 2, 64).transpose(1, 2, 0).reshape(128, 4))
    lam_col = np.stack([np.stack([colz(a_re[d, gs]), colz(a_im[d, gs])]) for d in range(2)])
    logdt_col = np.stack([colz(np.repeat(log_dt[d, gs][:, None], 64, 1)) for d in range(2)])

    def bsm(x):
        return np.ascontiguousarray(x.reshape(4, 2, 64, 16).transpose(1, 2, 0, 3).reshape(128, 4, 16))
    b_sm = np.stack([bsm(b_re[gs]), bsm(b_im[gs])])
    c_nat = np.stack([np.stack([c_re[d, gs].reshape(128, 64), c_im[d, gs].reshape(128, 64)]) for d in range(2)])
    f = lambda z: np.ascontiguousarray(z, dtype=np.float32)
    return f(lam_col), f(logdt_col), f(b_sm), f(c_nat)


_A_IN = [("h_ctx", None), ("h_lat", None), ("c_lat", [128, 8]), ("c_ctx", [128, 8]), ("wmod", [1024, 6144]), ("bmod", [6144]),
         ("g1", [1024]), ("w_my", [1024, NCOL]), ("lamb", [128]), ("da_g", [64, 1]), ("wg2", [2, 16, 64]), ("bg2", [2, 64, 1]),
         ("gla_g", [64]), ("convw", [3, 128, 5]), ("alog", [4, 1]), ("dtb", [4, 1]), ("dn_g", [64]),
         ("lam_col", [2, 2, 128, 4]), ("logdt_col", [2, 128, 4]), ("b_sm", [2, 128, 4, 16]), ("c_nat", [2, 2, 128, 64]),
         ("dcol", [128, 1])]


def build_A(cfg, lam_init, with_ctx):
    nc = bass.Bass("TRN2", target_bir_lowering=False)
    T, B = cfg.T, blk_size(cfg)
    dt = lambda n, s, d=F32, k="ExternalInput": nc.dram_tensor(n, list(s), d, kind=k).ap()
    a = {}
    for n, s in _A_IN:
        if n == "h_ctx":
            s = [cfg.TC, 1024]
        if n == "h_lat":
            s = [cfg.TL, 1024]
        a[n] = dt(n, s)
    cshape = dict(identb=([128, 128], BF16), identf=([128, 128], F32), rope=([cfg.TL, 32], F32), sel65=([65, 64], F32),
                  ones64=([64, 64], F32), maskU=([64, 64], F32), cmask=([64, B], F32), cmask2=([2, B], F32), Jr=([64, 64], F32),
                  onesbd=([128, 128], F32), Eh=([2, 2, 64], F32), mLs=([64, 64], F32), mUs=([64, 64], F32), mUi=([64, 64], F32),
                  svals=([128, B + 1], F32), mask4=([4, 128, 128], F32), mask4T=([4, 128, 128], F32))
    for n, (s, d) in cshape.items():
        a[n] = dt(n, s, d)
    yT = dt("yT", [4, 128, T], F32, "ExternalOutput")
    p_tok = dt("p_tok", [T, NCOL], F32, "Internal")
    p_T = dt("p_T", [12, 128, T], F32, "Internal")
    dn_qkv = dt("dn_qkv", [3, 128, T], F32, "Internal")
    dn_gates = dt("dn_gates", [2, 4, T], F32, "Internal")
    o_gla = dt("o_gla", [2, T, 128], F32, "Internal")
    o_dn = dt("o_dn", [2, T, 128], F32, "Internal")
    ys5 = dt("ys5", [2, 128, T], F32, "Internal")
    with contextlib.ExitStack() as st:
        fw = FW(nc, st)
        psb = [fw.ps("bank%d" % i, [128, 512]) for i in range(8)]
        idb = fw.sb("idb", [128, 128], BF16)
        fw.dma(idb[:], a["identb"])
        idf = fw.sb("idf", [128, 128])
        fw.dma(idf[:], a["identf"])
        with fw_scope(fw):
            modc = fw.sb("modc", [128, 2048])
            modl = fw.sb("modl", [128, 2048])
            phase_mod(fw, a["c_ctx"], a["wmod"], a["bmod"], 0, 2048, modc, psb[7])
            phase_mod(fw, a["c_lat"], a["wmod"], a["bmod"], 0, 2048, modl, psb[7])
            phase_a1(fw, cfg, a["h_ctx"], a["h_lat"], a["w_my"], a["g1"], modc, modl, idb, p_tok, p_T, psb)
        with fw_scope(fw):
            s65 = fw.sb("s65", [65, 64])
            fw.dma(s65[:], a["sel65"])
            o64 = fw.sb("o64", [64, 64])
            fw.dma(o64[:], a["ones64"])
            phase_da(fw, cfg, p_tok, a["rope"], a["lamb"], a["da_g"], lam_init, with_ctx, idb, s65, o64, yT[2], psb)
        with fw_scope(fw):
            Jsb = fw.sb("Jsb", [64, 64])
            fw.dma(Jsb[:], a["Jr"])
            phase_gla(fw, cfg, p_tok, p_T, a["wg2"], a["bg2"], a["gla_g"], a["maskU"], a["cmask"], idf, Jsb, o_gla, yT[3], psb)
        with fw_scope(fw):
            phase_gla_fin(fw, cfg, p_tok, o_gla, a["gla_g"], idf, yT[3], psb, ctile=T_GR)
        with fw_scope(fw):
            obd = fw.sb("obd", [128, 128])
            fw.dma(obd[:], a["onesbd"])
            phase_dn_prep(fw, cfg, p_T, a["convw"], a["alog"], a["dtb"], obd, dn_qkv, dn_gates, psb)
        with fw_scope(fw):
            Jsb = fw.sb("Jsb2", [64, 64])
            fw.dma(Jsb[:], a["Jr"])
            phase_dn(fw, cfg, dn_qkv, dn_gates, a["cmask2"], a["Eh"], a["mLs"], a["mUs"], a["mUi"], idf, Jsb, o_dn, psb)
        with fw_scope(fw):
            phase_gla_fin(fw, cfg, p_tok, o_dn, a["dn_g"], idf, yT[0], psb, ctile=T_DNZ)
        with fw_scope(fw):
            phase_s5(fw, cfg, p_T, a["lam_col"], a["logdt_col"], a["b_sm"], a["c_nat"], a["svals"], a["mask4"], a["mask4T"],
                     idf, ys5, psb)
        with fw_scope(fw):
            phase_s5_fin(fw, cfg, p_T, ys5, a["dcol"], yT[1])
        fw.finish()
    return nc


def build_B(ntile, nfirst, final):
    nc = bass.Bass("TRN2", target_bir_lowering=False)
    N = ntile * 128
    dt = lambda n, s, d=F32, k="ExternalInput": nc.dram_tensor(n, list(s), d, kind=k).ap()
    a = {}
    for n, s in (("h_in", [N, 1024]), ("yT", [4, 256, N]), ("cA", [128, 8]), ("cB", [128, 8]), ("wmod", [1024, 6144]),
                 ("bmod", [6144]), ("g1", [1024]), ("g2", [1024]), ("wgate", [1024, 4096]), ("bgc", [128, 32]),
                 ("wbr", [4, 256, 1024]), ("wout", [1024, 1024]), ("gluw", [256, 256]), ("glub", [128, 2]),
                 ("wrouter", [1024, 16]), ("brouter", [16]), ("weg", [16, 1024, 512]), ("weu", [16, 1024, 512]),
                 ("wed", [16, 512, 1024]), ("fg", [1024]), ("identf", [128, 128])):
        a[n] = dt(n, s)
    a["identb"] = dt("identb", [128, 128], BF16)
    out = dt("out", [N, 1024], F32, "ExternalOutput")
    h_new = dt("h_new", [N, 1024], F32, "Internal")
    fT = dt("fT", [8, 128, N], BF16, "Internal")
    Wr = dt("Wr", [N, 16], F32, "Internal")
    with contextlib.ExitStack() as st:
        fw = FW(nc, st)
        psb = [fw.ps("bank%d" % i, [128, 512]) for i in range(8)]
        idb = fw.sb("idb", [128, 128], BF16)
        fw.dma(idb[:], a["identb"])
        idf = fw.sb("idf", [128, 128])
        fw.dma(idf[:], a["identf"])
        with fw_scope(fw):
            phase_b1a(fw, ntile, nfirst, a["h_in"], a["yT"], a["cA"], a["cB"], a["wmod"], a["bmod"], a["g1"], a["wgate"],
                      a["bgc"], a["wbr"], a["wout"], a["gluw"], a["glub"], idb, h_new, psb)
        with fw_scope(fw):
            phase_b1b(fw, ntile, nfirst, h_new, a["cA"], a["cB"], a["wmod"], a["bmod"], a["g2"], a["wrouter"], a["brouter"],
                      idb, idf, fT, Wr, psb)
        with fw_scope(fw):
            phase_b2(fw, ntile, nfirst, (ntile + 1) // 2, fT, Wr, h_new, a["cA"], a["cB"], a["wmod"], a["bmod"],
                     a["weg"], a["weu"], a["wed"], a["fg"] if final else None, out, psb)
        fw.finish()
    return nc


def kernel(x, c, ctx, c_ctx, w_mod, b_mod, norm1_g, norm2_g, w_in, dn_conv, dn_a_log, dn_dt_bias, dn_norm_g,
           s5_a_re, s5_a_im, s5_log_dt, s5_b_re, s5_b_im, s5_c_re, s5_c_im, s5_d, s5_glu_w, s5_glu_b,
           da_lambda, da_norm_g, gla_w_gate, gla_b_gate, gla_norm_g, w_branch, w_gate, b_gate, w_out,
           w_router, b_router, w_e_gate, w_e_up, w_e_down, final_g):
    f32 = lambda v: np.ascontiguousarray(np.asarray(v), dtype=np.float32)
    x, c, ctx, c_ctx = f32(x), f32(c), f32(ctx), f32(c_ctx)
    nb, TL, _ = x.shape
    TC = ctx.shape[1]
    assert nb == 4
    cfg = Cfg(TC, TL)
    T = cfg.T
    consts = _consts(cfg)
    cols = [my_cols(j) for j in range(2)]
    h_lat, h_ctx = x.copy(), ctx.copy()
    ncl, nll = TC // 128, TL // 128
    for L in range(DEPTH):
        with_ctx = L < DEPTH - 1
        lam_init = 0.8 - 0.6 * float(np.exp(-0.3 * L))
        ncA = build_A(cfg, lam_init, with_ctx)
        wl = f32(w_in[L])
        in_maps = []
        for core in range(8):
            b, j = core // 2, core % 2
            cj = cols[j]
            w_my = np.where(cj[None, :] >= 0, wl[:, np.maximum(cj, 0)], np.float32(0)).astype(np.float32)
            lam_col, logdt_col, b_sm, c_nat = _s5_layout(f32(s5_a_re[L]), f32(s5_a_im[L]), f32(s5_log_dt[L]), f32(s5_b_re[L]),
                                                         f32(s5_b_im[L]), f32(s5_c_re[L]), f32(s5_c_im[L]), j)
            dnc = f32(dn_conv[L])
            convw = np.stack([np.ascontiguousarray(dnc[:, a * 256 + 128 * j: a * 256 + 128 * j + 128].T) for a in range(3)])
            m = dict(h_ctx=h_ctx[b], h_lat=h_lat[b], c_lat=_col(c[b], 8), c_ctx=_col(c_ctx, 8), wmod=f32(w_mod[L]),
                     bmod=f32(b_mod[L]), g1=f32(norm1_g[L]), w_my=w_my, lamb=f32(da_lambda[L]).reshape(128),
                     da_g=f32(da_norm_g[L]).reshape(64, 1), wg2=f32(gla_w_gate[L][:, :, 64 * j:64 * j + 64]),
                     bg2=f32(gla_b_gate[L][:, 64 * j:64 * j + 64]).reshape(2, 64, 1), gla_g=f32(gla_norm_g[L]),
                     convw=f32(convw), alog=f32(dn_a_log[L][:, 2 * j:2 * j + 2]).reshape(4, 1),
                     dtb=f32(dn_dt_bias[L][:, 2 * j:2 * j + 2]).reshape(4, 1), dn_g=f32(dn_norm_g[L]),
                     lam_col=lam_col, logdt_col=logdt_col, b_sm=b_sm, c_nat=c_nat,
                     dcol=f32(s5_d[L][128 * j:128 * j + 128]).reshape(128, 1))
            m.update(consts)
            in_maps.append(m)
        resA = run_bass_kernel_spmd(ncA, in_maps, core_ids=list(range(8))).results
        if with_ctx:
            nt = (ncl + nll) // 2
            nfirst = ncl
        else:
            nt = nll // 2
            nfirst = 0
        final = (L == DEPTH - 1)
        ncB = build_B(nt, nfirst, final)
        in_maps = []
        for core in range(8):
            b, j = core // 2, core % 2
            yfull = np.concatenate([np.asarray(resA[2 * b]["yT"]), np.asarray(resA[2 * b + 1]["yT"])], axis=1)
            if with_ctx:
                hj = np.concatenate([h_ctx[b], h_lat[b]], axis=0)
                t0 = j * nt * 128
            else:
                hj = h_lat[b]
                t0 = j * nt * 128
                yfull = yfull[:, :, TC:]
            N = nt * 128
            cA = _col(c_ctx, 8) if (with_ctx and j == 0) else _col(c[b], 8)
            m = dict(h_in=np.ascontiguousarray(hj[t0:t0 + N]), yT=np.ascontiguousarray(yfull[:, :, t0:t0 + N]),
                     cA=cA, cB=_col(c[b], 8), wmod=f32(w_mod[L]), bmod=f32(b_mod[L]), g1=f32(norm1_g[L]), g2=f32(norm2_g[L]),
                     wgate=f32(w_gate[L]), bgc=_col(b_gate[L], 32), wbr=f32(w_branch[L]), wout=f32(w_out[L]),
                     gluw=f32(s5_glu_w[L]), glub=_col(s5_glu_b[L], 2), wrouter=f32(w_router), brouter=f32(b_router),
                     weg=f32(w_e_gate[L]), weu=f32(w_e_up[L]), wed=f32(w_e_down[L]), fg=f32(final_g),
                     identf=consts["identf"], identb=consts["identb"])
            in_maps.append(m)
        resB = run_bass_kernel_spmd(ncB, in_maps, core_ids=list(range(8))).results
        for core in range(8):
            b, j = core // 2, core % 2
            o = np.asarray(resB[core]["out"])
            N = nt * 128
            t0 = j * N
            if with_ctx:
                full = np.concatenate([h_ctx[b], h_lat[b]], axis=0)
                full[t0:t0 + N] = o
                h_ctx[b] = full[:TC]
                h_lat[b] = full[TC:]
            else:
                h_lat[b, t0:t0 + N] = o
    return h_lat.astype(np.float32)


_AJ_IN = [("w_my", [1024, NCOL]), ("wg2", [2, 16, 64]), ("bg2", [2, 64, 1]), ("convw", [3, 128, 5]), ("alog", [4, 1]),
          ("dtb", [4, 1]), ("lam_col", [2, 2, 128, 4]), ("logdt_col", [2, 128, 4]), ("b_sm", [2, 128, 4, 16]),
          ("c_nat", [2, 2, 128, 64]), ("dcol", [128, 1])]
_AL_IN = [("wmod", [1024, 6144]), ("bmod", [6144]), ("g1", [1024]), ("g2", [1024]), ("lamb", [128]), ("da_g", [64, 1]),
          ("gla_g", [64]), ("dn_g", [64]), ("wgate", [1024, 4096]), ("bgc", [128, 32]), ("wbr", [4, 256, 1024]),
          ("wout", [1024, 1024]), ("gluw", [256, 256]), ("glub", [128, 2]), ("weg", [16, 1024, 512]),
          ("weu", [16, 1024, 512]), ("wed", [16, 512, 1024])]


def build_fused(cfg):
    nc = bass.Bass("TRN2", target_bir_lowering=False)
    T, TC, TL, B = cfg.T, cfg.TC, cfg.TL, blk_size(cfg)
    dt = lambda n, s, d=F32, k="ExternalInput": nc.dram_tensor(n, list(s), d, kind=k).ap()
    a = {}
    a["h0"] = dt("h0", [T, 1024])
    for n, s in (("c_lat", [128, 8]), ("c_ctx", [128, 8]), ("wrouter", [1024, 16]), ("brouter", [16]), ("fg", [1024])):
        a[n] = dt(n, s)
    for L in range(DEPTH):
        for n, s in _AL_IN:
            a["%s_%d" % (n, L)] = dt("%s_%d" % (n, L), s)
        for j in range(2):
            for n, s in _AJ_IN:
                a["%s_%d%d" % (n, L, j)] = dt("%s_%d%d" % (n, L, j), s)
    cshape = dict(identb=([128, 128], BF16), identf=([128, 128], F32), rope=([cfg.TL, 32], F32), sel65=([65, 64], F32),
                  ones64=([64, 64], F32), maskU=([64, 64], F32), cmask=([64, B], F32), cmask2=([2, B], F32), Jr=([64, 64], F32),
                  onesbd=([128, 128], F32), Eh=([2, 2, 64], F32), mLs=([64, 64], F32), mUs=([64, 64], F32), mUi=([64, 64], F32),
                  svals=([128, B + 1], F32), mask4=([4, 128, 128], F32), mask4T=([4, 128, 128], F32))
    for n, (s, d) in cshape.items():
        a[n] = dt(n, s, d)
    out = dt("out", [TL, 1024], F32, "ExternalOutput")
    I = "Internal"
    h1 = dt("h1", [T, 1024], F32, I)
    yT = dt("yT", [4, 256, T], F32, I)
    p_tok = dt("p_tok", [T, NCOL], F32, I)
    p_T = dt("p_T", [12, 128, T], F32, I)
    dn_qkv = dt("dn_qkv", [3, 128, T], F32, I)
    dn_gates = dt("dn_gates", [2, 4, T], F32, I)
    o_gla = dt("o_gla", [2, T, 128], F32, I)
    o_dn = dt("o_dn", [2, T, 128], F32, I)
    ys5 = dt("ys5", [2, 128, T], F32, I)
    h_new = dt("h_new", [T, 1024], F32, I)
    fT = dt("fT", [8, 128, T], BF16, I)
    Wr = dt("Wr", [T, 16], F32, I)
    with contextlib.ExitStack() as st:
        fw = FW(nc, st)
        psb = [fw.ps("bank%d" % i, [128, 512]) for i in range(8)]
        idb = fw.sb("idb", [128, 128], BF16)
        fw.dma(idb[:], a["identb"])
        idf = fw.sb("idf", [128, 128])
        fw.dma(idf[:], a["identf"])
        import os as _os
        for L in range(1 if _os.environ.get("FUSED_PROBE") else DEPTH):
            lam_init = 0.8 - 0.6 * float(np.exp(-0.3 * L))
            g = lambda n: a["%s_%d" % (n, L)]
            hsrc = a["h0"] if L == 0 else h1
            for j in range(2):
                gj = lambda n: a["%s_%d%d" % (n, L, j)]
                ys = lambda i: yT[i, 128 * j:128 * j + 128, :]
                with fw_scope(fw):
                    modc = fw.sb("modc", [128, 2048])
                    modl = fw.sb("modl", [128, 2048])
                    phase_mod(fw, a["c_ctx"], g("wmod"), g("bmod"), 0, 2048, modc, psb[7])
                    phase_mod(fw, a["c_lat"], g("wmod"), g("bmod"), 0, 2048, modl, psb[7])
                    phase_a1(fw, cfg, hsrc[0:TC, :], hsrc[TC:T, :], gj("w_my"), g("g1"), modc, modl, idb, p_tok, p_T, psb)
                with fw_scope(fw):
                    s65 = fw.sb("s65", [65, 64])
                    fw.dma(s65[:], a["sel65"])
                    o64 = fw.sb("o64", [64, 64])
                    fw.dma(o64[:], a["ones64"])
                    phase_da(fw, cfg, p_tok, a["rope"], g("lamb"), g("da_g"), lam_init, True, idb, s65, o64, ys(2), psb)
                with fw_scope(fw):
                    Jsb = fw.sb("Jsb", [64, 64])
                    fw.dma(Jsb[:], a["Jr"])
                    phase_gla(fw, cfg, p_tok, p_T, gj("wg2"), gj("bg2"), g("gla_g"), a["maskU"], a["cmask"], idf, Jsb,
                              o_gla, ys(3), psb)
                with fw_scope(fw):
                    phase_gla_fin(fw, cfg, p_tok, o_gla, g("gla_g"), idf, ys(3), psb, ctile=T_GR)
                with fw_scope(fw):
                    obd = fw.sb("obd", [128, 128])
                    fw.dma(obd[:], a["onesbd"])
                    phase_dn_prep(fw, cfg, p_T, gj("convw"), gj("alog"), gj("dtb"), obd, dn_qkv, dn_gates, psb)
                with fw_scope(fw):
                    Jsb = fw.sb("Jsb2", [64, 64])
                    fw.dma(Jsb[:], a["Jr"])
                    phase_dn(fw, cfg, dn_qkv, dn_gates, a["cmask2"], a["Eh"], a["mLs"], a["mUs"], a["mUi"], idf, Jsb,
                             o_dn, psb)
                with fw_scope(fw):
                    phase_gla_fin(fw, cfg, p_tok, o_dn, g("dn_g"), idf, ys(0), psb, ctile=T_DNZ)
                with fw_scope(fw):
                    phase_s5(fw, cfg, p_T, gj("lam_col"), gj("logdt_col"), gj("b_sm"), gj("c_nat"), a["svals"], a["mask4"],
                             a["mask4T"], idf, ys5, psb)
                with fw_scope(fw):
                    phase_s5_fin(fw, cfg, p_T, ys5, gj("dcol"), ys(1))
            last = (L == DEPTH - 1)
            if not last:
                ntile, nfirst, hin, yv, dst = T // 128, TC // 128, hsrc, yT, h1
            else:
                ntile, nfirst, hin, yv, dst = TL // 128, 0, hsrc[TC:T, :], yT[:, :, TC:T], out
            N = ntile * 128
            with fw_scope(fw):
                phase_b1a(fw, ntile, nfirst, hin, yv, a["c_ctx"], a["c_lat"], g("wmod"), g("bmod"), g("g1"), g("wgate"),
                          g("bgc"), g("wbr"), g("wout"), g("gluw"), g("glub"), idb, h_new[0:N, :], psb)
            with fw_scope(fw):
                phase_b1b(fw, ntile, nfirst, h_new[0:N, :], a["c_ctx"], a["c_lat"], g("wmod"), g("bmod"), g("g2"),
                          a["wrouter"], a["brouter"], idb, idf, fT[:, :, 0:N], Wr[0:N, :], psb)
            with fw_scope(fw):
                phase_b2(fw, ntile, nfirst, 17, fT[:, :, 0:N], Wr[0:N, :], h_new[0:N, :], a["c_ctx"], a["c_lat"],
                         g("wmod"), g("bmod"), g("weg"), g("weu"), g("wed"), a["fg"] if last else None, dst, psb)
        fw.finish()
    return nc


def kernel_fused(x, c, ctx, c_ctx, w_mod, b_mod, norm1_g, norm2_g, w_in, dn_conv, dn_a_log, dn_dt_bias, dn_norm_g,
                 s5_a_re, s5_a_im, s5_log_dt, s5_b_re, s5_b_im, s5_c_re, s5_c_im, s5_d, s5_glu_w, s5_glu_b,
                 da_lambda, da_norm_g, gla_w_gate, gla_b_gate, gla_norm_g, w_branch, w_gate, b_gate, w_out,
                 w_router, b_router, w_e_gate, w_e_up, w_e_down, final_g):
    f32 = lambda v: np.ascontiguousarray(np.asarray(v), dtype=np.float32)
    x, c, ctx, c_ctx = f32(x), f32(c), f32(ctx), f32(c_ctx)
    nb, TL, _ = x.shape
    TC = ctx.shape[1]
    cfg = Cfg(TC, TL)
    consts = _consts(cfg)
    cols = [my_cols(j) for j in range(2)]
    shared = dict(consts)
    shared.update(c_ctx=_col(c_ctx, 8), wrouter=f32(w_router), brouter=f32(b_router), fg=f32(final_g))
    for L in range(DEPTH):
        wl = f32(w_in[L])
        shared.update({"wmod_%d" % L: f32(w_mod[L]), "bmod_%d" % L: f32(b_mod[L]), "g1_%d" % L: f32(norm1_g[L]),
                       "g2_%d" % L: f32(norm2_g[L]), "lamb_%d" % L: f32(da_lambda[L]).reshape(128),
                       "da_g_%d" % L: f32(da_norm_g[L]).reshape(64, 1), "gla_g_%d" % L: f32(gla_norm_g[L]),
                       "dn_g_%d" % L: f32(dn_norm_g[L]), "wgate_%d" % L: f32(w_gate[L]), "bgc_%d" % L: _col(b_gate[L], 32),
                       "wbr_%d" % L: f32(w_branch[L]), "wout_%d" % L: f32(w_out[L]), "gluw_%d" % L: f32(s5_glu_w[L]),
                       "glub_%d" % L: _col(s5_glu_b[L], 2), "weg_%d" % L: f32(w_e_gate[L]), "weu_%d" % L: f32(w_e_up[L]),
                       "wed_%d" % L: f32(w_e_down[L])})
        for j in range(2):
            cj = cols[j]
            w_my = np.where(cj[None, :] >= 0, wl[:, np.maximum(cj, 0)], np.float32(0)).astype(np.float32)
            lam_col, logdt_col, b_sm, c_nat = _s5_layout(f32(s5_a_re[L]), f32(s5_a_im[L]), f32(s5_log_dt[L]), f32(s5_b_re[L]),
                                                         f32(s5_b_im[L]), f32(s5_c_re[L]), f32(s5_c_im[L]), j)
            dnc = f32(dn_conv[L])
            convw = np.stack([np.ascontiguousarray(dnc[:, q * 256 + 128 * j: q * 256 + 128 * j + 128].T) for q in range(3)])
            sfx = "_%d%d" % (L, j)
            shared.update({"w_my" + sfx: w_my, "wg2" + sfx: f32(gla_w_gate[L][:, :, 64 * j:64 * j + 64]),
                           "bg2" + sfx: f32(gla_b_gate[L][:, 64 * j:64 * j + 64]).reshape(2, 64, 1), "convw" + sfx: f32(convw),
                           "alog" + sfx: f32(dn_a_log[L][:, 2 * j:2 * j + 2]).reshape(4, 1),
                           "dtb" + sfx: f32(dn_dt_bias[L][:, 2 * j:2 * j + 2]).reshape(4, 1),
                           "lam_col" + sfx: lam_col, "logdt_col" + sfx: logdt_col, "b_sm" + sfx: b_sm, "c_nat" + sfx: c_nat,
                           "dcol" + sfx: f32(s5_d[L][128 * j:128 * j + 128]).reshape(128, 1)})
    ncf = build_fused(cfg)
    in_maps = []
    for core in range(8):
        b = core // 2
        m = dict(shared)
        m["h0"] = np.ascontiguousarray(np.concatenate([ctx[b], x[b]], axis=0))
        m["c_lat"] = _col(c[b], 8)
        in_maps.append(m)
    res = run_bass_kernel_spmd(ncf, in_maps, core_ids=list(range(8))).results
    return np.stack([np.asarray(res[2 * b]["out"]) for b in range(nb)]).astype(np.float32)


kernel_unfused = kernel
kernel = kernel_fused
```

```python
import contextlib
import numpy as np
import concourse.bass as bass
import concourse.mybir as mybir

F32 = mybir.dt.float32
BF16 = mybir.dt.bfloat16
I32 = mybir.dt.int32
U32 = mybir.dt.uint32
AF = mybir.ActivationFunctionType
ALU = mybir.AluOpType
AX = mybir.AxisListType

NDMA_SEMS = 40
NDMA_HW = 24
DBG = {}


def _box(ap):
    t = ap.tensor
    pat = ap.ap
    off = int(ap.offset)
    space = str(ap.space)
    if space == "PSUM":
        return (t.name, 0, 128, 0, 1 << 30)
    if space in ("SB", "PSUM"):
        psz = 1
        for s in list(t.shape)[1:]:
            psz *= int(s)
        p0 = off // psz
        f0 = off % psz
        st0, n0 = pat[0]
        pstep = (st0 // psz) if psz else 1
        p1 = p0 + (n0 - 1) * max(pstep, 0) + 1
        lo, hi = f0, f0
        for st, n in pat[1:]:
            if st < 0:
                lo += (n - 1) * st
            else:
                hi += (n - 1) * st
        return (t.name, p0, p1, lo, hi + 1)
    lo, hi = off, off
    for st, n in pat:
        if st < 0:
            lo += (n - 1) * st
        else:
            hi += (n - 1) * st
    return (t.name, 0, 1, lo, hi + 1)


class FW:
    def __init__(self, nc, stack, same_engine_waits=True):
        self.nc = nc
        self.stack = stack
        self.E = dict(pe=nc.tensor, act=nc.scalar, dve=nc.vector, pool=nc.gpsimd, sp=nc.sync)
        self.csem = {}
        self.ccnt = {}
        for e in ("pe", "act", "dve", "pool"):
            self.csem[e] = stack.enter_context(nc.semaphore("c_" + e))
            self.ccnt[e] = 0
        self.dsem = [stack.enter_context(nc.semaphore("d%d" % i)) for i in range(NDMA_SEMS)]
        self.dval = [0] * NDMA_SEMS
        self.dnext = 0
        self.dnext_sw = 0
        self.known = {q: {} for q in self.E}
        self.reg = {}
        self.same = same_engine_waits
        self.n_inst = 0
        self.n_wait = 0
        self.out_events = []
        self._uid = 0

    def sb(self, name, shape, dtype=F32):
        self._uid += 1
        return self.stack.enter_context(self.nc.sbuf_tensor("%s_%d" % (name, self._uid), list(shape), dtype))

    def ps(self, name, shape, dtype=F32):
        self._uid += 1
        return self.stack.enter_context(self.nc.psum_tensor("%s_%d" % (name, self._uid), list(shape), dtype))

    def _wait(self, q, ev):
        sem, val = ev
        k = id(sem)
        if self.known[q].get(k, 0) >= val:
            return
        self.E[q].wait_ge(sem, val)
        self.known[q][k] = val
        self.n_wait += 1

    def _deps(self, q, reads, writes):
        deps = []
        for ap in reads:
            b = _box(ap)
            for ent in self.reg.get(b[0], ()):
                eb = ent[0]
                if eb[1] < b[2] and b[1] < eb[2] and eb[3] < b[4] and b[3] < eb[4]:
                    if ent[1] is not None:
                        deps.append(ent[1])
        for ap in writes:
            b = _box(ap)
            for ent in self.reg.get(b[0], ()):
                eb = ent[0]
                if eb[1] < b[2] and b[1] < eb[2] and eb[3] < b[4] and b[3] < eb[4]:
                    if ent[1] is not None:
                        deps.append(ent[1])
                    deps.extend(ent[2].values())
        for ev in deps:
            if (not self.same or q == "pe") and q in self.csem and ev[0] is self.csem[q]:
                continue
            self._wait(q, ev)

    def _record(self, ev, reads, writes):
        for ap in reads:
            b = _box(ap)
            lst = self.reg.setdefault(b[0], [])
            for ent in lst:
                if ent[0] == b:
                    ent[2][id(ev[0])] = ev
                    break
            else:
                lst.append([b, None, {id(ev[0]): ev}])
        for ap in writes:
            b = _box(ap)
            lst = self.reg.setdefault(b[0], [])
            keep = []
            for ent in lst:
                eb = ent[0]
                if eb[1] >= b[1] and eb[2] <= b[2] and eb[3] >= b[3] and eb[4] <= b[4]:
                    continue
                keep.append(ent)
            keep.append([b, ev, {}])
            self.reg[b[0]] = keep

    def op(self, q, fn, reads, writes):
        reads = [r for r in reads if r is not None and not isinstance(r, (int, float))]
        self._deps(q, reads, writes)
        ins = fn()
        self.ccnt[q] += 1
        ev = (self.csem[q], self.ccnt[q])
        ins.then_inc(self.csem[q], 1)
        if not self.same or q == "pe":
            self.known[q][id(self.csem[q])] = self.ccnt[q]
        self._record(ev, reads, writes)
        self.n_inst += 1
        return ins

    def dma(self, out, in_, q="sp", is_output=False, **kw):
        if q == "pool":
            i = NDMA_HW + (self.dnext_sw % (NDMA_SEMS - NDMA_HW))
            self.dnext_sw += 1
        else:
            i = self.dnext % NDMA_HW
            self.dnext += 1
        if self.dval[i] > 0:
            self._wait(q, (self.dsem[i], self.dval[i]))
        self._deps(q, [in_], [out])
        ins = self.E[q].dma_start(out=out, in_=in_, **kw)
        self.dval[i] += 16
        ev = (self.dsem[i], self.dval[i])
        ins.then_inc(self.dsem[i], 16)
        self._record(ev, [in_], [out])
        if is_output:
            self.out_events.append(ev)
        self.n_inst += 1
        return ins

    def cc(self, kind, ins, outs, groups, op=None):
        q = "pool"
        i = NDMA_HW + (self.dnext_sw % (NDMA_SEMS - NDMA_HW))
        self.dnext_sw += 1
        if self.dval[i] > 0:
            self._wait(q, (self.dsem[i], self.dval[i]))
        self._deps(q, list(ins), list(outs))
        inst = self.nc.gpsimd.collective_compute(kind, op if op is not None else ALU.bypass, replica_groups=groups, ins=list(ins), outs=list(outs))
        self.dval[i] += 16
        ev = (self.dsem[i], self.dval[i])
        inst.then_inc(self.dsem[i], 16)
        self._record(ev, list(ins), list(outs))
        self.n_inst += 1
        return inst

    def barrier(self):
        evs = [(self.csem[e], self.ccnt[e]) for e in self.csem if self.ccnt[e] > 0]
        evs += [(self.dsem[i], self.dval[i]) for i in range(NDMA_SEMS) if self.dval[i] > 0]
        for q in self.E:
            for ev in evs:
                self._wait(q, ev)
        self.reg = {}

    def finish(self):
        evs = [(self.dsem[i], self.dval[i]) for i in range(NDMA_SEMS) if self.dval[i] > 0]
        evs += [(self.csem[e], self.ccnt[e]) for e in self.csem if self.ccnt[e] > 0]
        for ev in evs:
            self._wait("sp", ev)

    def mm(self, out, lhsT, rhs, start=True, stop=True, **kw):
        return self.op("pe", lambda: self.nc.tensor.matmul(out, lhsT, rhs, start=start, stop=stop, **kw),
                       [lhsT, rhs] + ([] if start else [out]), [out])

    def tr(self, out, in_, ident):
        return self.op("pe", lambda: self.nc.tensor.transpose(out, in_, ident), [in_, ident], [out])

    def act(self, out, in_, func, bias=None, scale=None, accum_out=None, q="act"):
        kw = {}
        if bias is not None:
            kw["bias"] = bias
        if scale is not None:
            kw["scale"] = scale
        if accum_out is not None:
            kw["accum_out"] = accum_out
        rd = [in_, bias, scale]
        wr = [out] + ([accum_out] if accum_out is not None else [])
        return self.op(q, lambda: self.nc.scalar.activation(out, in_, func, **kw), rd, wr)

    def tt(self, out, a, b, op, q="dve"):
        return self.op(q, lambda: self.E[q].tensor_tensor(out, a, b, op), [a, b], [out])

    def ts(self, out, a, s1, s2=None, op0=ALU.mult, op1=None, accum_out=None, q="dve"):
        kw = {}
        if op1 is not None:
            kw["op1"] = op1
        if accum_out is not None:
            kw["accum_out"] = accum_out
        wr = [out] + ([accum_out] if accum_out is not None else [])
        return self.op(q, lambda: self.E[q].tensor_scalar(out, a, s1, s2, op0, **kw), [a, s1, s2], wr)

    def stt(self, out, in0, scalar, in1, op0, op1, accum_out=None):
        kw = {}
        if accum_out is not None:
            kw["accum_out"] = accum_out
        wr = [out] + ([accum_out] if accum_out is not None else [])
        return self.op("dve", lambda: self.nc.vector.scalar_tensor_tensor(out, in0, scalar, in1, op0, op1, **kw),
                       [in0, scalar, in1], wr)

    def copy(self, out, in_, q="dve"):
        if q == "act":
            return self.op("act", lambda: self.nc.scalar.copy(out, in_), [in_], [out])
        return self.op(q, lambda: self.E[q].tensor_copy(out, in_), [in_], [out])

    def memset(self, out, val, q="dve"):
        return self.op(q, lambda: self.E[q].memset(out, val), [], [out])

    def reduce(self, out, in_, op, axis=AX.X, q="dve", **kw):
        return self.op(q, lambda: self.E[q].tensor_reduce(out, in_, axis, op, **kw), [in_], [out])

    def recip(self, out, in_):
        return self.op("dve", lambda: self.nc.vector.reciprocal(out, in_), [in_], [out])

    def scan(self, out, d0, d1, initial, op0=ALU.mult, op1=ALU.add):
        rd = [d0, d1, initial]
        return self.op("dve", lambda: self.nc.vector.tensor_tensor_scan(out, d0, d1, initial, op0, op1), rd, [out])


@contextlib.contextmanager
def fw_scope(fw):
    outer = fw.stack
    with contextlib.ExitStack() as st:
        fw.stack = st
        try:
            yield
        finally:
            fw.barrier()
            fw.stack = outer


D_MODEL = 1024
DN_CHUNK = 64
NORM_EPS = 1e-6
_PROJ = (("dn_qkv", 768), ("dn_z", 256), ("dn_a", 8), ("dn_b", 8), ("s5_u", 256), ("da_q", 256), ("da_k", 256),
         ("da_v", 256), ("gla_q", 128), ("gla_k", 128), ("gla_v", 256), ("gla_r", 256), ("gla_gate", 32))
_OFF = {}
_o = 0
for _n, _w in _PROJ:
    _OFF[_n] = _o
    _o += _w
D_IN = _o
NCOL = 12 * 128
T_DNQ, T_DNK, T_DNV, T_DNZ, T_S5U, T_DAQ, T_DAK, T_DAV, T_GQK, T_GV, T_GR, T_MISC = range(12)


def my_cols(j):
    hs = (2 * j, 2 * j + 1)
    ar = np.arange
    cols = []
    cols += [_OFF["dn_qkv"] + h * 64 + ar(64) for h in hs]
    cols += [_OFF["dn_qkv"] + 256 + h * 64 + ar(64) for h in hs]
    cols += [_OFF["dn_qkv"] + 512 + h * 64 + ar(64) for h in hs]
    cols += [_OFF["dn_z"] + h * 64 + ar(64) for h in hs]
    cols += [_OFF["s5_u"] + j * 128 + ar(128)]
    cols += [_OFF["da_q"] + h * 64 + ar(64) for h in hs]
    cols += [_OFF["da_k"] + h * 64 + ar(64) for h in hs]
    cols += [_OFF["da_v"] + h * 64 + ar(64) for h in hs]
    cols += [_OFF["gla_q"] + h * 32 + ar(32) for h in hs]
    cols += [_OFF["gla_k"] + h * 32 + ar(32) for h in hs]
    cols += [_OFF["gla_v"] + h * 64 + ar(64) for h in hs]
    cols += [_OFF["gla_r"] + h * 64 + ar(64) for h in hs]
    misc = -np.ones(128, np.int64)
    misc[0:16] = _OFF["gla_gate"] + ar(16)
    misc[32:48] = _OFF["gla_gate"] + 16 + ar(16)
    k = 64
    for nm in ("dn_a", "dn_b"):
        for d in range(2):
            for h in hs:
                misc[k] = _OFF[nm] + d * 4 + h
                k += 1
    cols.append(misc)
    cols = np.concatenate(cols)
    assert cols.shape[0] == NCOL
    return cols


class Cfg:
    def __init__(self, tc=256, tl=8192):
        self.TC = tc
        self.TL = tl
        self.T = tc + tl
        assert tc % 128 == 0 and tl % 128 == 0


def phase_mod(fw, c_col, wmod, bmod, lo, hi, out, ps, wbufs=None):
    n = hi - lo
    fw.dma(out[:, 0:n], bmod[lo:hi].partition_broadcast(128))
    with fw_scope(fw):
        sc = fw.sb("sc", [128, 8])
        rep = fw.sb("screp", [128, 8, 128])
        sg = fw.sb("scsg", [128, 8])
        wb2 = [fw.sb("wmodb%d" % i, [128, 8, 256]) for i in range(2)]
        fw.dma(sc[:], c_col)
        fw.act(sg[:], sc[:], AF.Sigmoid)
        fw.tt(sc[:], sc[:], sg[:], ALU.mult)
        fw.copy(rep[:], sc[:].unsqueeze(2).to_broadcast([128, 8, 128]))
        wv = wmod.rearrange("(k p) n -> p k n", p=128)
        i = 0
        for c0 in range(0, n, 256):
            wb = wb2[i % 2]
            fw.dma(wb[:], wv[:, :, lo + c0:lo + c0 + 256], q=("sp" if i % 2 == 0 else "pool"))
            for k in range(8):
                fw.mm(ps[:, :256], rep[:, k, :], wb[:, k, :], start=(k == 0), stop=(k == 7))
            fw.tt(out[:, c0:c0 + 256], ps[:, :256], out[:, c0:c0 + 256], ALU.add)
            i += 1


def phase_a1(fw, cfg, h_ctx, h_lat, w_my, g1, modc, modl, identb, p_tok, p_T, psb):
    nc = fw.nc
    wb = fw.sb("a1_w", [128, 8, NCOL], BF16)
    wv = w_my.rearrange("(k p) n -> p k n", p=128)
    for k in range(8):
        fw.dma(wb[:, k, :], wv[:, k, :], q="pool")
    g1b = fw.sb("a1_g1", [128, 1024])
    fw.dma(g1b[:], g1.partition_broadcast(128))
    gs = {}
    for nm, m in (("c", modc), ("l", modl)):
        t = fw.sb("a1_gs" + nm, [128, 1024])
        fw.stt(t[:], m[:, 1024:2048], 1.0, g1b[:], ALU.add, ALU.mult)
        gs[nm] = (t, m)
    xt = [fw.sb("a1_x%d" % i, [128, 1024]) for i in range(2)]
    junk = fw.sb("a1_junk", [128, 1024])
    tmp = fw.sb("a1_tmp", [128, 1024])
    ab = [fw.sb("a1_ab%d" % i, [128, 1024], BF16) for i in range(2)]
    ss = [fw.sb("a1_ss%d" % i, [128, 1]) for i in range(2)]
    rs = [fw.sb("a1_rs%d" % i, [128, 1]) for i in range(2)]
    aT = [fw.sb("a1_aT%d" % i, [128, 8, 512], BF16) for i in range(2)]
    otok = [fw.sb("a1_ot%d" % i, [128, NCOL]) for i in range(2)]
    ofm = [fw.sb("a1_of%d" % i, [128, 512]) for i in range(3)]
    pst = psb[0].bitcast(BF16)
    ntile = cfg.T // 128
    ti = 0
    gi = 0
    ev = 0
    for g0 in range(0, ntile, 4):
        gn = min(4, ntile - g0)
        aTg = aT[gi % 2]
        for tt_ in range(gn):
            t = g0 + tt_
            x = xt[ti % 2]
            if t * 128 < cfg.TC:
                src = h_ctx[t * 128:(t + 1) * 128, :]
                gst, m = gs["c"]
            else:
                r0 = t * 128 - cfg.TC
                src = h_lat[r0:r0 + 128, :]
                gst, m = gs["l"]
            fw.dma(x[:], src)
            s_, r_ = ss[ti % 2], rs[ti % 2]
            fw.act(junk[:], x[:], AF.Square, accum_out=s_[:])
            fw.act(r_[:], s_[:], AF.Sqrt, bias=NORM_EPS, scale=1.0 / D_MODEL)
            fw.recip(r_[:], r_[:])
            fw.stt(tmp[:], x[:], r_[:], gst[:], ALU.mult, ALU.mult)
            a_ = ab[ti % 2]
            fw.tt(a_[:], tmp[:], m[:, 0:1024], ALU.add, q="pool")
            for k in range(8):
                fw.tr(pst[:, k * 128:(k + 1) * 128], a_[:, k * 128:(k + 1) * 128], identb[:])
            fw.copy(aTg[:, :, tt_ * 128:(tt_ + 1) * 128], pst.rearrange("p (k t) -> p k t", k=8),
                    q=("act" if ti % 2 else "dve"))
            ti += 1
        ntok = gn * 128
        for tt_ in range(gn):
            t = g0 + tt_
            ot = otok[tt_ % 2]
            for cc in range(3):
                ps = psb[1 + (ev % 4)]
                for k in range(8):
                    fw.mm(ps[:, :512], aTg[:, k, tt_ * 128:(tt_ + 1) * 128], wb[:, k, cc * 512:(cc + 1) * 512],
                          start=(k == 0), stop=(k == 7))
                fw.copy(ot[:, cc * 512:(cc + 1) * 512], ps[:, :512], q=("act" if ev % 2 else "dve"))
                ev += 1
            fw.dma(p_tok[t * 128:(t + 1) * 128, :], ot[:], q="sp")
        for ct in range(12):
            ps = psb[1 + (ev % 4)]
            for k in range(8):
                fw.mm(ps[:, :ntok], wb[:, k, ct * 128:(ct + 1) * 128], aTg[:, k, :ntok], start=(k == 0), stop=(k == 7))
            of = ofm[ct % 3]
            fw.copy(of[:, :ntok], ps[:, :ntok], q=("act" if ev % 2 else "dve"))
            ev += 1
            fw.dma(p_T[ct, :, g0 * 128:g0 * 128 + ntok], of[:, :ntok], q="sp")
        gi += 1


def phase_da(fw, cfg, p_tok, rope, da_lambda_b, da_g_col, lam_init, with_ctx, identb, sel65, ones64, y_da, psb):
    TC, TL, T = cfg.TC, cfg.TL, cfg.T
    PQ = "dve" if DBG.get("nopool") else "pool"
    nkt = T // 128
    scale = 32 ** -0.5
    lb = fw.sb("da_lb", [64, 4, 32])
    fw.dma(lb[:], da_lambda_b.partition_broadcast(64).rearrange("p (a b) -> p a b", a=4))
    pr = fw.sb("da_pr", [64, 2, 32])
    fw.tt(pr[:, 0, :], lb[:, 0, :], lb[:, 1, :], ALU.mult)
    fw.tt(pr[:, 1, :], lb[:, 2, :], lb[:, 3, :], ALU.mult)
    s2 = fw.sb("da_s2", [64, 2])
    fw.reduce(s2[:], pr[:], ALU.add)
    fw.act(s2[:], s2[:], AF.Exp)
    neglam = fw.sb("da_nl", [64, 1])
    fw.tt(neglam[:], s2[:, 1:2], s2[:, 0:1], ALU.subtract)
    fw.ts(neglam[:], neglam[:], -float(lam_init), None, op0=ALU.add)
    gcol = fw.sb("da_g", [64, 1])
    fw.dma(gcol[:], da_g_col)
    fw.ts(gcol[:], gcol[:], 1.0 - float(lam_init), None, op0=ALU.mult)
    if DBG.get("da_stop") == 1:
        return
    qT = [fw.sb("da_qT%d" % h, [64, T], BF16) for h in range(2)]
    kT = [fw.sb("da_kT%d" % h, [64, T], BF16) for h in range(2)]
    va = fw.sb("da_va", [128, nkt, 2, 66], BF16)
    if not DBG.get("nova"):
        fw.memset(va[:], 1.0, q=PQ)
    qk = [fw.sb("da_qk%d" % i, [128, 256]) for i in range(2)]
    vt = [fw.sb("da_vt%d" % i, [128, 128]) for i in range(2)]
    cs = [fw.sb("da_cs%d" % i, [128, 32]) for i in range(2)]
    t1 = fw.sb("da_t1", [128, 8, 16])
    t2 = fw.sb("da_t2", [128, 8, 16])
    rb = [fw.sb("da_rb%d" % i, [128, 256], BF16) for i in range(2)]
    pst = psb[0].bitcast(BF16)
    c0q = T_DAQ * 128
    for t in range(nkt):
        x = qk[t % 2]
        v = vt[t % 2]
        fw.dma(x[:], p_tok[t * 128:(t + 1) * 128, c0q:c0q + 256])
        fw.dma(v[:], p_tok[t * 128:(t + 1) * 128, T_DAV * 128:(T_DAV + 1) * 128], q=("pool" if PQ == "pool" else "sp"))
        if not DBG.get("nova"):
            fw.copy(va[:, t, :, 0:64], v[:].rearrange("p (h d) -> p h d", h=2), q=PQ)
        r = rb[t % 2]
        if t * 128 >= TC and not DBG.get("norope"):
            c = cs[t % 2]
            fw.dma(c[:], rope[t * 128 - TC:(t + 1) * 128 - TC, :])
            xv = x[:].rearrange("p (g two d) -> p g two d", g=8, two=2)
            rv = r[:].rearrange("p (g two d) -> p g two d", g=8, two=2)
            cb = c[:, 0:16].unsqueeze(1).to_broadcast([128, 8, 16])
            sb_ = c[:, 16:32].unsqueeze(1).to_broadcast([128, 8, 16])
            fw.tt(t1[:], xv[:, :, 0, :], cb, ALU.mult)
            fw.tt(t2[:], xv[:, :, 1, :], sb_, ALU.mult)
            fw.tt(rv[:, :, 0, :], t1[:], t2[:], ALU.subtract)
            fw.tt(t1[:], xv[:, :, 0, :], sb_, ALU.mult, q=PQ)
            fw.tt(t2[:], xv[:, :, 1, :], cb, ALU.mult, q=PQ)
            fw.tt(rv[:, :, 1, :], t1[:], t2[:], ALU.add, q=PQ)
        else:
            fw.copy(r[:], x[:], q="act")
        if DBG.get("notr"):
            continue
        for i in range(4):
            fw.tr(pst[0:64, i * 128:(i + 1) * 128], r[:, i * 64:(i + 1) * 64], identb[:])
        if DBG.get("nocp"):
            continue
        for h in range(2):
            fw.copy(qT[h][:, t * 128:(t + 1) * 128], pst[0:64, h * 128:(h + 1) * 128], q="act")
            fw.copy(kT[h][:, t * 128:(t + 1) * 128], pst[0:64, (2 + h) * 128:(3 + h) * 128], q=("dve" if DBG.get("cpdve") else "act"))
    if DBG.get("da_stop") == 2:
        return
    pT = [fw.sb("da_pT%d" % i, [128, 512], BF16) for i in range(6)]
    sbanks = [psb[0], psb[3], psb[4], psb[5], psb[6], psb[7]]
    A = [fw.sb("da_A%d" % i, [65, 512]) for i in range(2)]
    rr = [fw.sb("da_rr%d" % i, [64, 512]) for i in range(2)]
    o = fw.sb("da_o", [64, 512])
    sq = fw.sb("da_sq", [64, 512])
    yo = [fw.sb("da_yo%d" % i, [64, 512]) for i in range(2)]
    chunks = []
    if with_ctx:
        chunks.append((0, TC, 0, TC // 128))
    for q0 in range(TC, T, 512):
        chunks.append((q0, min(512, T - q0), 0, nkt))
    it = 0
    yi = 0
    for (q0, nq, kt0, kt1) in chunks:
        for h in range(2):
            acc = [psb[1], psb[2]]
            its = [(kt, m) for kt in range(kt0, kt1) for m in range(2)]
            SK = 4
            base = it
            for idx in range(len(its) + SK):
                if idx < len(its):
                    kt, m = its[idx]
                    sp = sbanks[(base + idx) % 6]
                    fw.mm(sp[:, :nq], kT[h][m * 32:(m + 1) * 32, kt * 128:(kt + 1) * 128],
                          qT[h][m * 32:(m + 1) * 32, q0:q0 + nq])
                    pt = pT[(base + idx) % 6]
                    fw.act(pt[:, :nq], sp[:, :nq], AF.Exp, scale=scale)
                if idx >= SK:
                    kt, m = its[idx - SK]
                    pt = pT[(base + idx - SK) % 6]
                    fw.mm(acc[m][0:65, :nq], va[:, kt, h, 0:65], pt[:, :nq], start=(kt == kt0), stop=(kt == kt1 - 1))
            it += len(its)
            for m in range(2):
                fw.copy(A[m][:, :nq], acc[m][0:65, :nq], q=("dve" if m == 0 else "act"))
            if DBG.get("da_stop") == 3:
                continue
            for m in range(2):
                bp = psb[1 + m]
                fw.mm(bp[0:64, :nq], sel65[:], A[m][:, :nq])
                fw.recip(rr[m][:, :nq], bp[0:64, :nq])
            fw.tt(o[:, :nq], A[0][0:64, :nq], rr[0][:, :nq], ALU.mult)
            fw.tt(sq[:, :nq], A[1][0:64, :nq], rr[1][:, :nq], ALU.mult, q="pool")
            fw.stt(o[:, :nq], sq[:, :nq], neglam[:], o[:, :nq], ALU.mult, ALU.add)
            fw.tt(sq[:, :nq], o[:, :nq], o[:, :nq], ALU.mult, q="pool")
            bp = psb[1]
            fw.mm(bp[0:64, :nq], ones64[:], sq[:, :nq])
            fw.act(rr[0][:, :nq], bp[0:64, :nq], AF.Sqrt, bias=1e-5, scale=1.0 / 64)
            fw.recip(rr[0][:, :nq], rr[0][:, :nq])
            y = yo[yi % 2]
            yi += 1
            fw.stt(y[:, :nq], o[:, :nq], gcol[:], rr[0][:, :nq], ALU.mult, ALU.mult)
            fw.dma(y_da[h * 64:(h + 1) * 64, q0:q0 + nq], y[:, :nq])


def tile_groups(ntile, first):
    gs = []
    if first:
        gs.append((0, first, "A"))
    t = first
    while t < ntile:
        n = min(4, ntile - t)
        gs.append((t, n, "B"))
        t += n
    return gs


def norm_mod_tile(fw, x, tmp, ss, rs, gs_b, sh_b, out, out_q="pool"):
    fw.act(tmp[:], x[:], AF.Square, accum_out=ss[:])
    fw.act(rs[:], ss[:], AF.Sqrt, bias=NORM_EPS, scale=1.0 / D_MODEL)
    fw.recip(rs[:], rs[:])
    fw.stt(tmp[:], x[:], rs[:], gs_b, ALU.mult, ALU.mult)
    fw.tt(out[:], tmp[:], sh_b, ALU.add, q=out_q)


def phase_b1a(fw, ntile, nfirst, h_in, yT, cA, cB, wmod, bmod, g1, wgate, bgate_col, wbranch, wout,
              gluw, glub_col, identb, h_new, psb):
    N = ntile * 128
    wg = fw.sb("b1_wg", [128, 8, 4096], BF16)
    wgv = wgate.rearrange("(k p) n -> p k n", p=128)
    for k in range(8):
        for c in range(2):
            fw.dma(wg[:, k, c * 2048:(c + 1) * 2048], wgv[:, k, c * 2048:(c + 1) * 2048], q="pool")
    wbr = fw.sb("b1_wbr", [128, 4, 2, 1024], BF16)
    for i in range(4):
        fw.dma(wbr[:, i, :, :], wbranch[i].rearrange("(c p) n -> p c n", p=128), q="pool")
    wo = fw.sb("b1_wo", [128, 8, 1024], BF16)
    fw.dma(wo[:], wout.rearrange("(k p) n -> p k n", p=128), q="pool")
    glw = fw.sb("b1_glw", [128, 2, 256], BF16)
    fw.dma(glw[:], gluw.rearrange("(c p) n -> p c n", p=128), q="pool")
    bgc = fw.sb("b1_bgc", [128, 32])
    fw.dma(bgc[:], bgate_col)
    glb = fw.sb("b1_glb", [128, 2])
    fw.dma(glb[:], glub_col)
    mod = fw.sb("b1_mod", [128, 3072])
    x = [fw.sb("b1_x%d" % i, [128, 1024]) for i in range(2)]
    tmp = fw.sb("b1_tmp", [128, 1024])
    ab = [fw.sb("b1_ab%d" % i, [128, 1024], BF16) for i in range(2)]
    ss = [fw.sb("b1_ss%d" % i, [128, 1]) for i in range(2)]
    rs = [fw.sb("b1_rs%d" % i, [128, 1]) for i in range(2)]
    aT = fw.sb("b1_aT", [128, 8, 512], BF16)
    ybf = fw.sb("b1_ybf", [128, 4, 2, 512], BF16)
    y32 = fw.sb("b1_y32", [128, 2, 512])
    z32 = fw.sb("b1_z32", [128, 2, 512])
    u32 = fw.sb("b1_u32", [128, 512])
    zb = fw.sb("b1_zb", [128, 2, 512], BF16)
    gate = [fw.sb("b1_gate%d" % i, [128, 512], BF16) for i in range(2)]
    t2 = [fw.sb("b1_t2%d" % i, [128, 512]) for i in range(2)]
    acc = [fw.sb("b1_acc%d" % i, [128, 512]) for i in range(2)]
    accT = fw.sb("b1_accT", [128, 8, 512], BF16)
    hn = [fw.sb("b1_hn%d" % i, [128, 1024]) for i in range(2)]
    pst = psb[0].bitcast(BF16)
    cur = None
    ti = 0
    ev = 0
    for (t0, gn, which) in tile_groups(ntile, nfirst):
        if which != cur:
            phase_mod(fw, cA if which == "A" else cB, wmod, bmod, 0, 3072, mod, psb[7])
            fw.dma(tmp[:], g1.partition_broadcast(128))
            fw.stt(mod[:, 1024:2048], mod[:, 1024:2048], 1.0, tmp[:], ALU.add, ALU.mult)
            cur = which
        ntok = gn * 128
        tok0 = t0 * 128
        for tt_ in range(gn):
            xx = x[ti % 2]
            fw.dma(xx[:], h_in[tok0 + tt_ * 128: tok0 + (tt_ + 1) * 128, :])
            a_ = ab[ti % 2]
            norm_mod_tile(fw, xx, tmp, ss[ti % 2], rs[ti % 2], mod[:, 1024:2048], mod[:, 0:1024], a_)
            for k in range(8):
                fw.tr(pst[:, k * 128:(k + 1) * 128], a_[:, k * 128:(k + 1) * 128], identb[:])
            fw.copy(aT[:, :, tt_ * 128:(tt_ + 1) * 128], pst.rearrange("p (k t) -> p k t", k=8),
                    q=("act" if ti % 2 else "dve"))
            ti += 1
        for i in (0, 2, 3):
            for c in range(2):
                fw.dma(ybf[:, i, c, :ntok], yT[i, c * 128:(c + 1) * 128, tok0:tok0 + ntok], q="pool")
        for c in range(2):
            fw.dma(y32[:, c, :ntok], yT[1, c * 128:(c + 1) * 128, tok0:tok0 + ntok])
        yv, zv = y32[:, :, :ntok], z32[:, :, :ntok]
        fw.tt(zv, yv, yv, ALU.mult)
        fw.ts(zv, zv, 0.044715, 1.0, op0=ALU.mult, op1=ALU.add)
        fw.tt(zv, zv, yv, ALU.mult)
        fw.act(zv, zv, AF.Sigmoid, scale=1.5957691216057308)
        fw.tt(zv, zv, yv, ALU.mult)
        fw.copy(zb[:, :, :ntok], zv, q="pool")
        for oc in range(2):
            ps = psb[1 + (ev % 2)]
            ev += 1
            for c in range(2):
                fw.mm(ps[:, :ntok], glw[:, c, oc * 128:(oc + 1) * 128], zb[:, c, :ntok], start=(c == 0), stop=(c == 1))
            fw.act(u32[:, :ntok], ps[:, :ntok], AF.Sigmoid, bias=glb[:, oc:oc + 1])
            fw.tt(ybf[:, 1, oc, :ntok], u32[:, :ntok], z32[:, oc, :ntok], ALU.mult)
        gi = 0
        for ot in range(8):
            ac = acc[ot % 2]
            for i in range(4):
                psg = psb[1 + (gi % 2)]
                psy = psb[3 + (gi % 2)]
                for k in range(8):
                    fw.mm(psg[:, :ntok], wg[:, k, i * 1024 + ot * 128: i * 1024 + (ot + 1) * 128], aT[:, k, :ntok],
                          start=(k == 0), stop=(k == 7))
                for c in range(2):
                    fw.mm(psy[:, :ntok], wbr[:, i, c, ot * 128:(ot + 1) * 128], ybf[:, i, c, :ntok],
                          start=(c == 0), stop=(c == 1))
                g_ = gate[gi % 2]
                fw.act(g_[:, :ntok], psg[:, :ntok], AF.Sigmoid, bias=bgc[:, i * 8 + ot: i * 8 + ot + 1])
                if i == 0:
                    fw.tt(ac[:, :ntok], psy[:, :ntok], g_[:, :ntok], ALU.mult)
                else:
                    tq = t2[gi % 2]
                    fw.tt(tq[:, :ntok], psy[:, :ntok], g_[:, :ntok], ALU.mult)
                    if i < 3:
                        fw.tt(ac[:, :ntok], ac[:, :ntok], tq[:, :ntok], ALU.add, q="pool")
                    else:
                        fw.tt(accT[:, ot, :ntok], ac[:, :ntok], tq[:, :ntok], ALU.add, q="pool")
                gi += 1
        for tt_ in range(gn):
            xx = x[ti % 2]
            ti += 1
            fw.dma(xx[:], h_in[tok0 + tt_ * 128: tok0 + (tt_ + 1) * 128, :])
            hh = hn[tt_ % 2]
            for cc in range(2):
                ps = psb[5 + (ev % 2)]
                ev += 1
                for k in range(8):
                    fw.mm(ps[:, :512], accT[:, k, tt_ * 128:(tt_ + 1) * 128], wo[:, k, cc * 512:(cc + 1) * 512],
                          start=(k == 0), stop=(k == 7))
                fw.tt(hh[:, cc * 512:(cc + 1) * 512], ps[:, :512], mod[:, 2048 + cc * 512: 2048 + (cc + 1) * 512], ALU.mult)
            fw.tt(hh[:], hh[:], xx[:], ALU.add, q="pool")
            fw.dma(h_new[tok0 + tt_ * 128: tok0 + (tt_ + 1) * 128, :], hh[:])


def phase_b1b(fw, ntile, nfirst, h_new, cA, cB, wmod, bmod, g2, wrouter, brouter, identb, identf, fT, Wr, psb):
    mod = fw.sb("bb_mod", [128, 2048])
    wr = fw.sb("bb_wr", [128, 8, 16])
    fw.dma(wr[:], wrouter.rearrange("(k p) n -> p k n", p=128))
    brb = fw.sb("bb_brb", [128, 16])
    fw.dma(brb[:], brouter.partition_broadcast(128))
    x = [fw.sb("bb_x%d" % i, [128, 1024]) for i in range(2)]
    tmp = fw.sb("bb_tmp", [128, 1024])
    f32 = [fw.sb("bb_f%d" % i, [128, 1024]) for i in range(2)]
    fb = [fw.sb("bb_fb%d" % i, [128, 1024], BF16) for i in range(2)]
    ss = [fw.sb("bb_ss%d" % i, [128, 1]) for i in range(2)]
    rs = [fw.sb("bb_rs%d" % i, [128, 1]) for i in range(2)]
    fTg = [fw.sb("bb_fT%d" % i, [128, 8, 128], BF16) for i in range(2)]
    fT32 = [fw.sb("bb_fT32%d" % i, [128, 8, 128]) for i in range(2)]
    sc = [fw.sb("bb_sc%d" % i, [128, 16]) for i in range(2)]
    bi = fw.sb("bb_bi", [128, 4, 4])
    eq = fw.sb("bb_eq", [128, 4, 4])
    tb = fw.sb("bb_tb", [128, 4, 4])
    m1 = fw.sb("bb_m1", [128, 4])
    m2 = fw.sb("bb_m2", [128, 4])
    gsum = fw.sb("bb_gs", [128, 4])
    gmax = fw.sb("bb_gm", [128, 1])
    oh = fw.sb("bb_oh", [128, 4])
    den = fw.sb("bb_den", [128, 1])
    wout_t = [fw.sb("bb_w%d" % i, [128, 16]) for i in range(2)]
    pst = psb[0].bitcast(BF16)
    cur = None
    ti = 0
    for (t0, gn, which) in tile_groups(ntile, nfirst):
        if which != cur:
            phase_mod(fw, cA if which == "A" else cB, wmod, bmod, 3072, 5120, mod, psb[7])
            fw.dma(tmp[:], g2.partition_broadcast(128))
            fw.stt(mod[:, 1024:2048], mod[:, 1024:2048], 1.0, tmp[:], ALU.add, ALU.mult)
            cur = which
        for tt_ in range(gn):
            t = t0 + tt_
            xx = x[ti % 2]
            ff = f32[ti % 2]
            fbb = fb[ti % 2]
            fw.dma(xx[:], h_new[t * 128:(t + 1) * 128, :])
            norm_mod_tile(fw, xx, tmp, ss[ti % 2], rs[ti % 2], mod[:, 1024:2048], mod[:, 0:1024], ff)
            fw.copy(fbb[:], ff[:], q="act")
            for k in range(8):
                fw.tr(pst[:, k * 128:(k + 1) * 128], fbb[:, k * 128:(k + 1) * 128], identb[:])
            fg = fTg[ti % 2]
            fw.copy(fg[:], pst.rearrange("p (k t) -> p k t", k=8), q="dve")
            fw.dma(fT[:, :, t * 128:(t + 1) * 128].rearrange("k p t -> p k t"), fg[:])
            f3 = fT32[ti % 2]
            for half in range(2):
                pb = psb[1 + half]
                for kk in range(4):
                    k = half * 4 + kk
                    fw.tr(pb[:, kk * 128:(kk + 1) * 128], ff[:, k * 128:(k + 1) * 128], identf[:])
                fw.copy(f3[:, half * 4:(half + 1) * 4, :], pb[:].rearrange("p (k t) -> p k t", k=4),
                        q=("act" if half else "dve"))
            pl = psb[3]
            for k in range(8):
                fw.mm(pl[:, 0:16], f3[:, k, :], wr[:, k, :], start=(k == 0), stop=(k == 7))
            s_ = sc[ti % 2]
            fw.act(s_[:], pl[:, 0:16], AF.Sigmoid)
            biv = bi[:].rearrange("p a b -> p (a b)")
            fw.tt(biv, s_[:], brb[:], ALU.add)
            fw.reduce(m1[:], bi[:], ALU.max)
            fw.tt(eq[:], bi[:], m1[:].unsqueeze(2).to_broadcast([128, 4, 4]), ALU.is_equal)
            fw.stt(tb[:], eq[:], -1.0e9, bi[:], ALU.mult, ALU.add)
            fw.reduce(m2[:], tb[:], ALU.max)
            fw.tt(gsum[:], m1[:], m2[:], ALU.add)
            fw.reduce(gmax[:], gsum[:], ALU.max)
            fw.ts(oh[:], gsum[:], gmax[:], None, op0=ALU.is_equal)
            fw.tt(eq[:], bi[:], m2[:].unsqueeze(2).to_broadcast([128, 4, 4]), ALU.is_ge)
            fw.tt(eq[:], eq[:], oh[:].unsqueeze(2).to_broadcast([128, 4, 4]), ALU.mult)
            w_ = wout_t[ti % 2]
            fw.tt(w_[:], eq[:].rearrange("p a b -> p (a b)"), s_[:], ALU.mult)
            fw.reduce(den[:], w_[:], ALU.add)
            fw.recip(den[:], den[:])
            fw.ts(w_[:], w_[:], den[:], None, op0=ALU.mult)
            fw.dma(Wr[t * 128:(t + 1) * 128, :], w_[:])
            ti += 1


def phase_b2(fw, ntile, nfirst, sg_tiles, fT, Wr, h_new, cA, cB, wmod, bmod, weg, weu, wed, final_g, out, psb):
    gA = fw.sb("b2_gA", [128, 1024])
    gB = fw.sb("b2_gB", [128, 1024])
    phase_mod(fw, cA, wmod, bmod, 5120, 6144, gA, psb[7])
    phase_mod(fw, cB, wmod, bmod, 5120, 6144, gB, psb[7])
    fgb = None
    if final_g is not None:
        fgb = fw.sb("b2_fg", [128, 1024])
        fw.dma(fgb[:], final_g.partition_broadcast(128))
    SGT = sg_tiles
    fTs = fw.sb("b2_fT", [128, 8, SGT * 128], BF16)
    acc = fw.sb("b2_acc", [128, SGT, 1024])
    wrs = fw.sb("b2_wr", [128, SGT, 16])
    wgu = [fw.sb("b2_wgu%d" % i, [128, 8, 1024], BF16) for i in range(2)]
    wd = [fw.sb("b2_wd%d" % i, [128, 4, 1024], BF16) for i in range(2)]
    hid = [fw.sb("b2_hid%d" % i, [128, 4, 512], BF16) for i in range(2)]
    sgt = [fw.sb("b2_sg%d" % i, [128, 512]) for i in range(2)]
    hn = [fw.sb("b2_hn%d" % i, [128, 1024]) for i in range(2)]
    ot = [fw.sb("b2_ot%d" % i, [128, 1024]) for i in range(2)]
    ss = fw.sb("b2_ss", [128, 1])
    rs = fw.sb("b2_rs", [128, 1])
    wi = 0
    ev = 0
    for s0 in range(0, ntile, SGT):
        sn = min(SGT, ntile - s0)
        ntok = sn * 128
        for k in range(8):
            fw.dma(fTs[:, k, :ntok], fT[k, :, s0 * 128: s0 * 128 + ntok])
        fw.dma(wrs[:, :sn, :], Wr[s0 * 128: s0 * 128 + ntok, :].rearrange("(t p) e -> p t e", p=128))
        for e in range(16):
            wg_, wd_ = wgu[wi % 2], wd[wi % 2]
            wi += 1
            gv = weg[e].rearrange("(k p) n -> p k n", p=128)
            uv = weu[e].rearrange("(k p) n -> p k n", p=128)
            dv = wed[e].rearrange("(k p) n -> p k n", p=128)
            if not (DBG.get("b2_nodma") and wi > 2):
                for k in range(8):
                    fw.dma(wg_[:, k, 0:512], gv[:, k, :], q="pool")
                    fw.dma(wg_[:, k, 512:1024], uv[:, k, :], q="pool")
                for k in range(4):
                    fw.dma(wd_[:, k, :], dv[:, k, :], q="pool")
            for g0 in range(0, sn, 4):
                gn = min(4, sn - g0)
                gt = gn * 128
                c0 = g0 * 128
                hd = hid[ev % 2]
                for mt in range(4):
                    psg = psb[1 + (ev % 2)]
                    psu = psb[3 + (ev % 2)]
                    for k in range(8):
                        fw.mm(psg[:, :gt], wg_[:, k, mt * 128:(mt + 1) * 128], fTs[:, k, c0:c0 + gt],
                              start=(k == 0), stop=(k == 7))
                    for k in range(8):
                        fw.mm(psu[:, :gt], wg_[:, k, 512 + mt * 128: 512 + (mt + 1) * 128], fTs[:, k, c0:c0 + gt],
                              start=(k == 0), stop=(k == 7))
                    sg_ = sgt[ev % 2]
                    fw.act(sg_[:, :gt], psg[:, :gt], AF.Silu)
                    fw.tt(hd[:, mt, :gt], psu[:, :gt], sg_[:, :gt], ALU.mult)
                    ev += 1
                for tt_ in range(gn):
                    t = g0 + tt_
                    for cc in range(2):
                        ps = psb[5 + (ev % 2)]
                        ev += 1
                        for mt in range(4):
                            fw.mm(ps[:, :512], hd[:, mt, tt_ * 128:(tt_ + 1) * 128], wd_[:, mt, cc * 512:(cc + 1) * 512],
                                  start=(mt == 0), stop=(mt == 3))
                        a_ = acc[:, t, cc * 512:(cc + 1) * 512]
                        if e == 0:
                            fw.ts(a_, ps[:, :512], wrs[:, t, e:e + 1], None, op0=ALU.mult)
                        else:
                            fw.stt(a_, ps[:, :512], wrs[:, t, e:e + 1], a_, ALU.mult, ALU.add)
        for tt_ in range(sn):
            t = s0 + tt_
            h_ = hn[tt_ % 2]
            o_ = ot[tt_ % 2]
            fw.dma(h_[:], h_new[t * 128:(t + 1) * 128, :])
            gb = gA if t < nfirst else gB
            fw.tt(o_[:], acc[:, tt_, :], gb[:], ALU.mult, q="pool")
            fw.tt(o_[:], o_[:], h_[:], ALU.add, q="pool")
            if fgb is not None:
                fw.act(h_[:], o_[:], AF.Square, accum_out=ss[:])
                fw.act(rs[:], ss[:], AF.Sqrt, bias=NORM_EPS, scale=1.0 / D_MODEL)
                fw.recip(rs[:], rs[:])
                fw.stt(o_[:], o_[:], rs[:], fgb[:], ALU.mult, ALU.mult)
            fw.dma(out[t * 128:(t + 1) * 128, :], o_[:], is_output=True)


def blk_size(cfg):
    return 256 if cfg.TC % 256 == 0 else 128


def blk_src(cfg, d, i):
    B = blk_size(cfg)
    if d == 0:
        return i * B, (i + 1) * B, False
    nbc = cfg.TC // B
    nbl = cfg.TL // B
    if i < nbc:
        j = nbc - 1 - i
    else:
        j = nbc + (nbl - 1 - (i - nbc))
    return j * B, (j + 1) * B, True


def rows_ap(dram, t0, t1, c0, c1, rev, chunk=64):
    W = dram.shape[1]
    n = (t1 - t0) // chunk
    base = dram[t0:t1, c0:c1]
    if not rev:
        return base.rearrange("(c p) w -> p c w", p=chunk)
    off = int(dram.offset) + (t1 - 1) * W + c0
    return bass.AP(dram.tensor, off, [[-W, chunk], [-chunk * W, n], [1, c1 - c0]])


def frev(ap2d, rev):
    if not rev:
        return ap2d
    (ps, pn), (fs, fn) = ap2d.ap
    return bass.AP(ap2d.tensor, int(ap2d.offset) + (fn - 1) * fs, [[ps, pn], [-fs, fn]])


def tok_reverse(fw, dst, src, J, ps, nch, width, q="act"):
    for c in range(nch):
        fw.mm(ps[0:64, c * width:(c + 1) * width], J[:], src[:, nch - 1 - c, :])
    fw.copy(dst[:], ps[0:64, :nch * width].rearrange("p (c w) -> p c w", w=width), q=q)

def phase_gla(fw, cfg, p_tok, p_T, wg2, bg2neg_col, gla_g, maskU, cmask, identf, Jrev, o_scr, y_gla, psb):
    T, B = cfg.T, blk_size(cfg)
    NCH = B // 64
    nblk = T // B
    mU = fw.sb("gl_mU", [64, 64])
    fw.dma(mU[:], maskU)
    cm = fw.sb("gl_cm", [64, B])
    fw.dma(cm[:], cmask)
    w2 = fw.sb("gl_w2", [16, 2, 64])
    fw.dma(w2[:], wg2.rearrange("d r c -> r d c"))
    nb = fw.sb("gl_nb", [64, 2])
    for d in range(2):
        fw.dma(nb[:, d:d + 1], bg2neg_col[d])
    fw.ts(nb[:], nb[:], -1.0, None, op0=ALU.mult)
    S = [fw.sb("gl_S%d" % d, [64, 128]) for d in range(2)]
    for d in range(2):
        fw.memset(S[d][:], 0.0)
    two = range(2)
    qs = [fw.sb("gl_qs%d" % i, [64, B]) for i in two]
    ks = [fw.sb("gl_ks%d" % i, [64, B]) for i in two]
    ls = [fw.sb("gl_ls%d" % i, [16, B]) for i in two]
    vt = [fw.sb("gl_v%d" % i, [64, NCH, 128]) for i in two]
    vstg2 = [fw.sb("gl_vstg%d" % i_, [64, NCH, 128]) for i_ in range(2)]
    ostg2 = [fw.sb("gl_ostg%d" % i_, [64, NCH, 128]) for i_ in range(2)]
    lrev2 = [fw.sb("gl_lrev%d" % i_, [16, B]) for i_ in range(2)]
    la2 = [fw.sb("gl_la%d" % i_, [64, B]) for i_ in range(2)]
    bc2 = [fw.sb("gl_bc%d" % i_, [64, B]) for i_ in range(2)]
    eb = [fw.sb("gl_eb%d" % i, [64, B]) for i in two]
    en2 = [fw.sb("gl_en%d" % i_, [64, B]) for i_ in range(2)]
    qg = [fw.sb("gl_qg%d" % i, [64, B]) for i in two]
    kg2 = [fw.sb("gl_kg%d" % i_, [64, B]) for i_ in range(2)]
    kdT2 = [fw.sb("gl_kdT%d" % i_, [64, B]) for i_ in range(2)]
    kd = [fw.sb("gl_kd%d" % i, [64, NCH, 64]) for i in two]
    ai = [fw.sb("gl_ai%d" % i, [64, 2, NCH, 64]) for i in two]
    ob = [fw.sb("gl_ob%d" % i, [64, NCH, 128]) for i in two]
    def block(i, d):
        it = d
        if True:
            t0, t1, rev = blk_src(cfg, d, i)
            la = la2[d]
            bc = bc2[d]
            en = en2[d]
            kg = kg2[d]
            kdT = kdT2[d]
            vstg = vstg2[d]
            ostg = ostg2[d]
            lrev = lrev2[d]
            STOP = DBG.get('gla_stop', 99)
            q_, k_, l_, v_ = qs[it % 2], ks[it % 2], ls[it % 2], vt[it % 2]
            fw.dma(q_[:], p_T[T_GQK, 0:64, t0:t1])
            fw.dma(k_[:], p_T[T_GQK, 64:128, t0:t1])
            fw.dma(l_[:], p_T[T_MISC, 32 * d:32 * d + 16, t0:t1])
            if rev:
                fw.dma(vstg[:], rows_ap(p_tok, t0, t1, T_GV * 128, (T_GV + 1) * 128, False))
                tok_reverse(fw, v_, vstg, Jrev, psb[0], NCH, 128)
                fw.copy(lrev[:], frev(l_[:], True))
                lv = lrev[:]
            else:
                fw.dma(v_[:], rows_ap(p_tok, t0, t1, T_GV * 128, (T_GV + 1) * 128, False))
                lv = l_[:]
            qv, kv = frev(q_[:], rev), frev(k_[:], rev)
            yield
            pz = psb[1]
            fw.mm(pz[0:64, :B], w2[:, d, :], lv)
            fw.act(la[:], pz[0:64, :B], AF.Exp, bias=nb[:, d:d + 1], scale=-1.0)
            fw.act(la[:], la[:], AF.Ln, bias=1.0)
            fw.ts(la[:], la[:], -1.0 / 16.0, None, op0=ALU.mult)
            yield
            if STOP <= 1:
                return
            fw.scan(bc[:], cm[:], la[:], 0.0)
            e_ = eb[it % 2]
            fw.act(e_[:], bc[:], AF.Exp)
            fw.act(en[:], bc[:], AF.Exp, scale=-1.0)
            g_ = qg[it % 2]
            fw.stt(g_[:], qv, 32 ** -0.5, e_[:], ALU.mult, ALU.mult)
            fw.tt(kg[:], kv, en[:], ALU.mult, q="pool")
            bc3 = bc[:].rearrange("p (c t) -> p c t", t=64)
            fw.tt(kdT[:].rearrange("p (c t) -> p c t", t=64), bc3[:, :, 63:64].to_broadcast([64, NCH, 64]), bc3, ALU.subtract)
            fw.act(kdT[:], kdT[:], AF.Exp)
            fw.tt(kdT[:], kdT[:], kv, ALU.mult, q="pool")
            yield
            if STOP <= 2:
                return
            a_ = ai[it % 2]
            for h in range(2):
                pa = psb[2 + h]
                for c in range(NCH):
                    fw.mm(pa[0:64, c * 64:(c + 1) * 64], kg[32 * h:32 * h + 32, c * 64:(c + 1) * 64],
                          g_[32 * h:32 * h + 32, c * 64:(c + 1) * 64])
                fw.tt(a_[:, h, :, :], pa[0:64, :NCH * 64].rearrange("p (a t) -> p a t", t=64),
                      mU[:].unsqueeze(1).to_broadcast([64, NCH, 64]), ALU.mult, q=("dve" if h == 0 else "pool") if False else "dve")
            yield
            if STOP <= 3:
                return
            pk = psb[1]
            for c in range(NCH):
                fw.tr(pk[0:64, c * 64:(c + 1) * 64], kdT[:, c * 64:(c + 1) * 64], identf[0:64, 0:64])
            kd_ = kd[it % 2]
            fw.copy(kd_[:], pk[0:64, :NCH * 64].rearrange("p (c t) -> p c t", t=64), q="act")
            yield
            if STOP <= 4:
                return
            o_ = ob[it % 2]
            Sd = S[d]
            for c in range(NCH):
                po = psb[4 + (c % 2)]
                fw.mm(po[0:64, 0:128], g_[:, c * 64:(c + 1) * 64], Sd[:], start=True, stop=False)
                for h in range(2):
                    fw.mm(po[0:64, h * 64:(h + 1) * 64], a_[:, h, c, :], v_[:, c, h * 64:(h + 1) * 64],
                          start=False, stop=(h == 1))
                fw.copy(o_[:, c, :], po[0:64, 0:128], q="act")
                pss = psb[6 + (c % 2)]
                fw.mm(pss[0:64, 0:128], kd_[:, c, :], v_[:, c, :])
                for h in range(2):
                    blk = Sd[32 * h:32 * h + 32, 64 * h:64 * h + 64]
                    fw.stt(blk, blk, e_[32 * h:32 * h + 32, c * 64 + 63:c * 64 + 64], pss[32 * h:32 * h + 32, 64 * h:64 * h + 64],
                           ALU.mult, ALU.add)
                yield
            yield
            if rev:
                tok_reverse(fw, ostg, o_, Jrev, psb[0], NCH, 128)
                fw.dma(rows_ap(o_scr[d], t0, t1, 0, 128, False), ostg[:])
            else:
                fw.dma(rows_ap(o_scr[d], t0, t1, 0, 128, False), o_[:])

    for i in range(nblk):
        alive = [block(i, d) for d in DBG.get('gla_dirs', (0, 1))]
        while alive:
            for g_ in list(alive):
                try:
                    next(g_)
                except StopIteration:
                    alive.remove(g_)


def phase_gla_fin(fw, cfg, p_tok, o_scr, gla_g, identf, y_gla, psb, ctile=T_GR):
    T = cfg.T
    gb = fw.sb("gf_g", [128, 64])
    fw.dma(gb[:], gla_g.partition_broadcast(128))
    two = range(2)
    o0 = [fw.sb("gf_o0%d" % i, [128, 128]) for i in two]
    o1 = [fw.sb("gf_o1%d" % i, [128, 128]) for i in two]
    r_ = [fw.sb("gf_r%d" % i, [128, 128]) for i in two]
    sq = fw.sb("gf_sq", [128, 128])
    ss = fw.sb("gf_ss", [128, 2])
    yt = [fw.sb("gf_y%d" % i, [128, 128]) for i in two]
    yo = [fw.sb("gf_yo%d" % i, [128, 128]) for i in two]
    for t in range(T // 128):
        a, b, r, y = o0[t % 2], o1[t % 2], r_[t % 2], yt[t % 2]
        fw.dma(a[:], o_scr[0, t * 128:(t + 1) * 128, :])
        fw.dma(b[:], o_scr[1, t * 128:(t + 1) * 128, :])
        fw.dma(r[:], p_tok[t * 128:(t + 1) * 128, ctile * 128:(ctile + 1) * 128])
        fw.tt(a[:], a[:], b[:], ALU.add)
        fw.tt(sq[:], a[:], a[:], ALU.mult, q="pool")
        fw.reduce(ss[:], sq[:].rearrange("p (h d) -> p h d", h=2), ALU.add)
        fw.act(ss[:], ss[:], AF.Sqrt, bias=NORM_EPS, scale=1.0 / 64)
        fw.recip(ss[:], ss[:])
        fw.act(b[:], r[:], AF.Silu)
        for h in range(2):
            fw.stt(y[:, h * 64:(h + 1) * 64], a[:, h * 64:(h + 1) * 64], ss[:, h:h + 1], gb[:], ALU.mult, ALU.mult)
        fw.tt(y[:], y[:], b[:], ALU.mult, q="pool")
        pt = psb[1 + (t % 2)]
        fw.tr(pt[:, 0:128], y[:], identf[:])
        o = yo[t % 2]
        fw.copy(o[:], pt[:, 0:128], q="act")
        fw.dma(y_gla[:, t * 128:(t + 1) * 128], o[:])


def phase_dn_prep(fw, cfg, p_T, convw, alog_col, dtb_col, ones_bd, dn_qkv, dn_gates, psb):
    T, TC = cfg.T, cfg.TC
    cw = fw.sb("dp_cw", [128, 3, 5])
    fw.dma(cw[:], convw.rearrange("a p k -> p a k"))
    segs = []
    for (a, b) in ((0, TC), (TC, T)):
        s = a
        while s < b:
            e = min(s + 512, b)
            segs.append((s, e, a, b))
            s = e
    two = range(2)
    xp = [fw.sb("dp_xp%d" % i, [128, 516]) for i in two]
    acc = [fw.sb("dp_acc%d" % i, [128, 512]) for i in two]
    sq = fw.sb("dp_sq", [128, 512])
    rn = fw.sb("dp_rn", [128, 512])
    xo = [fw.sb("dp_xo%d" % i, [128, 512]) for i in two]
    it = 0
    for (s, e, a, b) in segs:
        n = e - s
        for ti, tile_id in enumerate((T_DNQ, T_DNK, T_DNV)):
            x = xp[it % 2]
            lo, hi = max(s - 2, a), min(e + 2, b)
            if lo > s - 2:
                fw.memset(x[:, 0:2], 0.0, q="pool")
            if hi < e + 2:
                fw.memset(x[:, n + 2:n + 4], 0.0, q="pool")
            fw.dma(x[:, lo - (s - 2): hi - (s - 2)], p_T[tile_id, :, lo:hi])
            ac = acc[it % 2]
            fw.ts(ac[:, :n], x[:, 0:n], cw[:, ti, 0:1], None, op0=ALU.mult)
            for k in range(1, 5):
                fw.stt(ac[:, :n], x[:, k:k + n], cw[:, ti, k:k + 1], ac[:, :n], ALU.mult, ALU.add)
            o = xo[it % 2]
            if ti < 2:
                fw.act(ac[:, :n], ac[:, :n], AF.Silu)
                fw.tt(sq[:, :n], ac[:, :n], ac[:, :n], ALU.mult, q="pool")
                ps = psb[1 + (it % 2)]
                fw.mm(ps[:, :n], ones_bd[:], sq[:, :n])
                fw.act(rn[:, :n], ps[:, :n], AF.Sqrt, bias=1e-6)
                fw.recip(rn[:, :n], rn[:, :n])
                fw.tt(o[:, :n], ac[:, :n], rn[:, :n], ALU.mult)
            else:
                fw.act(o[:, :n], ac[:, :n], AF.Silu)
            fw.dma(dn_qkv[ti, :, s:e], o[:, :n])
            it += 1
    ga = fw.sb("dp_ga", [4, T])
    gb = fw.sb("dp_gb", [4, T])
    al = fw.sb("dp_al", [4, 1])
    db = fw.sb("dp_db", [4, 1])
    fw.dma(al[:], alog_col)
    fw.dma(db[:], dtb_col)
    fw.act(al[:], al[:], AF.Exp)
    fw.ts(al[:], al[:], -1.0, None, op0=ALU.mult)
    fw.dma(ga[:], p_T[T_MISC, 64:68, :])
    fw.dma(gb[:], p_T[T_MISC, 68:72, :])
    fw.act(ga[:], ga[:], AF.Exp, bias=db[:])
    fw.act(ga[:], ga[:], AF.Ln, bias=1.0)
    fw.ts(ga[:], ga[:], al[:], None, op0=ALU.mult)
    fw.act(gb[:], gb[:], AF.Sigmoid)
    fw.dma(dn_gates[0], ga[:])
    fw.dma(dn_gates[1], gb[:])


def phase_dn(fw, cfg, dn_qkv, dn_gates, cmask2, Eh, maskLs, maskUs, maskUi, identf, Jrev, o_scr, psb):
    T, B = cfg.T, blk_size(cfg)
    NCH = B // 64
    NM = 2 * NCH
    nblk = T // B
    W = NM * 64
    sc = 64 ** -0.5
    cm = fw.sb("dn_cm", [2, B])
    fw.dma(cm[:], cmask2)
    eh = fw.sb("dn_eh", [2, 2, 64])
    fw.dma(eh[:], Eh.rearrange("h k m -> k h m"))
    mLs = fw.sb("dn_mLs", [64, 64]); fw.dma(mLs[:], maskLs)
    mUs = fw.sb("dn_mUs", [64, 64]); fw.dma(mUs[:], maskUs)
    mUi = fw.sb("dn_mUi", [64, 64]); fw.dma(mUi[:], maskUi)
    S = [fw.sb("dn_S%d" % d, [64, 2, 64]) for d in range(2)]
    for d in range(2):
        fw.memset(S[d][:], 0.0)
    mk = lambda n, shape: fw.sb(n, shape)
    ld = [[mk("dn_ld%d%d" % (i, j), [64, B]) for j in range(6)] for i in range(2)]
    qk2 = [[mk("dn_in%d%d" % (i, j), [64, B]) for j in range(6)] for i in range(2)]
    gl = [mk("dn_gl%d" % i, [2, 2, B]) for i in range(2)]
    g22 = [mk("dn_g2%d" % i, [2, B]) for i in range(2)]; ng22 = [mk("dn_ng2%d" % i, [2, B]) for i in range(2)]; be2 = [mk("dn_be%d" % i, [2, B]) for i in range(2)]
    eg2 = [mk("dn_eg%d" % i, [2, B]) for i in range(2)]; beg2 = [mk("dn_beg%d" % i, [2, B]) for i in range(2)]; ekd2 = [mk("dn_ekd%d" % i, [2, B]) for i in range(2)]
    ebc2 = [mk("dn_ebc%d" % i, [64, 2, B]) for i in range(2)]
    KbT2 = [mk("dn_KbT%d" % i, [64, 2, B]) for i in range(2)]; KbegT2 = [mk("dn_KbegT%d" % i, [64, 2, B]) for i in range(2)]; VbT2 = [mk("dn_VbT%d" % i, [64, 2, B]) for i in range(2)]
    KdT2 = [mk("dn_KdT%d" % i, [64, 2, B]) for i in range(2)]; QdT = [mk("dn_QdT%d" % i, [64, 2, B]) for i in range(2)]; QsT2 = [mk("dn_QsT%d" % i, [64, 2, B]) for i in range(2)]
    mG2 = [mk("dn_mG%d" % i, [64, W]) for i in range(2)]; mGT2 = [mk("dn_mGT%d" % i, [64, W]) for i in range(2)]
    t12 = [mk("dn_t1%d" % i, [64, W]) for i in range(2)]; t22 = [mk("dn_t2%d" % i, [64, W]) for i in range(2)]; t32 = [mk("dn_t3%d" % i, [64, W]) for i in range(2)]
    mkb = lambda n, shape: fw.sb(n, shape, BF16)
    Bp2 = [[mkb("dn_Bp%d%d" % (i, j), [64, W]) for j in range(2)] for i in range(2)]
    Np2 = [[mkb("dn_Np%d%d" % (i, j), [64, W]) for j in range(2)] for i in range(2)]
    Pb2 = [mkb("dn_Pb%d" % i, [64, W]) for i in range(2)]
    P2 = [mk("dn_P%d" % i, [64, W]) for i in range(2)]
    AiT = [mk("dn_AiT%d" % i, [64, W]) for i in range(2)]
    Rw2 = [mkb("dn_Rw%d" % i, [64, W]) for i in range(2)]; Rv2 = [mkb("dn_Rv%d" % i, [64, W]) for i in range(2)]
    Kd = [mk("dn_Kd%d" % i, [64, W]) for i in range(2)]
    U = [mk("dn_U%d" % i, [64, W]) for i in range(2)]
    WT = [mk("dn_WT%d" % i, [64, W]) for i in range(2)]
    vn2 = [[mk("dn_vn%d%d" % (i, j), [64, 2, 64]) for j in range(2)] for i in range(2)]
    ob = [mk("dn_ob%d" % i, [64, NCH, 128]) for i in range(2)]
    ostg2 = [mk("dn_ostg%d" % i, [64, NCH, 128]) for i in range(2)]
    v3 = lambda t: t[:].rearrange("p (m t) -> p m t", t=64)
    psb_rot = [psb[0]] + [psb[((k_ - 1 + 3) % 7) + 1] for k_ in range(1, 8)]

    def block(i, d):
        it = d
        PSB = psb if d == 0 else psb_rot
        if True:
            t0, t1_, rev = blk_src(cfg, d, i)
            par = it % 2
            qk, Bp, Np, vn, Pb = qk2[par], Bp2[par], Np2[par], vn2[par], Pb2[par]
            g2 = g22[par]
            ng2 = ng22[par]
            be = be2[par]
            eg = eg2[par]
            beg = beg2[par]
            ekd = ekd2[par]
            ebc = ebc2[par]
            KbT = KbT2[par]
            KbegT = KbegT2[par]
            VbT = VbT2[par]
            KdT = KdT2[par]
            QsT = QsT2[par]
            mG = mG2[par]
            mGT = mGT2[par]
            t1 = t12[par]
            t2 = t22[par]
            t3 = t32[par]
            P = P2[par]
            Rw = Rw2[par]
            Rv = Rv2[par]
            ostg = ostg2[par]
            L = ld[it % 2]
            for a in range(3):
                for h in range(2):
                    fw.dma(L[a * 2 + h][:], dn_qkv[a, h * 64:(h + 1) * 64, t0:t1_], q=("sp" if h == 0 else "pool"))
            G = gl[it % 2]
            fw.dma(G[:], dn_gates[:, 2 * d:2 * d + 2, t0:t1_].rearrange("a h t -> h a t"))
            if rev:
                for j in range(6):
                    fw.copy(qk[j][:], frev(L[j][:], True), q=("pool" if j % 2 else "dve"))
                X = qk
            else:
                X = L
            q_, k_, v_ = (X[0], X[1]), (X[2], X[3]), (X[4], X[5])
            yield
            fw.scan(g2[:], cm[:], frev(G[:, 0, :], rev), 0.0)
            fw.ts(ng2[:], g2[:], -1.0, None, op0=ALU.mult)
            fw.copy(be[:], frev(G[:, 1, :], rev))
            fw.act(eg[:], g2[:], AF.Exp)
            fw.tt(beg[:], eg[:], be[:], ALU.mult)
            g3 = g2[:].rearrange("p (c t) -> p c t", t=64)
            fw.tt(ekd[:].rearrange("p (c t) -> p c t", t=64), g3[:, :, 63:64].to_broadcast([2, NCH, 64]), g3, ALU.subtract)
            fw.act(ekd[:], ekd[:], AF.Exp)
            yield
            for h in range(2):
                pb = PSB[1]
                fw.mm(pb[0:64, 0:B], eh[:, h, :], eg[:])
                fw.copy(ebc[:, h, :], pb[0:64, 0:B], q="act")
                pb2 = PSB[2]
                fw.mm(pb2[0:64, 0:B], eh[:, h, :], be[:])
                fw.tt(KbT[:, h, :], k_[h][:], pb2[0:64, 0:B], ALU.mult)
                fw.tt(VbT[:, h, :], v_[h][:], pb2[0:64, 0:B], ALU.mult)
                pb3 = PSB[3]
                fw.mm(pb3[0:64, 0:B], eh[:, h, :], beg[:])
                fw.tt(KbegT[:, h, :], k_[h][:], pb3[0:64, 0:B], ALU.mult)
                pb4 = PSB[4]
                fw.mm(pb4[0:64, 0:B], eh[:, h, :], ekd[:])
                fw.tt(KdT[:, h, :], k_[h][:], pb4[0:64, 0:B], ALU.mult)
                fw.stt(QdT[it % 2][:, h, :], q_[h][:], sc, ebc[:, h, :], ALU.mult, ALU.mult)
                fw.ts(QsT[:, h, :], q_[h][:], sc, None, op0=ALU.mult, q="pool")
            Qd = QdT[it % 2]
            yield
            pG, pGT, p1, p2, p3 = PSB[5], PSB[6], PSB[7], PSB[1], PSB[2]
            for h in range(2):
                for c in range(NCH):
                    m = h * NCH + c
                    cs = slice(c * 64, (c + 1) * 64)
                    ms = slice(m * 64, (m + 1) * 64)
                    fw.mm(pG[0:64, ms], g2[:, cs], eh[:, h, :], start=True, stop=False)
                    fw.mm(pG[0:64, ms], eh[:, h, :], ng2[:, cs], start=False, stop=True)
                    fw.mm(pGT[0:64, ms], eh[:, h, :], g2[:, cs], start=True, stop=False)
                    fw.mm(pGT[0:64, ms], ng2[:, cs], eh[:, h, :], start=False, stop=True)
                    fw.mm(p1[0:64, ms], k_[h][:, cs], KbT[:, h, cs])
                    fw.mm(p2[0:64, ms], KbT[:, h, cs], k_[h][:, cs])
                    fw.mm(p3[0:64, ms], k_[h][:, cs], QsT[:, h, cs])
            fw.ts(mG[:], pG[0:64, :W], 0.0, None, op0=ALU.min)
            fw.act(mG[:], mG[:], AF.Exp)
            fw.ts(mGT[:], pGT[0:64, :W], 0.0, None, op0=ALU.min)
            fw.act(mGT[:], mGT[:], AF.Exp)
            bcm = lambda mt: mt[:].unsqueeze(1).to_broadcast([64, NM, 64])
            fw.tt(t1[:], p1[0:64, :W], mGT[:], ALU.mult)
            Bc, Nc = Bp[0], Np[0]
            fw.stt(v3(Bc), v3(t1), -1.0, bcm(mUs), ALU.mult, ALU.mult)
            fw.tt(t2[:], p2[0:64, :W], mG[:], ALU.mult)
            fw.stt(v3(Nc), v3(t2), -1.0, bcm(mLs), ALU.mult, ALU.mult)
            fw.tt(t3[:], p3[0:64, :W], mGT[:], ALU.mult)
            Ai = AiT[it % 2]
            fw.tt(v3(Ai), v3(t3), bcm(mUi), ALU.mult, q="pool")
            yield
            fw.tt(v3(P), v3(Bc), identf[0:64, 0:64].unsqueeze(1).to_broadcast([64, NM, 64]), ALU.add)
            fw.copy(Pb[:], P[:], q="act")
            cur = 0
            for lvl in range(5):
                Bn, Nn = Bp[1 - cur], Np[1 - cur]
                pa, pb_, pc = PSB[3], PSB[4], PSB[5]
                for m in range(NM):
                    ms = slice(m * 64, (m + 1) * 64)
                    fw.mm(pa[0:64, ms], Np[cur][:, ms], Bp[cur][:, ms])
                    fw.mm(pb_[0:64, ms], Bp[cur][:, ms], Np[cur][:, ms])
                fw.copy(Bn[:], pa[0:64, :W], q="act")
                fw.copy(Nn[:], pb_[0:64, :W], q="dve")
                for m in range(NM):
                    ms = slice(m * 64, (m + 1) * 64)
                    fw.mm(pc[0:64, ms], Nn[:, ms], Pb[:, ms])
                fw.tt(P[:], P[:], pc[0:64, :W], ALU.add)
                fw.copy(Pb[:], P[:], q="act")
                cur = 1 - cur
                yield
            yield
            pr, pv, pk = PSB[6], PSB[7], PSB[1]
            for h in range(2):
                for c in range(NCH):
                    m = h * NCH + c
                    cs = slice(c * 64, (c + 1) * 64)
                    ms = slice(m * 64, (m + 1) * 64)
                    fw.tr(pr[0:64, ms], KbegT[:, h, cs], identf[0:64, 0:64])
                    fw.tr(pv[0:64, ms], VbT[:, h, cs], identf[0:64, 0:64])
                    fw.tr(pk[0:64, ms], KdT[:, h, cs], identf[0:64, 0:64])
            fw.copy(Rw[:], pr[0:64, :W], q="act")
            fw.copy(Rv[:], pv[0:64, :W], q="dve")
            Kd_ = Kd[it % 2]
            fw.copy(Kd_[:], pk[0:64, :W], q="act")
            yield
            pu, pw = PSB[2], PSB[3]
            for m in range(NM):
                ms = slice(m * 64, (m + 1) * 64)
                fw.mm(pu[0:64, ms], Pb[:, ms], Rv[:, ms])
                fw.mm(pw[0:64, ms], Rw[:, ms], Pb[:, ms])
            U_, WT_ = U[it % 2], WT[it % 2]
            fw.copy(U_[:], pu[0:64, :W], q="dve")
            fw.copy(WT_[:], pw[0:64, :W], q="act")
            yield
            Sd = S[d]
            o_ = ob[it % 2]
            for c in range(NCH):
                cs = slice(c * 64, (c + 1) * 64)
                ps1, ps2, ps3 = PSB[4], PSB[5 + (c % 2)], PSB[7]
                for h in range(2):
                    m = h * NCH + c
                    fw.mm(ps1[0:64, h * 64:(h + 1) * 64], WT_[:, m * 64:(m + 1) * 64], Sd[:, h, :])
                vn_ = vn[c % 2]
                for h in range(2):
                    m = h * NCH + c
                    fw.tt(vn_[:, h, :], U_[:, m * 64:(m + 1) * 64], ps1[0:64, h * 64:(h + 1) * 64], ALU.subtract)
                yield
                for h in range(2):
                    m = h * NCH + c
                    fw.mm(ps2[0:64, h * 64:(h + 1) * 64], Qd[:, h, cs], Sd[:, h, :], start=True, stop=False)
                    fw.mm(ps2[0:64, h * 64:(h + 1) * 64], Ai[:, m * 64:(m + 1) * 64], vn_[:, h, :], start=False, stop=True)
                fw.copy(o_[:, c, :], ps2[0:64, 0:128], q="act")
                yield
                for h in range(2):
                    m = h * NCH + c
                    fw.mm(ps3[0:64, h * 64:(h + 1) * 64], Kd_[:, m * 64:(m + 1) * 64], vn_[:, h, :])
                for h in range(2):
                    fw.stt(Sd[:, h, :], Sd[:, h, :], ebc[:, h, c * 64 + 63:c * 64 + 64], ps3[0:64, h * 64:(h + 1) * 64],
                           ALU.mult, ALU.add)
            yield
            if rev:
                tok_reverse(fw, ostg, o_, Jrev, PSB[0], NCH, 128)
                fw.dma(rows_ap(o_scr[d], t0, t1_, 0, 128, False), ostg[:])
            else:
                fw.dma(rows_ap(o_scr[d], t0, t1_, 0, 128, False), o_[:])

    for i in range(nblk):
        alive = [block(i, d) for d in DBG.get("dn_dirs", (0, 1))]
        while alive:
            for g_ in list(alive):
                try:
                    next(g_)
                except StopIteration:
                    alive.remove(g_)


def phase_s5(fw, cfg, p_T, lam_col, logdt_col, b_sm, c_nat, svals, mask4, mask4T, identf, ys5, psb):
    T, B = cfg.T, blk_size(cfg)
    nblk = T // B
    TWO_PI = 2.0 * np.pi
    sv = fw.sb("s5_sv", [128, B + 1])
    fw.dma(sv[:], svals)
    m4 = fw.sb("s5_m4", [128, 4, 128]); fw.dma(m4[:], mask4.rearrange("s p q -> p s q"))
    m4T = fw.sb("s5_m4T", [128, 4, 128]); fw.dma(m4T[:], mask4T.rearrange("s p q -> p s q"))
    bsm = fw.sb("s5_bsm", [128, 2, 4, 16])
    for r in range(2):
        fw.dma(bsm[:, r, :, :], b_sm[r])
    rho, cosT, sinT, BbT, CT = [], [], [], [], []
    for d in range(2):
        rho.append(fw.sb("s5_rho%d" % d, [128, 4]))
        cosT.append(fw.sb("s5_cos%d" % d, [128, 4, B + 1]))
        sinT.append(fw.sb("s5_sin%d" % d, [128, 4, B + 1]))
        BbT.append(fw.sb("s5_BbT%d" % d, [128, 2, 4, 128]))
        CT.append(fw.sb("s5_CT%d" % d, [128, 2, 4, 128]))
    with fw_scope(fw):
        lam = fw.sb("s5_lam", [128, 2, 4]); ldt = fw.sb("s5_ldt", [128, 4]); th = fw.sb("s5_th", [128, 4])
        ang = fw.sb("s5_ang", [128, B + 1]); wi = fw.sb("s5_wi", [128, B + 1], I32); wf = fw.sb("s5_wf", [128, B + 1])
        fx = fw.sb("s5_fx", [128, B + 1])
        col = lambda n: fw.sb(n, [128, 4])
        abr, abi, den, fre, fim, tmpc = col("s5_abr"), col("s5_abi"), col("s5_den"), col("s5_fre"), col("s5_fim"), col("s5_tmpc")
        bb = fw.sb("s5_bb", [128, 2, 4, 16]); tb = fw.sb("s5_tb", [128, 4, 16])
        M = fw.sb("s5_M", [128, 128]); cn = fw.sb("s5_cn", [128, 2, 64])
        for d in range(2):
            for r in range(2):
                fw.dma(lam[:, r, :], lam_col[d, r])
            fw.dma(ldt[:], logdt_col[d])
            fw.act(ldt[:], ldt[:], AF.Exp)
            fw.tt(th[:], lam[:, 1, :], ldt[:], ALU.mult)
            fw.tt(rho[d][:], lam[:, 0, :], ldt[:], ALU.mult)
            fw.act(rho[d][:], rho[d][:], AF.Exp)
            for st in range(4):
                for (tab, shift) in ((sinT[d], 0.0), (cosT[d], 0.25)):
                    fw.ts(ang[:], sv[:], th[:, st:st + 1], 1.0 / TWO_PI, op0=ALU.mult, op1=ALU.mult)
                    if shift:
                        fw.ts(ang[:], ang[:], shift, None, op0=ALU.add)
                    fw.copy(wi[:], ang[:])
                    fw.copy(wf[:], wi[:])
                    fw.tt(ang[:], ang[:], wf[:], ALU.subtract)
                    fw.ts(fx[:], ang[:], 0.5, None, op0=ALU.is_gt)
                    fw.tt(ang[:], ang[:], fx[:], ALU.subtract)
                    fw.ts(fx[:], ang[:], -0.5, None, op0=ALU.is_lt)
                    fw.tt(ang[:], ang[:], fx[:], ALU.add)
                    fw.act(tab[:, st, :], ang[:], AF.Sin, scale=TWO_PI)
            fw.tt(abr[:], rho[d][:], cosT[d][:, :, 1], ALU.mult)
            fw.tt(abi[:], rho[d][:], sinT[d][:, :, 1], ALU.mult)
            fw.ts(abr[:], abr[:], -1.0, None, op0=ALU.add)
            fw.tt(den[:], lam[:, 0, :], lam[:, 0, :], ALU.mult)
            fw.tt(tmpc[:], lam[:, 1, :], lam[:, 1, :], ALU.mult)
            fw.tt(den[:], den[:], tmpc[:], ALU.add)
            fw.recip(den[:], den[:])
            fw.tt(fre[:], abr[:], lam[:, 0, :], ALU.mult)
            fw.tt(tmpc[:], abi[:], lam[:, 1, :], ALU.mult)
            fw.tt(fre[:], fre[:], tmpc[:], ALU.add)
            fw.tt(fre[:], fre[:], den[:], ALU.mult)
            fw.tt(fim[:], abi[:], lam[:, 0, :], ALU.mult)
            fw.tt(tmpc[:], abr[:], lam[:, 1, :], ALU.mult)
            fw.tt(fim[:], fim[:], tmpc[:], ALU.subtract)
            fw.tt(fim[:], fim[:], den[:], ALU.mult)
            frb = fre[:].unsqueeze(2).to_broadcast([128, 4, 16])
            fib = fim[:].unsqueeze(2).to_broadcast([128, 4, 16])
            fw.tt(bb[:, 0, :, :], bsm[:, 0, :, :], frb, ALU.mult)
            fw.tt(tb[:], bsm[:, 1, :, :], fib, ALU.mult)
            fw.tt(bb[:, 0, :, :], bb[:, 0, :, :], tb[:], ALU.subtract)
            fw.tt(bb[:, 1, :, :], bsm[:, 1, :, :], frb, ALU.mult)
            fw.tt(tb[:], bsm[:, 0, :, :], fib, ALU.mult)
            fw.tt(bb[:, 1, :, :], bb[:, 1, :, :], tb[:], ALU.add)
            for r in range(2):
                fw.dma(cn[:, r, :], c_nat[d, r])
            for st in range(4):
                for r in range(2):
                    fw.tt(M[:].rearrange("p (g c) -> p g c", g=8), bb[:, r, st, :].unsqueeze(1).to_broadcast([128, 8, 16]),
                          m4T[:, st, :].rearrange("p (g c) -> p g c", g=8), ALU.mult)
                    pt = psb[1 + r]
                    fw.tr(pt[:, 0:128], M[:], identf[:])
                    fw.copy(BbT[d][:, r, st, :], pt[:, 0:128], q="act")
                    fw.tt(M[:].rearrange("p (l q) -> p l q", l=2), cn[:, r, :].unsqueeze(1).to_broadcast([128, 2, 64]),
                          m4[:, st, :].rearrange("p (l q) -> p l q", l=2), ALU.mult)
                    pt2 = psb[3 + r]
                    fw.tr(pt2[:, 0:128], M[:], identf[:])
                    if r == 0:
                        fw.copy(CT[d][:, r, st, :], pt2[:, 0:128], q="act")
                    else:
                        fw.act(CT[d][:, r, st, :], pt2[:, 0:128], AF.Copy, scale=-1.0)
    init = [fw.sb("s5_init%d" % d, [128, 2, 4]) for d in range(2)]
    for d in range(2):
        fw.memset(init[d][:], 0.0)
    two = range(2)
    mk2 = lambda n, shape: [fw.sb("%s%d" % (n, i), shape) for i in two]
    ub, ur2 = mk2("s5_u", [128, B]), mk2("s5_ur", [128, B])
    bur, bui = mk2("s5_bur", [128, B]), mk2("s5_bui", [128, B])
    a1_, a2_, a3_, a4_ = mk2("s5_a1", [128, B]), mk2("s5_a2", [128, B]), mk2("s5_a3", [128, B]), mk2("s5_a4", [128, B])
    rre_, rim_ = mk2("s5_rre", [128, B]), mk2("s5_rim", [128, B])
    xtr, xti = mk2("s5_xtr", [128, B]), mk2("s5_xti", [128, B])
    xre, xim = mk2("s5_xre", [128, B]), mk2("s5_xim", [128, B])
    c1_, c2_ = mk2("s5_c1", [128, 1]), mk2("s5_c2", [128, 1])
    yb, yr_ = mk2("s5_yb", [128, B]), mk2("s5_yr", [128, B])

    def block(i, d):
        t0, t1, rev = blk_src(cfg, d, i)
        a1, a2, a3, a4, rre, rim, c1, c2 = a1_[d], a2_[d], a3_[d], a4_[d], rre_[d], rim_[d], c1_[d], c2_[d]
        pr_, pi_, py = psb[1 + 3 * d], psb[2 + 3 * d], psb[3 + 3 * d]
        u = ub[d]
        fw.dma(u[:], p_T[T_S5U, :, t0:t1])
        if rev:
            fw.copy(ur2[d][:], frev(u[:], True), q="pool")
            u = ur2[d]
        yield
        for st in range(4):
            fw.mm(pr_[:, :B], BbT[d][:, 0, st, :], u[:])
            fw.mm(pi_[:, :B], BbT[d][:, 1, st, :], u[:])
            br, bi_ = bur[d], bui[d]
            fw.copy(br[:], pr_[:, :B], q="act")
            fw.copy(bi_[:], pi_[:, :B], q="act")
            yield
            cs, sn = cosT[d][:, st, 0:B], sinT[d][:, st, 0:B]
            fw.tt(a1[:], br[:], cs, ALU.mult)
            fw.tt(a2[:], bi_[:], sn, ALU.mult, q="pool")
            fw.tt(a3[:], bi_[:], cs, ALU.mult, q="pool")
            fw.tt(a4[:], br[:], sn, ALU.mult)
            fw.tt(rre[:], a1[:], a2[:], ALU.add, q="pool")
            fw.tt(rim[:], a3[:], a4[:], ALU.subtract)
            yield
            xr_, xi_ = xtr[d], xti[d]
            rb = rho[d][:, st:st + 1].to_broadcast([128, B])
            fw.scan(xr_[:], rb, rre[:], init[d][:, 0, st:st + 1])
            fw.scan(xi_[:], rb, rim[:], init[d][:, 1, st:st + 1])
            er, ei = cosT[d][:, st, B:B + 1], sinT[d][:, st, B:B + 1]
            fw.tt(c1[:], xr_[:, B - 1:B], er, ALU.mult, q="pool")
            fw.tt(c2[:], xi_[:, B - 1:B], ei, ALU.mult, q="pool")
            fw.tt(init[d][:, 0, st:st + 1], c1[:], c2[:], ALU.subtract, q="pool")
            fw.tt(c1[:], xi_[:, B - 1:B], er, ALU.mult, q="pool")
            fw.tt(c2[:], xr_[:, B - 1:B], ei, ALU.mult, q="pool")
            fw.tt(init[d][:, 1, st:st + 1], c1[:], c2[:], ALU.add, q="pool")
            yield
            fw.tt(a1[:], xr_[:], cs, ALU.mult)
            fw.tt(a2[:], xi_[:], sn, ALU.mult, q="pool")
            fw.tt(a3[:], xi_[:], cs, ALU.mult, q="pool")
            fw.tt(a4[:], xr_[:], sn, ALU.mult)
            xr2, xi2 = xre[d], xim[d]
            fw.tt(xr2[:], a1[:], a2[:], ALU.subtract, q="pool")
            fw.tt(xi2[:], a3[:], a4[:], ALU.add)
            fw.mm(py[:, :B], CT[d][:, 0, st, :], xr2[:], start=(st == 0), stop=False)
            fw.mm(py[:, :B], CT[d][:, 1, st, :], xi2[:], start=False, stop=(st == 3))
            yield
        y = yb[d]
        fw.copy(y[:], py[:, :B], q="act")
        if rev:
            fw.copy(yr_[d][:], frev(y[:], True), q="pool")
            fw.dma(ys5[d, :, t0:t1], yr_[d][:])
        else:
            fw.dma(ys5[d, :, t0:t1], y[:])

    for i in range(nblk):
        alive = [block(i, d) for d in DBG.get("s5_dirs", (0, 1))]
        while alive:
            for g_ in list(alive):
                try:
                    next(g_)
                except StopIteration:
                    alive.remove(g_)


def phase_s5_fin(fw, cfg, p_T, ys5, dcol, y_s5):
    T = cfg.T
    dc = fw.sb("sf_d", [128, 1])
    fw.dma(dc[:], dcol)
    two = range(2)
    a = [fw.sb("sf_a%d" % i, [128, 512]) for i in two]
    b = [fw.sb("sf_b%d" % i, [128, 512]) for i in two]
    u = [fw.sb("sf_u%d" % i, [128, 512]) for i in two]
    i = 0
    for s in range(0, T, 512):
        e = min(s + 512, T)
        n = e - s
        aa, bb, uu = a[i % 2], b[i % 2], u[i % 2]
        fw.dma(aa[:, :n], ys5[0, :, s:e])
        fw.dma(bb[:, :n], ys5[1, :, s:e])
        fw.dma(uu[:, :n], p_T[T_S5U, :, s:e], q="pool")
        fw.tt(aa[:, :n], aa[:, :n], bb[:, :n], ALU.add)
        fw.stt(aa[:, :n], uu[:, :n], dc[:], aa[:, :n], ALU.mult, ALU.add)
        fw.dma(y_s5[:, s:e], aa[:, :n])
        i += 1


import ml_dtypes
from concourse.bass_utils import run_bass_kernel_spmd

DEPTH = 2
GRID_W = 64


def _col(v, n):
    return np.ascontiguousarray(np.asarray(v, np.float32).reshape(n, 128).T)


def _consts(cfg):
    B = blk_size(cfg)
    c = {}
    c["identb"] = np.eye(128).astype(ml_dtypes.bfloat16)
    c["identf"] = np.eye(128, dtype=np.float32)
    t = np.arange(cfg.TL)
    row = (t // GRID_W).astype(np.float32)
    colp = (t % GRID_W).astype(np.float32)
    inv = (10000.0 ** (-np.arange(8, dtype=np.float32) / 8)).astype(np.float32)
    ang = np.concatenate([row[:, None] * inv, colp[:, None] * inv], -1)
    c["rope"] = np.concatenate([np.cos(ang), np.sin(ang)], -1).astype(np.float32)
    sel = np.zeros((65, 64), np.float32)
    sel[64] = 1
    c["sel65"] = sel
    c["ones64"] = np.ones((64, 64), np.float32)
    c["maskU"] = np.triu(np.ones((64, 64), np.float32))
    cm = np.ones((64, B), np.float32)
    cm[:, ::64] = 0
    c["cmask"] = cm
    c["cmask2"] = np.ascontiguousarray(cm[:2])
    c["Jr"] = np.ascontiguousarray(np.eye(64, dtype=np.float32)[::-1])
    obd = np.zeros((128, 128), np.float32)
    obd[:64, :64] = 1
    obd[64:, 64:] = 1
    c["onesbd"] = obd
    Eh = np.zeros((2, 2, 64), np.float32)
    Eh[0, 0] = 1
    Eh[1, 1] = 1
    c["Eh"] = Eh
    ii, jj = np.meshgrid(np.arange(64), np.arange(64), indexing="ij")
    c["mLs"] = (ii > jj).astype(np.float32)
    c["mUs"] = (jj > ii).astype(np.float32)
    c["mUi"] = (jj >= ii).astype(np.float32)
    c["svals"] = np.tile(np.arange(B + 1, dtype=np.float32), (128, 1))
    m4 = np.zeros((4, 128, 128), np.float32)
    for st in range(4):
        for gl2 in range(2):
            g8 = 2 * st + gl2
            m4[st, g8 * 16:(g8 + 1) * 16, gl2 * 64:(gl2 + 1) * 64] = 1
    c["mask4"] = m4
    c["mask4T"] = np.ascontiguousarray(m4.transpose(0, 2, 1))
    return c


def _s5_layout(a_re, a_im, log_dt, b_re, b_im, c_re, c_im, j):
    gs = slice(8 * j, 8 * j + 8)

    def colz(x):
        return np.ascontiguousarray(x.reshape(4, 2, 64).transpose(1, 2, 0).reshape(128, 4))
    lam_col = np.stack([np.stack([colz(a_re[d, gs]), colz(a_im[d, gs])]) for d in range(2)])
    logdt_col = np.stack([colz(np.repeat(log_dt[d, gs][:, None], 64, 1)) for d in range(2)])

    def bsm(x):
        return np.ascontiguousarray(x.reshape(4, 2, 64, 16).transpose(1, 2, 0, 3).reshape(128, 4, 16))
    b_sm = np.stack([bsm(b_re[gs]), bsm(b_im[gs])])
    c_nat = np.stack([np.stack([c_re[d, gs].reshape(128, 64), c_im[d, gs].reshape(128, 64)]) for d in range(2)])
    f = lambda z: np.ascontiguousarray(z, dtype=np.float32)
    return f(lam_col), f(logdt_col), f(b_sm), f(c_nat)


_A_IN = [("h_ctx", None), ("h_lat", None), ("c_lat", [128, 8]), ("c_ctx", [128, 8]), ("wmod", [1024, 6144]), ("bmod", [6144]),
         ("g1", [1024]), ("w_my", [1024, NCOL]), ("lamb", [128]), ("da_g", [64, 1]), ("wg2", [2, 16, 64]), ("bg2", [2, 64, 1]),
         ("gla_g", [64]), ("convw", [3, 128, 5]), ("alog", [4, 1]), ("dtb", [4, 1]), ("dn_g", [64]),
         ("lam_col", [2, 2, 128, 4]), ("logdt_col", [2, 128, 4]), ("b_sm", [2, 128, 4, 16]), ("c_nat", [2, 2, 128, 64]),
         ("dcol", [128, 1])]


def build_A(cfg, lam_init, with_ctx):
    nc = bass.Bass("TRN2", target_bir_lowering=False)
    T, B = cfg.T, blk_size(cfg)
    dt = lambda n, s, d=F32, k="ExternalInput": nc.dram_tensor(n, list(s), d, kind=k).ap()
    a = {}
    for n, s in _A_IN:
        if n == "h_ctx":
            s = [cfg.TC, 1024]
        if n == "h_lat":
            s = [cfg.TL, 1024]
        a[n] = dt(n, s)
    cshape = dict(identb=([128, 128], BF16), identf=([128, 128], F32), rope=([cfg.TL, 32], F32), sel65=([65, 64], F32),
                  ones64=([64, 64], F32), maskU=([64, 64], F32), cmask=([64, B], F32), cmask2=([2, B], F32), Jr=([64, 64], F32),
                  onesbd=([128, 128], F32), Eh=([2, 2, 64], F32), mLs=([64, 64], F32), mUs=([64, 64], F32), mUi=([64, 64], F32),
                  svals=([128, B + 1], F32), mask4=([4, 128, 128], F32), mask4T=([4, 128, 128], F32))
    for n, (s, d) in cshape.items():
        a[n] = dt(n, s, d)
    yT = dt("yT", [4, 128, T], F32, "ExternalOutput")
    p_tok = dt("p_tok", [T, NCOL], F32, "Internal")
    p_T = dt("p_T", [12, 128, T], F32, "Internal")
    dn_qkv = dt("dn_qkv", [3, 128, T], F32, "Internal")
    dn_gates = dt("dn_gates", [2, 4, T], F32, "Internal")
    o_gla = dt("o_gla", [2, T, 128], F32, "Internal")
    o_dn = dt("o_dn", [2, T, 128], F32, "Internal")
    ys5 = dt("ys5", [2, 128, T], F32, "Internal")
    with contextlib.ExitStack() as st:
        fw = FW(nc, st)
        psb = [fw.ps("bank%d" % i, [128, 512]) for i in range(8)]
        idb = fw.sb("idb", [128, 128], BF16)
        fw.dma(idb[:], a["identb"])
        idf = fw.sb("idf", [128, 128])
        fw.dma(idf[:], a["identf"])
        with fw_scope(fw):
            modc = fw.sb("modc", [128, 2048])
            modl = fw.sb("modl", [128, 2048])
            phase_mod(fw, a["c_ctx"], a["wmod"], a["bmod"], 0, 2048, modc, psb[7])
            phase_mod(fw, a["c_lat"], a["wmod"], a["bmod"], 0, 2048, modl, psb[7])
            phase_a1(fw, cfg, a["h_ctx"], a["h_lat"], a["w_my"], a["g1"], modc, modl, idb, p_tok, p_T, psb)
        with fw_scope(fw):
            s65 = fw.sb("s65", [65, 64])
            fw.dma(s65[:], a["sel65"])
            o64 = fw.sb("o64", [64, 64])
            fw.dma(o64[:], a["ones64"])
            phase_da(fw, cfg, p_tok, a["rope"], a["lamb"], a["da_g"], lam_init, with_ctx, idb, s65, o64, yT[2], psb)
        with fw_scope(fw):
            Jsb = fw.sb("Jsb", [64, 64])
            fw.dma(Jsb[:], a["Jr"])
            phase_gla(fw, cfg, p_tok, p_T, a["wg2"], a["bg2"], a["gla_g"], a["maskU"], a["cmask"], idf, Jsb, o_gla, yT[3], psb)
        with fw_scope(fw):
            phase_gla_fin(fw, cfg, p_tok, o_gla, a["gla_g"], idf, yT[3], psb, ctile=T_GR)
        with fw_scope(fw):
            obd = fw.sb("obd", [128, 128])
            fw.dma(obd[:], a["onesbd"])
            phase_dn_prep(fw, cfg, p_T, a["convw"], a["alog"], a["dtb"], obd, dn_qkv, dn_gates, psb)
        with fw_scope(fw):
            Jsb = fw.sb("Jsb2", [64, 64])
            fw.dma(Jsb[:], a["Jr"])
            phase_dn(fw, cfg, dn_qkv, dn_gates, a["cmask2"], a["Eh"], a["mLs"], a["mUs"], a["mUi"], idf, Jsb, o_dn, psb)
        with fw_scope(fw):
            phase_gla_fin(fw, cfg, p_tok, o_dn, a["dn_g"], idf, yT[0], psb, ctile=T_DNZ)
        with fw_scope(fw):
            phase_s5(fw, cfg, p_T, a["lam_col"], a["logdt_col"], a["b_sm"], a["c_nat"], a["svals"], a["mask4"], a["mask4T"],
                     idf, ys5, psb)
        with fw_scope(fw):
            phase_s5_fin(fw, cfg, p_T, ys5, a["dcol"], yT[1])
        fw.finish()
    return nc


def build_B(ntile, nfirst, final):
    nc = bass.Bass("TRN2", target_bir_lowering=False)
    N = ntile * 128
    dt = lambda n, s, d=F32, k="ExternalInput": nc.dram_tensor(n, list(s), d, kind=k).ap()
    a = {}
    for n, s in (("h_in", [N, 1024]), ("yT", [4, 256, N]), ("cA", [128, 8]), ("cB", [128, 8]), ("wmod", [1024, 6144]),
                 ("bmod", [6144]), ("g1", [1024]), ("g2", [1024]), ("wgate", [1024, 4096]), ("bgc", [128, 32]),
                 ("wbr", [4, 256, 1024]), ("wout", [1024, 1024]), ("gluw", [256, 256]), ("glub", [128, 2]),
                 ("wrouter", [1024, 16]), ("brouter", [16]), ("weg", [16, 1024, 512]), ("weu", [16, 1024, 512]),
                 ("wed", [16, 512, 1024]), ("fg", [1024]), ("identf", [128, 128])):
        a[n] = dt(n, s)
    a["identb"] = dt("identb", [128, 128], BF16)
    out = dt("out", [N, 1024], F32, "ExternalOutput")
    h_new = dt("h_new", [N, 1024], F32, "Internal")
    fT = dt("fT", [8, 128, N], BF16, "Internal")
    Wr = dt("Wr", [N, 16], F32, "Internal")
    with contextlib.ExitStack() as st:
        fw = FW(nc, st)
        psb = [fw.ps("bank%d" % i, [128, 512]) for i in range(8)]
        idb = fw.sb("idb", [128, 128], BF16)
        fw.dma(idb[:], a["identb"])
        idf = fw.sb("idf", [128, 128])
        fw.dma(idf[:], a["identf"])
        with fw_scope(fw):
            phase_b1a(fw, ntile, nfirst, a["h_in"], a["yT"], a["cA"], a["cB"], a["wmod"], a["bmod"], a["g1"], a["wgate"],
                      a["bgc"], a["wbr"], a["wout"], a["gluw"], a["glub"], idb, h_new, psb)
        with fw_scope(fw):
            phase_b1b(fw, ntile, nfirst, h_new, a["cA"], a["cB"], a["wmod"], a["bmod"], a["g2"], a["wrouter"], a["brouter"],
                      idb, idf, fT, Wr, psb)
        with fw_scope(fw):
            phase_b2(fw, ntile, nfirst, (ntile + 1) // 2, fT, Wr, h_new, a["cA"], a["cB"], a["wmod"], a["bmod"],
                     a["weg"], a["weu"], a["wed"], a["fg"] if final else None, out, psb)
        fw.finish()
    return nc


def kernel(x, c, ctx, c_ctx, w_mod, b_mod, norm1_g, norm2_g, w_in, dn_conv, dn_a_log, dn_dt_bias, dn_norm_g,
           s5_a_re, s5_a_im, s5_log_dt, s5_b_re, s5_b_im, s5_c_re, s5_c_im, s5_d, s5_glu_w, s5_glu_b,
           da_lambda, da_norm_g, gla_w_gate, gla_b_gate, gla_norm_g, w_branch, w_gate, b_gate, w_out,
           w_router, b_router, w_e_gate, w_e_up, w_e_down, final_g):
    f32 = lambda v: np.ascontiguousarray(np.asarray(v), dtype=np.float32)
    x, c, ctx, c_ctx = f32(x), f32(c), f32(ctx), f32(c_ctx)
    nb, TL, _ = x.shape
    TC = ctx.shape[1]
    assert nb == 4
    cfg = Cfg(TC, TL)
    T = cfg.T
    consts = _consts(cfg)
    cols = [my_cols(j) for j in range(2)]
    h_lat, h_ctx = x.copy(), ctx.copy()
    ncl, nll = TC // 128, TL // 128
    for L in range(DEPTH):
        with_ctx = L < DEPTH - 1
        lam_init = 0.8 - 0.6 * float(np.exp(-0.3 * L))
        ncA = build_A(cfg, lam_init, with_ctx)
        wl = f32(w_in[L])
        in_maps = []
        for core in range(8):
            b, j = core // 2, core % 2
            cj = cols[j]
            w_my = np.where(cj[None, :] >= 0, wl[:, np.maximum(cj, 0)], np.float32(0)).astype(np.float32)
            lam_col, logdt_col, b_sm, c_nat = _s5_layout(f32(s5_a_re[L]), f32(s5_a_im[L]), f32(s5_log_dt[L]), f32(s5_b_re[L]),
                                                         f32(s5_b_im[L]), f32(s5_c_re[L]), f32(s5_c_im[L]), j)
            dnc = f32(dn_conv[L])
            convw = np.stack([np.ascontiguousarray(dnc[:, a * 256 + 128 * j: a * 256 + 128 * j + 128].T) for a in range(3)])
            m = dict(h_ctx=h_ctx[b], h_lat=h_lat[b], c_lat=_col(c[b], 8), c_ctx=_col(c_ctx, 8), wmod=f32(w_mod[L]),
                     bmod=f32(b_mod[L]), g1=f32(norm1_g[L]), w_my=w_my, lamb=f32(da_lambda[L]).reshape(128),
                     da_g=f32(da_norm_g[L]).reshape(64, 1), wg2=f32(gla_w_gate[L][:, :, 64 * j:64 * j + 64]),
                     bg2=f32(gla_b_gate[L][:, 64 * j:64 * j + 64]).reshape(2, 64, 1), gla_g=f32(gla_norm_g[L]),
                     convw=f32(convw), alog=f32(dn_a_log[L][:, 2 * j:2 * j + 2]).reshape(4, 1),
                     dtb=f32(dn_dt_bias[L][:, 2 * j:2 * j + 2]).reshape(4, 1), dn_g=f32(dn_norm_g[L]),
                     lam_col=lam_col, logdt_col=logdt_col, b_sm=b_sm, c_nat=c_nat,
                     dcol=f32(s5_d[L][128 * j:128 * j + 128]).reshape(128, 1))
            m.update(consts)
            in_maps.append(m)
        resA = run_bass_kernel_spmd(ncA, in_maps, core_ids=list(range(8))).results
        if with_ctx:
            nt = (ncl + nll) // 2
            nfirst = ncl
        else:
            nt = nll // 2
            nfirst = 0
        final = (L == DEPTH - 1)
        ncB = build_B(nt, nfirst, final)
        in_maps = []
        for core in range(8):
            b, j = core // 2, core % 2
            yfull = np.concatenate([np.asarray(resA[2 * b]["yT"]), np.asarray(resA[2 * b + 1]["yT"])], axis=1)
            if with_ctx:
                hj = np.concatenate([h_ctx[b], h_lat[b]], axis=0)
                t0 = j * nt * 128
            else:
                hj = h_lat[b]
                t0 = j * nt * 128
                yfull = yfull[:, :, TC:]
            N = nt * 128
            cA = _col(c_ctx, 8) if (with_ctx and j == 0) else _col(c[b], 8)
            m = dict(h_in=np.ascontiguousarray(hj[t0:t0 + N]), yT=np.ascontiguousarray(yfull[:, :, t0:t0 + N]),
                     cA=cA, cB=_col(c[b], 8), wmod=f32(w_mod[L]), bmod=f32(b_mod[L]), g1=f32(norm1_g[L]), g2=f32(norm2_g[L]),
                     wgate=f32(w_gate[L]), bgc=_col(b_gate[L], 32), wbr=f32(w_branch[L]), wout=f32(w_out[L]),
                     gluw=f32(s5_glu_w[L]), glub=_col(s5_glu_b[L], 2), wrouter=f32(w_router), brouter=f32(b_router),
                     weg=f32(w_e_gate[L]), weu=f32(w_e_up[L]), wed=f32(w_e_down[L]), fg=f32(final_g),
                     identf=consts["identf"], identb=consts["identb"])
            in_maps.append(m)
        resB = run_bass_kernel_spmd(ncB, in_maps, core_ids=list(range(8))).results
        for core in range(8):
            b, j = core // 2, core % 2
            o = np.asarray(resB[core]["out"])
            N = nt * 128
            t0 = j * N
            if with_ctx:
                full = np.concatenate([h_ctx[b], h_lat[b]], axis=0)
                full[t0:t0 + N] = o
                h_ctx[b] = full[:TC]
                h_lat[b] = full[TC:]
            else:
                h_lat[b, t0:t0 + N] = o
    return h_lat.astype(np.float32)


_AJ_IN = [("w_my", [1024, NCOL]), ("wg2", [2, 16, 64]), ("bg2", [2, 64, 1]), ("convw", [3, 128, 5]), ("alog", [4, 1]),
          ("dtb", [4, 1]), ("lam_col", [2, 2, 128, 4]), ("logdt_col", [2, 128, 4]), ("b_sm", [2, 128, 4, 16]),
          ("c_nat", [2, 2, 128, 64]), ("dcol", [128, 1])]
_AL_IN = [("wmod", [1024, 6144]), ("bmod", [6144]), ("g1", [1024]), ("g2", [1024]), ("lamb", [128]), ("da_g", [64, 1]),
          ("gla_g", [64]), ("dn_g", [64]), ("wgate", [1024, 4096]), ("bgc", [128, 32]), ("wbr", [4, 256, 1024]),
          ("wout", [1024, 1024]), ("gluw", [256, 256]), ("glub", [128, 2]), ("weg", [16, 1024, 512]),
          ("weu", [16, 1024, 512]), ("wed", [16, 512, 1024])]


def build_fused(cfg):
    nc = bass.Bass("TRN2", target_bir_lowering=False)
    T, TC, TL, B = cfg.T, cfg.TC, cfg.TL, blk_size(cfg)
    dt = lambda n, s, d=F32, k="ExternalInput": nc.dram_tensor(n, list(s), d, kind=k).ap()
    a = {}
    a["h0"] = dt("h0", [T, 1024])
    for n, s in (("c_lat", [128, 8]), ("c_ctx", [128, 8]), ("wrouter", [1024, 16]), ("brouter", [16]), ("fg", [1024])):
        a[n] = dt(n, s)
    for L in range(DEPTH):
        for n, s in _AL_IN:
            a["%s_%d" % (n, L)] = dt("%s_%d" % (n, L), s)
        for j in range(2):
            for n, s in _AJ_IN:
                a["%s_%d%d" % (n, L, j)] = dt("%s_%d%d" % (n, L, j), s)
    cshape = dict(identb=([128, 128], BF16), identf=([128, 128], F32), rope=([cfg.TL, 32], F32), sel65=([65, 64], F32),
                  ones64=([64, 64], F32), maskU=([64, 64], F32), cmask=([64, B], F32), cmask2=([2, B], F32), Jr=([64, 64], F32),
                  onesbd=([128, 128], F32), Eh=([2, 2, 64], F32), mLs=([64, 64], F32), mUs=([64, 64], F32), mUi=([64, 64], F32),
                  svals=([128, B + 1], F32), mask4=([4, 128, 128], F32), mask4T=([4, 128, 128], F32))
    for n, (s, d) in cshape.items():
        a[n] = dt(n, s, d)
    out = dt("out", [TL, 1024], F32, "ExternalOutput")
    I = "Internal"
    h1 = dt("h1", [T, 1024], F32, I)
    yT = dt("yT", [4, 256, T], F32, I)
    p_tok = dt("p_tok", [T, NCOL], F32, I)
    p_T = dt("p_T", [12, 128, T], F32, I)
    dn_qkv = dt("dn_qkv", [3, 128, T], F32, I)
    dn_gates = dt("dn_gates", [2, 4, T], F32, I)
    o_gla = dt("o_gla", [2, T, 128], F32, I)
    o_dn = dt("o_dn", [2, T, 128], F32, I)
    ys5 = dt("ys5", [2, 128, T], F32, I)
    h_new = dt("h_new", [T, 1024], F32, I)
    fT = dt("fT", [8, 128, T], BF16, I)
    Wr = dt("Wr", [T, 16], F32, I)
    with contextlib.ExitStack() as st:
        fw = FW(nc, st)
        psb = [fw.ps("bank%d" % i, [128, 512]) for i in range(8)]
        idb = fw.sb("idb", [128, 128], BF16)
        fw.dma(idb[:], a["identb"])
        idf = fw.sb("idf", [128, 128])
        fw.dma(idf[:], a["identf"])
        import os as _os
        for L in range(1 if _os.environ.get("FUSED_PROBE") else DEPTH):
            lam_init = 0.8 - 0.6 * float(np.exp(-0.3 * L))
            g = lambda n: a["%s_%d" % (n, L)]
            hsrc = a["h0"] if L == 0 else h1
            for j in range(2):
                gj = lambda n: a["%s_%d%d" % (n, L, j)]
                ys = lambda i: yT[i, 128 * j:128 * j + 128, :]
                with fw_scope(fw):
                    modc = fw.sb("modc", [128, 2048])
                    modl = fw.sb("modl", [128, 2048])
                    phase_mod(fw, a["c_ctx"], g("wmod"), g("bmod"), 0, 2048, modc, psb[7])
                    phase_mod(fw, a["c_lat"], g("wmod"), g("bmod"), 0, 2048, modl, psb[7])
                    phase_a1(fw, cfg, hsrc[0:TC, :], hsrc[TC:T, :], gj("w_my"), g("g1"), modc, modl, idb, p_tok, p_T, psb)
                with fw_scope(fw):
                    s65 = fw.sb("s65", [65, 64])
                    fw.dma(s65[:], a["sel65"])
                    o64 = fw.sb("o64", [64, 64])
                    fw.dma(o64[:], a["ones64"])
                    phase_da(fw, cfg, p_tok, a["rope"], g("lamb"), g("da_g"), lam_init, True, idb, s65, o64, ys(2), psb)
                with fw_scope(fw):
                    Jsb = fw.sb("Jsb", [64, 64])
                    fw.dma(Jsb[:], a["Jr"])
                    phase_gla(fw, cfg, p_tok, p_T, gj("wg2"), gj("bg2"), g("gla_g"), a["maskU"], a["cmask"], idf, Jsb,
                              o_gla, ys(3), psb)
                with fw_scope(fw):
                    phase_gla_fin(fw, cfg, p_tok, o_gla, g("gla_g"), idf, ys(3), psb, ctile=T_GR)
                with fw_scope(fw):
                    obd = fw.sb("obd", [128, 128])
                    fw.dma(obd[:], a["onesbd"])
                    phase_dn_prep(fw, cfg, p_T, gj("convw"), gj("alog"), gj("dtb"), obd, dn_qkv, dn_gates, psb)
                with fw_scope(fw):
                    Jsb = fw.sb("Jsb2", [64, 64])
                    fw.dma(Jsb[:], a["Jr"])
                    phase_dn(fw, cfg, dn_qkv, dn_gates, a["cmask2"], a["Eh"], a["mLs"], a["mUs"], a["mUi"], idf, Jsb,
                             o_dn, psb)
                with fw_scope(fw):
                    phase_gla_fin(fw, cfg, p_tok, o_dn, g("dn_g"), idf, ys(0), psb, ctile=T_DNZ)
                with fw_scope(fw):
                    phase_s5(fw, cfg, p_T, gj("lam_col"), gj("logdt_col"), gj("b_sm"), gj("c_nat"), a["svals"], a["mask4"],
                             a["mask4T"], idf, ys5, psb)
                with fw_scope(fw):
                    phase_s5_fin(fw, cfg, p_T, ys5, gj("dcol"), ys(1))
            last = (L == DEPTH - 1)
            if not last:
                ntile, nfirst, hin, yv, dst = T // 128, TC // 128, hsrc, yT, h1
            else:
                ntile, nfirst, hin, yv, dst = TL // 128, 0, hsrc[TC:T, :], yT[:, :, TC:T], out
            N = ntile * 128
            with fw_scope(fw):
                phase_b1a(fw, ntile, nfirst, hin, yv, a["c_ctx"], a["c_lat"], g("wmod"), g("bmod"), g("g1"), g("wgate"),
                          g("bgc"), g("wbr"), g("wout"), g("gluw"), g("glub"), idb, h_new[0:N, :], psb)
            with fw_scope(fw):
                phase_b1b(fw, ntile, nfirst, h_new[0:N, :], a["c_ctx"], a["c_lat"], g("wmod"), g("bmod"), g("g2"),
                          a["wrouter"], a["brouter"], idb, idf, fT[:, :, 0:N], Wr[0:N, :], psb)
            with fw_scope(fw):
                phase_b2(fw, ntile, nfirst, 17, fT[:, :, 0:N], Wr[0:N, :], h_new[0:N, :], a["c_ctx"], a["c_lat"],
                         g("wmod"), g("bmod"), g("weg"), g("weu"), g("wed"), a["fg"] if last else None, dst, psb)
        fw.finish()
    return nc


def kernel_fused(x, c, ctx, c_ctx, w_mod, b_mod, norm1_g, norm2_g, w_in, dn_conv, dn_a_log, dn_dt_bias, dn_norm_g,
                 s5_a_re, s5_a_im, s5_log_dt, s5_b_re, s5_b_im, s5_c_re, s5_c_im, s5_d, s5_glu_w, s5_glu_b,
                 da_lambda, da_norm_g, gla_w_gate, gla_b_gate, gla_norm_g, w_branch, w_gate, b_gate, w_out,
                 w_router, b_router, w_e_gate, w_e_up, w_e_down, final_g):
    f32 = lambda v: np.ascontiguousarray(np.asarray(v), dtype=np.float32)
    x, c, ctx, c_ctx = f32(x), f32(c), f32(ctx), f32(c_ctx)
    nb, TL, _ = x.shape
    TC = ctx.shape[1]
    cfg = Cfg(TC, TL)
    consts = _consts(cfg)
    cols = [my_cols(j) for j in range(2)]
    shared = dict(consts)
    shared.update(c_ctx=_col(c_ctx, 8), wrouter=f32(w_router), brouter=f32(b_router), fg=f32(final_g))
    for L in range(DEPTH):
        wl = f32(w_in[L])
        shared.update({"wmod_%d" % L: f32(w_mod[L]), "bmod_%d" % L: f32(b_mod[L]), "g1_%d" % L: f32(norm1_g[L]),
                       "g2_%d" % L: f32(norm2_g[L]), "lamb_%d" % L: f32(da_lambda[L]).reshape(128),
                       "da_g_%d" % L: f32(da_norm_g[L]).reshape(64, 1), "gla_g_%d" % L: f32(gla_norm_g[L]),
                       "dn_g_%d" % L: f32(dn_norm_g[L]), "wgate_%d" % L: f32(w_gate[L]), "bgc_%d" % L: _col(b_gate[L], 32),
                       "wbr_%d" % L: f32(w_branch[L]), "wout_%d" % L: f32(w_out[L]), "gluw_%d" % L: f32(s5_glu_w[L]),
                       "glub_%d" % L: _col(s5_glu_b[L], 2), "weg_%d" % L: f32(w_e_gate[L]), "weu_%d" % L: f32(w_e_up[L]),
                       "wed_%d" % L: f32(w_e_down[L])})
        for j in range(2):
            cj = cols[j]
            w_my = np.where(cj[None, :] >= 0, wl[:, np.maximum(cj, 0)], np.float32(0)).astype(np.float32)
            lam_col, logdt_col, b_sm, c_nat = _s5_layout(f32(s5_a_re[L]), f32(s5_a_im[L]), f32(s5_log_dt[L]), f32(s5_b_re[L]),
                                                         f32(s5_b_im[L]), f32(s5_c_re[L]), f32(s5_c_im[L]), j)
            dnc = f32(dn_conv[L])
            convw = np.stack([np.ascontiguousarray(dnc[:, q * 256 + 128 * j: q * 256 + 128 * j + 128].T) for q in range(3)])
            sfx = "_%d%d" % (L, j)
            shared.update({"w_my" + sfx: w_my, "wg2" + sfx: f32(gla_w_gate[L][:, :, 64 * j:64 * j + 64]),
                           "bg2" + sfx: f32(gla_b_gate[L][:, 64 * j:64 * j + 64]).reshape(2, 64, 1), "convw" + sfx: f32(convw),
                           "alog" + sfx: f32(dn_a_log[L][:, 2 * j:2 * j + 2]).reshape(4, 1),
                           "dtb" + sfx: f32(dn_dt_bias[L][:, 2 * j:2 * j + 2]).reshape(4, 1),
                           "lam_col" + sfx: lam_col, "logdt_col" + sfx: logdt_col, "b_sm" + sfx: b_sm, "c_nat" + sfx: c_nat,
                           "dcol" + sfx: f32(s5_d[L][128 * j:128 * j + 128]).reshape(128, 1)})
    ncf = build_fused(cfg)
    in_maps = []
    for core in range(8):
        b = core // 2
        m = dict(shared)
        m["h0"] = np.ascontiguousarray(np.concatenate([ctx[b], x[b]], axis=0))
        m["c_lat"] = _col(c[b], 8)
        in_maps.append(m)
    res = run_bass_kernel_spmd(ncf, in_maps, core_ids=list(range(8))).results
    return np.stack([np.asarray(res[2 * b]["out"]) for b in range(nb)]).astype(np.float32)


kernel_unfused = kernel
kernel = kernel_fused
```
